# Optimizing a Trainium2 kernel written in Bass

```python
import math
import jax, jax.numpy as jnp
from jax import lax
import numpy as np

D_MODEL = 1024
BATCH = 8
SEQ = 2048
DEPTH = 2

GRID_W = 64
CTX_LEN = 256
MIX_W = D_MODEL
GROUP_W = MIX_W // 4
ATTN_HEADS = 4
ATTN_D = GROUP_W // (2 * ATTN_HEADS)
ATTN_VD = 2 * ATTN_D
ROPE_THETA = 10000.0
Q_BLOCK = 128
POOL_WINDOWS = (2, 4, 8, 16)
POOL_GROUPS = 4
POOL_GW = GROUP_W // POOL_GROUPS
CONV_K = 31
SGU_CHUNK = 128
SGU_GROUPS = 4
SGU_GW = GROUP_W // SGU_GROUPS
N_EXPERTS = 16
EC_CAPACITY = 2
D_EXPERT = 2 * D_MODEL
N_MOD = 6
IN_W = 3 * GROUP_W + GROUP_W + 2 * GROUP_W + 2 * GROUP_W
SPLITS = (GROUP_W, 2 * GROUP_W, 3 * GROUP_W, 4 * GROUP_W, 6 * GROUP_W)
EPS = 1e-6

kernel_name = "hybrid_diffusion_parallel_heads_ec_moe"


def rms_norm(x, g):
    xf = x.astype(jnp.float32)
    y = xf * lax.rsqrt(jnp.mean(xf * xf, axis=-1, keepdims=True) + EPS)
    return (y * g.astype(jnp.float32)).astype(x.dtype)


def layer_norm(x, g, b):
    xf = x.astype(jnp.float32)
    mu = jnp.mean(xf, axis=-1, keepdims=True)
    var = jnp.mean(jnp.square(xf - mu), axis=-1, keepdims=True)
    y = (xf - mu) * lax.rsqrt(var + EPS) * g.astype(jnp.float32) + b.astype(jnp.float32)
    return y.astype(x.dtype)


def modulation(cond, w_mod, b_mod):
    m = jax.nn.silu(cond) @ w_mod + b_mod
    return jnp.split(m, N_MOD, axis=-1)


def axial_rope_tables(n_tokens, dtype):
    rows_n = n_tokens // GRID_W
    rows = jnp.repeat(jnp.arange(rows_n), GRID_W).astype(jnp.float32)
    cols = jnp.tile(jnp.arange(GRID_W), rows_n).astype(jnp.float32)
    n_freq = ATTN_D // 4
    inv = ROPE_THETA ** (-jnp.arange(n_freq, dtype=jnp.float32) / n_freq)
    ang_r = rows[:, None] * inv
    ang_c = cols[:, None] * inv
    shp = (n_tokens, 1, 1, n_freq)
    return tuple(t.reshape(shp).astype(dtype) for t in
                 (jnp.cos(ang_r), jnp.sin(ang_r), jnp.cos(ang_c), jnp.sin(ang_c)))


def rotate_axis(x, cos, sin):
    n = x.shape[-1] // 2
    x1, x2 = x[..., :n], x[..., n:]
    return jnp.concatenate([x1 * cos - x2 * sin, x1 * sin + x2 * cos], axis=-1)


def apply_rope2d(x, rope):
    cos_r, sin_r, cos_c, sin_c = rope
    h = x.shape[-1] // 2
    return jnp.concatenate([rotate_axis(x[..., :h], cos_r, sin_r),
                            rotate_axis(x[..., h:], cos_c, sin_c)], axis=-1)


def qk_heads(p, g, rope):
    B, T, _ = p.shape
    a = rms_norm(p.reshape(B, T, ATTN_HEADS, 2, ATTN_D), g)
    if rope is not None:
        a = apply_rope2d(a, rope)
    return a[..., 0, :], a[..., 1, :]


def diff_softmax_attention(q1, q2, k1, k2, v, lam):
    scale = ATTN_D ** -0.5
    s1 = jnp.einsum('bqhd,bkhd->bhqk', q1, k1).astype(jnp.float32) * scale
    s2 = jnp.einsum('bqhd,bkhd->bhqk', q2, k2).astype(jnp.float32) * scale
    w = jax.nn.softmax(s1, axis=-1) - lam * jax.nn.softmax(s2, axis=-1)
    return jnp.einsum('bhqk,bkhe->bqhe', w.astype(v.dtype), v)


def latent_diff_attention(q1, q2, k1, k2, v, lam):
    B, T, H, _ = q1.shape
    nb = T // Q_BLOCK

    def blocks(q):
        return q.reshape(B, nb, Q_BLOCK, H, q.shape[-1]).swapaxes(0, 1)

    out = lax.map(lambda qs: diff_softmax_attention(qs[0], qs[1], k1, k2, v, lam),
                  (blocks(q1), blocks(q2)))
    return out.swapaxes(0, 1).reshape(B, T, H, ATTN_VD)


def attn_head_out(o, g, lam_init):
    B, T = o.shape[:2]
    return (rms_norm(o, g) * (1.0 - lam_init)).reshape(B, T, GROUP_W)


def pool_mixer(z, w_pool, b_pool, pool_scale):
    B, T, _ = z.shape
    zg = z.reshape(B, T, POOL_GROUPS, POOL_GW)
    csum = jnp.concatenate([jnp.zeros((B, 1, POOL_GROUPS, POOL_GW), jnp.float32),
                            jnp.cumsum(zg.astype(jnp.float32), axis=1)], axis=1)
    t = jnp.arange(T)
    outs = []
    for g, w in enumerate(POOL_WINDOWS):
        lo = jnp.clip(t - w // 2, 0, T)
        hi = jnp.clip(t + w // 2, 0, T)
        cnt = (hi - lo).astype(jnp.float32)[None, :, None]
        mean = (csum[:, hi, g] - csum[:, lo, g]) / cnt
        d = mean.astype(z.dtype) - zg[:, :, g]
        outs.append(d @ w_pool[g] + b_pool[g])
    return jnp.concatenate(outs, axis=-1) * pool_scale


def conv_module(z, conv_w, conv_b, ln_g, ln_b, w_pw2):
    val, gate = jnp.split(z, 2, axis=-1)
    y = val * jax.nn.sigmoid(gate)
    y = lax.conv_general_dilated(y, conv_w[:, None, :], window_strides=(1,),
                                 padding=[(CONV_K // 2, CONV_K // 2)],
                                 dimension_numbers=('NWC', 'WIO', 'NWC'),
                                 feature_group_count=GROUP_W) + conv_b
    y = jax.nn.silu(layer_norm(y, ln_g, ln_b))
    return y @ w_pw2


def spatial_gating(z, ln_g, ln_b, w_spatial, b_spatial):
    B, T, _ = z.shape
    u, v = jnp.split(jax.nn.gelu(z), 2, axis=-1)
    v = layer_norm(v, ln_g, ln_b).reshape(B, T // SGU_CHUNK, SGU_CHUNK, SGU_GROUPS, SGU_GW)
    s = jnp.einsum('gpq,bnqgc->bnpgc', w_spatial, v) + b_spatial.T[:, :, None]
    return u * s.reshape(B, T, GROUP_W)


def expert_choice_moe(h, w_router, w_gate, w_up, w_down):
    B, T, D = h.shape
    cap = EC_CAPACITY * T // N_EXPERTS
    aff = jax.nn.softmax((h @ w_router).astype(jnp.float32), axis=-1)
    gates, idx = lax.top_k(aff.swapaxes(1, 2), cap)
    xs = jax.vmap(lambda hb, ib: hb[ib])(h, idx)
    a = jnp.einsum('becd,edf->becf', xs, w_gate)
    u = jnp.einsum('becd,edf->becf', xs, w_up)
    y = jnp.einsum('becf,efd->becd', jax.nn.silu(a) * u, w_down)
    y = y * gates[..., None].astype(y.dtype)
    return jax.vmap(lambda ib, yb: jnp.zeros((T, D), yb.dtype).at[ib.reshape(-1)].add(
        yb.reshape(-1, D)))(idx, y)


def setup_inputs(seed: int = 0) -> dict:
    key = jax.random.key(seed)
    ks = iter(jax.random.split(key, 40))

    def nrm(shape, scale):
        return jax.random.normal(next(ks), shape, jnp.float32) * scale

    def gain(shape):
        return 1.0 + nrm(shape, 0.01)

    L, D = DEPTH, D_MODEL
    return {
        "x": nrm((BATCH, SEQ, D), 1.0),
        "c": nrm((BATCH, D), 1.0),
        "ctx": nrm((BATCH, CTX_LEN, D), 1.0),
        "c_ctx": nrm((D,), 1.0),
        "w_mod": nrm((L, D, N_MOD * D), 0.5 * D ** -0.5),
        "b_mod": nrm((L, N_MOD * D), 0.01),
        "g_norm1": gain((L, D)),
        "g_norm2": gain((L, D)),
        "w_in": nrm((L, D, IN_W), D ** -0.5),
        "w_out": nrm((L, MIX_W, D), MIX_W ** -0.5),
        "g_q": gain((L, ATTN_D)),
        "g_k": gain((L, ATTN_D)),
        "lam_q1": nrm((L, ATTN_D), 0.1),
        "lam_k1": nrm((L, ATTN_D), 0.1),
        "lam_q2": nrm((L, ATTN_D), 0.1),
        "lam_k2": nrm((L, ATTN_D), 0.1),
        "g_attn_out": gain((L, ATTN_VD)),
        "w_pool": nrm((L, POOL_GROUPS, POOL_GW, POOL_GW), POOL_GW ** -0.5),
        "b_pool": nrm((L, POOL_GROUPS, POOL_GW), 0.01),
        "pool_scale": gain((L, GROUP_W)),
        "conv_w": nrm((L, CONV_K, GROUP_W), CONV_K ** -0.5),
        "conv_b": nrm((L, GROUP_W), 0.01),
        "conv_ln_g": gain((L, GROUP_W)),
        "conv_ln_b": nrm((L, GROUP_W), 0.01),
        "w_pw2": nrm((L, GROUP_W, GROUP_W), GROUP_W ** -0.5),
        "sgu_ln_g": gain((L, GROUP_W)),
        "sgu_ln_b": nrm((L, GROUP_W), 0.01),
        "w_spatial": nrm((L, SGU_GROUPS, SGU_CHUNK, SGU_CHUNK), SGU_CHUNK ** -0.5),
        "b_spatial": gain((L, SGU_GROUPS, SGU_CHUNK)),
        "w_router": nrm((L, D, N_EXPERTS), D ** -0.5),
        "w_gate": nrm((L, N_EXPERTS, D, D_EXPERT), D ** -0.5),
        "w_up": nrm((L, N_EXPERTS, D, D_EXPERT), D ** -0.5),
        "w_down": nrm((L, N_EXPERTS, D_EXPERT, D), D_EXPERT ** -0.5),
    }


def reference(x, c, ctx, c_ctx, w_mod, b_mod, g_norm1, g_norm2, w_in, w_out, g_q, g_k,
              lam_q1, lam_k1, lam_q2, lam_k2, g_attn_out, w_pool, b_pool, pool_scale,
              conv_w, conv_b, conv_ln_g, conv_ln_b, w_pw2, sgu_ln_g, sgu_ln_b,
              w_spatial, b_spatial, w_router, w_gate, w_up, w_down):
    B, T, _ = x.shape
    rope = axial_rope_tables(T, x.dtype)
    for l in range(DEPTH):
        last = l == DEPTH - 1
        lam_init = 0.8 - 0.6 * math.exp(-0.3 * l)
        lam = (jnp.exp(jnp.sum(lam_q1[l].astype(jnp.float32) * lam_k1[l].astype(jnp.float32)))
               - jnp.exp(jnp.sum(lam_q2[l].astype(jnp.float32) * lam_k2[l].astype(jnp.float32)))
               + lam_init)
        sh1, sc1, gt1, sh2, sc2, gt2 = modulation(c[:, None, :], w_mod[l], b_mod[l])
        csh1, csc1, cgt1, csh2, csc2, cgt2 = modulation(c_ctx[None, None, :], w_mod[l], b_mod[l])
        w_in_l = w_in[l]

        hx = rms_norm(x, g_norm1[l]) * (1.0 + sc1) + sh1
        hc = rms_norm(ctx, g_norm1[l]) * (1.0 + csc1) + csh1
        xq_in, xk_in, xv_in, xpool_in, xconv_in, xsgu_in = jnp.split(hx @ w_in_l, SPLITS, axis=-1)
        if last:
            ck_in, cv_in = jnp.split(hc @ w_in_l[:, SPLITS[0]:SPLITS[2]], 2, axis=-1)
        else:
            cq_in, ck_in, cv_in, cpool_in, cconv_in, csgu_in = jnp.split(hc @ w_in_l, SPLITS, axis=-1)

        ck1, ck2 = qk_heads(ck_in, g_k[l], None)
        cv = cv_in.reshape(B, -1, ATTN_HEADS, ATTN_VD)
        xq1, xq2 = qk_heads(xq_in, g_q[l], rope)
        xk1, xk2 = qk_heads(xk_in, g_k[l], rope)
        xv = xv_in.reshape(B, T, ATTN_HEADS, ATTN_VD)
        k1_all = jnp.concatenate([ck1, xk1], axis=1)
        k2_all = jnp.concatenate([ck2, xk2], axis=1)
        v_all = jnp.concatenate([cv, xv], axis=1)
        attn_x = attn_head_out(latent_diff_attention(xq1, xq2, k1_all, k2_all, v_all, lam),
                               g_attn_out[l], lam_init)
        mix_x = jnp.concatenate([
            attn_x,
            pool_mixer(xpool_in, w_pool[l], b_pool[l], pool_scale[l]),
            conv_module(xconv_in, conv_w[l], conv_b[l], conv_ln_g[l], conv_ln_b[l], w_pw2[l]),
            spatial_gating(xsgu_in, sgu_ln_g[l], sgu_ln_b[l], w_spatial[l], b_spatial[l]),
        ], axis=-1) @ w_out[l]
        x = x + gt1 * mix_x

        if not last:
            cq1, cq2 = qk_heads(cq_in, g_q[l], None)
            attn_c = attn_head_out(diff_softmax_attention(cq1, cq2, ck1, ck2, cv, lam),
                                   g_attn_out[l], lam_init)
            mix_c = jnp.concatenate([
                attn_c,
                pool_mixer(cpool_in, w_pool[l], b_pool[l], pool_scale[l]),
                conv_module(cconv_in, conv_w[l], conv_b[l], conv_ln_g[l], conv_ln_b[l], w_pw2[l]),
                spatial_gating(csgu_in, sgu_ln_g[l], sgu_ln_b[l], w_spatial[l], b_spatial[l]),
            ], axis=-1) @ w_out[l]
            ctx = ctx + cgt1 * mix_c
            hc2 = rms_norm(ctx, g_norm2[l]) * (1.0 + csc2) + csh2
            ctx = ctx + cgt2 * expert_choice_moe(hc2, w_router[l], w_gate[l], w_up[l], w_down[l])

        hx2 = rms_norm(x, g_norm2[l]) * (1.0 + sc2) + sh2
        x = x + gt2 * expert_choice_moe(hx2, w_router[l], w_gate[l], w_up[l], w_down[l])
    return x
```

```python
import contextlib
import math
import numpy as np
import concourse.bass as bass
import concourse.mybir as mybir
from concourse.bass_utils import run_bass_kernel_spmd

F32 = mybir.dt.float32
BF16 = mybir.dt.bfloat16
F32R = mybir.dt.float32r
FP8 = mybir.dt.float8e4
ALU = mybir.AluOpType
AF = mybir.ActivationFunctionType

ENGINES = ("sync", "scalar", "vector", "gpsimd", "tensor")
D = 1024
T = 2048
TC = 256
NE = 16
EPS = 1e-6
NV = 139
WARM_DUMMY = 2
PV_G1, PV_G2, PV_BPOOL, PV_PSC, PV_CONVB, PV_CLNG, PV_CLNB, PV_CONVW, PV_GQ, PV_GK, PV_GAO, PV_BMOD = (
    0, 8, 16, 18, 20, 22, 24, 26, 88, 89, 90, 91)
C_PERM, C_BD32, C_BD64, C_O1024, C_O256, C_IDENT, C_IOTA, C_JIDX, C_PRW, C_PFIX = (
    0, 128, 256, 384, 512, 640, 768, 1024, 1027, 1029)
NCST = 1029 + 32


class DmaSlot:
    def __init__(self, name):
        self.name = name
        self.sem = None
        self.count = 0


class _Op:
    __slots__ = ("eng", "fn", "deps", "raw", "signal", "sigval", "slot", "idx", "is_mm")

    def __init__(self, eng, fn, slot, is_mm):
        self.eng = eng
        self.fn = fn
        self.deps = set()
        self.raw = set()
        self.signal = False
        self.sigval = 0
        self.slot = slot
        self.is_mm = is_mm


class Prog:
    def __init__(self, nc):
        self.nc = nc
        self.ops = []
        self.last_writer = {}
        self.readers = {}
        self.slots = []
        self.last_on_eng = {}
        self.pool = {"hw": [self.slot(f"h{i}") for i in range(48)], "sw": [self.slot(f"s{i}") for i in range(48)]}
        self.pool_rr = {"hw": 0, "sw": 0}

    def slot(self, name):
        s = DmaSlot(name)
        self.slots.append(s)
        return s

    def op(self, eng, fn, reads=(), writes=(), slot=None, is_mm=False):
        o = _Op(eng, fn, slot, is_mm)
        o.idx = len(self.ops)
        for k in reads:
            w = self.last_writer.get(k)
            if w is not None:
                o.deps.add(w)
                o.raw.add(w)
        for k in writes:
            w = self.last_writer.get(k)
            if w is not None:
                o.deps.add(w)
            for r in self.readers.get(k, ()):
                o.deps.add(r)
        for k in reads:
            self.readers.setdefault(k, []).append(o.idx)
        for k in writes:
            self.last_writer[k] = o.idx
            self.readers[k] = []
        o.deps.discard(o.idx)
        self.ops.append(o)
        self.last_on_eng[(eng, slot)] = o.idx
        return o

    def dma(self, eng, out, in_, reads=(), writes=(), slot=None, **kw):
        kind = "sw" if eng == "gpsimd" else "hw"
        slot = self.pool[kind][self.pool_rr[kind] % len(self.pool[kind])]
        self.pool_rr[kind] += 1
        return self.op(eng, lambda e: e.dma_start(out=out, in_=in_, **kw), reads, writes, slot=slot)

    def barrier(self):
        last = set(self.last_on_eng.values())
        for e in ENGINES:
            o = self.op(e, lambda en: en.nop())
            o.deps |= {i for i in last if i != o.idx}
            o.raw |= o.deps

    def wait_all(self, eng, keys):
        return self.op(eng, lambda e: e.nop(), reads=keys)

    def _needs_wait(self, o, d):
        if d.slot is not None:
            return True
        if d.eng != o.eng:
            return True
        if o.slot is not None:
            return True
        if d.is_mm and o.is_mm:
            return False
        return True

    def emit(self, st):
        nc = self.nc
        ops = self.ops
        for o in ops:
            for di in o.deps:
                d = ops[di]
                if d.slot is None and self._needs_wait(o, d):
                    d.signal = True
        cnt = {e: 0 for e in ENGINES}
        for o in ops:
            if o.slot is not None:
                o.slot.count += 16
                o.sigval = o.slot.count
            elif o.signal:
                cnt[o.eng] += 1
                o.sigval = cnt[o.eng]
        esem = {e: st.enter_context(nc.semaphore("s_" + e)) for e in ENGINES}
        for s in self.slots:
            s.sem = st.enter_context(nc.semaphore("d_" + s.name))
        block = st.enter_context(nc.Block())

        def make(ename):
            def body(eng):
                known = {}
                for o in ops:
                    if o.eng != ename:
                        continue
                    need = {}
                    for di in o.deps:
                        d = ops[di]
                        if not self._needs_wait(o, d):
                            continue
                        sem = d.slot.sem if d.slot is not None else esem[d.eng]
                        k = id(sem)
                        if need.get(k, (None, 0))[1] < d.sigval:
                            need[k] = (sem, d.sigval)
                    for k, (sem, val) in need.items():
                        if known.get(k, 0) >= val:
                            continue
                        eng.wait_ge(sem, val)
                        known[k] = val
                    ins = o.fn(eng)
                    if o.slot is not None:
                        ins.then_inc(o.slot.sem, 16)
                    elif o.signal:
                        ins.then_inc(esem[ename], 1)
            return body

        for ename in ENGINES:
            if any(o.eng == ename for o in ops):
                getattr(block, ename)(make(ename))


class Arena:
    def __init__(self, tile, nwords):
        self.t = tile
        self.n = nwords
        self.off = 0

    def mark(self):
        return self.off

    def reset(self, to=0):
        self.off = to

    def alloc(self, free_shape, dtype, parts=128):
        n = int(np.prod(free_shape))
        esz = 2 if dtype == BF16 else (1 if dtype == FP8 else 4)
        words = (n * esz + 3) // 4
        words = (words + 7) // 8 * 8
        assert self.off + words <= self.n, ("arena overflow", self.off, words, self.n)
        ap = self.t[0:parts, self.off:self.off + words]
        self.off += words
        if dtype == BF16 or dtype == FP8:
            ap = ap.bitcast(dtype)
        ap = ap[:, 0:n]
        if len(free_shape) == 2:
            ap = ap.rearrange("p (a b) -> p a b", a=free_shape[0])
        elif len(free_shape) == 3:
            ap = ap.rearrange("p (a b c) -> p a b c", a=free_shape[0], b=free_shape[1])
        return ap


def _consts():
    cst = np.zeros((128, NCST), np.float32)
    p = np.arange(128)
    d = p % 32
    within = d % 16
    partner = np.where(within < 8, p + 8, p - 8)
    cst[partner, C_PERM + p] = 1.0
    for g in range(4):
        cst[g * 32:(g + 1) * 32, C_BD32 + g * 32:C_BD32 + (g + 1) * 32] = 1.0 / 32
    for g in range(2):
        cst[g * 64:(g + 1) * 64, C_BD64 + g * 64:C_BD64 + (g + 1) * 64] = 1.0 / 64
    cst[:, C_O1024:C_O1024 + 128] = 1.0 / 1024
    cst[:, C_O256:C_O256 + 128] = 1.0 / 256
    cst[p, C_IDENT + p] = 1.0
    cst[:, C_IOTA:C_IOTA + 256] = np.arange(256)[None, :]
    cst[:, C_JIDX] = p
    cst[:, C_JIDX + 1] = p + 128
    cst[:, C_JIDX + 2] = p
    wins = (2, 4, 8, 16)
    for c in range(2):
        for hf in range(2):
            w = wins[c * 2 + hf]
            rows = slice(hf * 64, hf * 64 + 64)
            cst[rows, C_PRW + c] = 1.0 / w
            for i in range(8):
                cnt_l = min(i + w // 2, 10 ** 9) - max(i - w // 2, 0)
                cst[rows, C_PFIX + c * 16 + i] = 1.0 / cnt_l
                cnt_r = min(w // 2, 8 - i) + w // 2
                cst[rows, C_PFIX + c * 16 + 8 + i] = 1.0 / cnt_r
    half = d // 16
    i_f = within % 8
    sign = np.where(within < 8, -1.0, 1.0)
    inv = 10000.0 ** (-(np.arange(8, dtype=np.float32)) / 8.0)
    t = np.arange(T)
    rows = (t // 64).astype(np.float32)
    cols = (t % 64).astype(np.float32)
    pos = np.where(half[:, None] == 0, rows[None, :], cols[None, :]).astype(np.float32)
    ang = pos * inv[i_f][:, None].astype(np.float32)
    rc = np.cos(ang).astype(np.float32)
    rs = (np.sin(ang) * sign[:, None]).astype(np.float32)
    return cst, np.ascontiguousarray(rc), np.ascontiguousarray(rs)


def _fm(v):
    return np.ascontiguousarray(v.reshape(-1, 128).T)


def _layer_arrays(inp, l):
    pv = np.zeros((128, NV), np.float32)
    pv[:, PV_G1:PV_G1 + 8] = _fm(inp["g_norm1"][l])
    pv[:, PV_G2:PV_G2 + 8] = _fm(inp["g_norm2"][l])
    pv[:, PV_BPOOL:PV_BPOOL + 2] = _fm(inp["b_pool"][l].reshape(-1))
    pv[:, PV_PSC:PV_PSC + 2] = _fm(inp["pool_scale"][l])
    pv[:, PV_CONVB:PV_CONVB + 2] = _fm(inp["conv_b"][l])
    pv[:, PV_CLNG:PV_CLNG + 2] = _fm(inp["conv_ln_g"][l])
    pv[:, PV_CLNB:PV_CLNB + 2] = _fm(inp["conv_ln_b"][l])
    cw = inp["conv_w"][l]
    for c in range(2):
        pv[:, PV_CONVW + c * 31:PV_CONVW + (c + 1) * 31] = cw[:, c * 128:(c + 1) * 128].T
    pv[:, PV_GQ] = np.tile(inp["g_q"][l], 4)
    pv[:, PV_GK] = np.tile(inp["g_k"][l], 4)
    pv[:, PV_GAO] = np.tile(inp["g_attn_out"][l], 2)
    pv[:, PV_BMOD:PV_BMOD + 48] = _fm(inp["b_mod"][l])
    rowv = np.concatenate([inp["sgu_ln_g"][l], inp["sgu_ln_b"][l], inp["lam_q1"][l], inp["lam_k1"][l],
                           inp["lam_q2"][l], inp["lam_k2"][l]]).astype(np.float32)
    wspT = np.ascontiguousarray(inp["w_spatial"][l].transpose(2, 0, 1))
    bs = inp["b_spatial"][l]
    bsp = np.zeros((128, 2, 128), np.float32)
    for cc in range(2):
        for hf in range(2):
            bsp[hf * 64:(hf + 1) * 64, cc, :] = bs[cc * 2 + hf][None, :]
    wr = np.ascontiguousarray(inp["w_router"][l].reshape(8, 128, NE).transpose(1, 0, 2))
    return {
        f"wmod{l}": inp["w_mod"][l], f"pvec{l}": pv, f"rowv{l}": rowv, f"w_in{l}": inp["w_in"][l],
        f"w_out{l}": inp["w_out"][l], f"wpool{l}": inp["w_pool"][l], f"wpw2{l}": inp["w_pw2"][l],
        f"wspT{l}": wspT, f"bsp{l}": bsp, f"wr{l}": wr,
        f"wg{l}": np.ascontiguousarray(inp["w_gate"][l].reshape(NE, 8, 128, 8, 256).transpose(0, 3, 2, 1, 4)),
        f"wu{l}": np.ascontiguousarray(inp["w_up"][l].reshape(NE, 8, 128, 8, 256).transpose(0, 3, 2, 1, 4)),
        f"wd{l}": np.ascontiguousarray(inp["w_down"][l].reshape(NE, 16, 128, 4, 256).transpose(0, 3, 2, 1, 4)),
    }


LAYER_SHAPES = {
    "wmod": [1024, 6144], "pvec": [128, NV], "rowv": [640], "w_in": [1024, 2048], "w_out": [1024, 1024],
    "wpool": [4, 64, 64], "wpw2": [256, 256], "wspT": [128, 4, 128], "bsp": [128, 2, 128], "wr": [128, 8, NE],
    "wg": [NE, 8, 128, 8, 256], "wu": [NE, 8, 128, 8, 256], "wd": [NE, 4, 128, 16, 256],
}


class Seq:
    def __init__(self, name, Tn, res, mcol, rope, h_ap, off):
        self.name = name
        self.Tn = Tn
        self.res = res
        self.mcol = mcol
        self.rope = rope
        self.h = h_ap
        self.off = off
        n = min(512, Tn)
        self.blocks = [(t0, n) for t0 in range(0, Tn, n)]
        self.nch = Tn // 128


class KB:
    def __init__(self, layers, dbg=()):
        self.layers = layers
        self.dbg = dbg
        self.nc = bass.Bass("TRN2", target_bir_lowering=False)
        self.P = Prog(self.nc)
        self._psrr = {}

    def tt(self, eng, out, in0, in1, op, r, w):
        return self.P.op(eng, lambda e: e.tensor_tensor(out=out, in0=in0, in1=in1, op=op), r, w)

    def ts(self, eng, out, in0, s1, s2, op0, op1=None, r=(), w=()):
        if op1 is None:
            return self.P.op(eng, lambda e: e.tensor_scalar(out=out, in0=in0, scalar1=s1, scalar2=None, op0=op0), r, w)
        return self.P.op(eng, lambda e: e.tensor_scalar(out=out, in0=in0, scalar1=s1, scalar2=s2, op0=op0, op1=op1), r, w)

    def stt(self, eng, out, in0, sc, in1, op0, op1, r, w):
        return self.P.op(eng, lambda e: e.scalar_tensor_tensor(out=out, in0=in0, scalar=sc, in1=in1, op0=op0, op1=op1), r, w)

    def cp(self, eng, out, in_, r, w):
        if eng == "scalar":
            return self.P.op(eng, lambda e: e.activation(out=out, in_=in_, func=AF.Copy), r, w)
        return self.P.op(eng, lambda e: e.tensor_copy(out=out, in_=in_), r, w)

    def act(self, out, in_, func, r, w, bias=None, scale=None):
        kw = {}
        if bias is not None:
            kw["bias"] = bias
        if scale is not None:
            kw["scale"] = scale
        return self.P.op("scalar", lambda e: e.activation(out=out, in_=in_, func=func, **kw), r, w)

    def mm(self, out, lhsT, rhs, start, stop, r, w, tp=None):
        kw = {}
        if tp is not None:
            kw["tile_position"] = tp
        return self.P.op("tensor", lambda e: e.matmul(out, lhsT=lhsT, rhs=rhs, start=start, stop=stop, **kw),
                         r, w, is_mm=True)

    def memset(self, eng, ap, val, w):
        return self.P.op(eng, lambda e: e.memset(ap, val), (), w)

    def ps(self, pool):
        lst = self.pspools[pool]
        i = self._psrr.get(pool, 0)
        self._psrr[pool] = i + 1
        b = lst[i % len(lst)]
        return self.PS[b], ("ps", b)

    def rstd_from(self, out, in_, r, w, tmpkey=None):
        self.act(out, in_, AF.Ln, r, w, bias=self.eps_ap[0:out.shape[0], :] if hasattr(out, "shape") else self.eps_ap)
        self.act(out, out, AF.Exp, w, w, scale=-0.5)

    def dump(self, name, ap, rkeys, shape):
        if name not in self.dbg:
            return
        d = self.nc.dram_tensor("dbg_" + name, list(shape), F32, kind="ExternalOutput").ap()
        self.P.dma("gpsimd", d, ap, reads=rkeys, writes=["dbgout_" + name], slot=self.s_out)
        self.outkeys.append("dbgout_" + name)

    def build(self):
        nc, P = self.nc, self.P
        st = contextlib.ExitStack()
        self.st = st
        dram = lambda n, s: nc.dram_tensor(n, list(s), F32, kind="ExternalInput").ap()
        self.I = {"xT": dram("xT", [D, T]), "cT": dram("cT", [D, TC]), "cvec": dram("cvec", [128, 8, 2]),
                  "cst": dram("cst", [128, NCST]), "rope_c": dram("rope_c", [128, T]), "rope_s": dram("rope_s", [128, T])}
        for l in self.layers:
            for k, s in LAYER_SHAPES.items():
                self.I[f"{k}{l}"] = dram(f"{k}{l}", s)
        self.out = nc.dram_tensor("outT", [D, T], F32, kind="ExternalOutput").ap()
        self.scr_pos = nc.dram_tensor("scr_pos", [2, NE, T + TC], BF16, kind="Internal").ap()
        self.scr_gate = nc.dram_tensor("scr_gate", [2, NE, T + TC], BF16, kind="Internal").ap()
        sb = lambda n, s, d: st.enter_context(nc.sbuf_tensor(n, s, d))
        self.xT = sb("xT_sb", [128, 8, T], F32)
        self.cT = sb("cT_sb", [128, 8, TC], F32)
        self.cst_p = sb("cst_sb", [128, NCST - 768], F32)
        self.cstR = sb("cstR_sb", [128, 768], F32R)
        self.pvec = sb("pvec_sb", [128, NV], F32)
        self.modT = sb("modT_sb", [128, 48, 2], F32)
        self.mA = sb("mA_sb", [128, 6, 8, 2], F32)
        self.small = sb("small_sb", [128, 64], F32)
        AW = 29328
        self.arena_t = sb("arena_sb", [128, AW], F32)
        self.A = Arena(self.arena_t, AW)
        self.arenaR_t = sb("arenaR_sb", [128, 3840], F32R)
        self.AR = Arena(self.arenaR_t, 3840)
        self.psall = st.enter_context(nc.psum_tensor("psall", [128, 4096], F32))
        self.PS = [self.psall[:, i * 512:(i + 1) * 512] for i in range(8)]
        self.pspools = {"a": [0, 1], "b": [2, 3], "c": [4, 5], "d": [6, 7], "sc": [0, 6, 1, 7]}
        self.s_in = None
        self.s_out = None
        self.s_w = [None]
        self.outkeys = []
        self.wslot_rr = 0

        P.dma("sync", self.xT[:], self.I["xT"].rearrange("(c p) t -> p c t", p=128), writes=["xT"], slot=self.s_in)
        P.dma("sync", self.cT[:], self.I["cT"].rearrange("(c p) t -> p c t", p=128), writes=["cT"], slot=self.s_in)
        P.dma("sync", self.cst_p[:], self.I["cst"][:, 768:NCST], writes=["cst"], slot=self.s_in)
        cst0 = self.A.alloc([768], F32)
        P.dma("sync", cst0, self.I["cst"][:, 0:768], writes=["cst0"], slot=self.s_in)
        self.cp("vector", self.cstR[:], cst0, ["cst0"], ["cstR"])
        P.barrier()
        self.A.reset(0)

        class _CstView:
            def __getitem__(_s, key):
                ps_, cs_ = key
                assert cs_.start >= 768, cs_
                return self.cst_p[ps_, cs_.start - 768:cs_.stop - 768]
        self.cst = _CstView()
        self.R_perm = self.cstR[:, C_PERM:C_PERM + 128]
        self.R_bd32 = self.cstR[:, C_BD32:C_BD32 + 128]
        self.R_bd64 = self.cstR[:, C_BD64:C_BD64 + 128]
        self.R_o1024 = self.cstR[:, C_O1024:C_O1024 + 128]
        self.R_o256 = self.cstR[:, C_O256:C_O256 + 128]
        self.R_ident = self.cstR[:, C_IDENT:C_IDENT + 128]

        for li, l in enumerate(self.layers):
            self.layer(l, last=(l == 1))
        for c in range(8):
            P.dma("sync", self.out[c * 128:(c + 1) * 128, :], self.xT[:, c, :], reads=["xT"], writes=[f"out{c}"], slot=self.s_out)
            self.outkeys.append(f"out{c}")
        P.wait_all("sync", self.outkeys)
        P.emit(st)
        return nc

    def wslot(self):
        s = self.s_w[self.wslot_rr % len(self.s_w)]
        self.wslot_rr += 1
        return s

    def modulation(self, l):
        P, A = self.P, self.A
        m0 = A.mark()
        cv = A.alloc([8, 2], F32)
        cvb = A.alloc([8, 2], BF16)
        sg = A.alloc([8, 2], F32)
        P.dma("sync", cv, self.I["cvec"], writes=["cv"], slot=self.s_in)
        P.dma("sync", self.pvec[:], self.I[f"pvec{l}"], writes=["pvec"], slot=self.s_in)
        self.act(sg, cv, AF.Sigmoid, ["cv"], ["sg"])
        self.tt("vector", cvb, cv, sg, ALU.mult, ["cv", "sg"], ["cvb"])
        wm = [A.alloc([8, 1024], BF16) for _ in range(2)]
        pst, pk = self.ps("d")
        wv = self.I[f"wmod{l}"].rearrange("(c p) m -> p c m", p=128)
        for j in range(6):
            P.dma("gpsimd", wm[j % 2], wv[:, :, j * 1024:(j + 1) * 1024], writes=[("wm", j % 2)], slot=self.wslot())
            for jj in range(8):
                col = j * 8 + jj
                for c in range(8):
                    self.mm(pst[:, 2 * col:2 * col + 2], wm[j % 2][:, c, jj * 128:(jj + 1) * 128], cvb[:, c, :],
                            c == 0, c == 7, [("wm", j % 2), "cvb"], [pk])
        self.tt("vector", self.modT[:], pst[:, 0:96].rearrange("p (j n) -> p j n", n=2),
                self.pvec[:, PV_BMOD:PV_BMOD + 48].unsqueeze(2).broadcast_to([128, 48, 2]), ALU.add,
                [pk, "pvec"], ["modT"])
        md = lambda i: self.modT[:, i * 8:(i + 1) * 8, :]
        for (dst, sc_i, g_off) in ((0, 1, PV_G1), (3, 4, PV_G2)):
            self.stt("vector", self.mA[:, dst], md(sc_i), 1.0,
                     self.pvec[:, g_off:g_off + 8].unsqueeze(2).broadcast_to([128, 8, 2]), ALU.add, ALU.mult,
                     ["modT", "pvec"], ["mA"])
        for (dst, src) in ((1, 0), (2, 2), (4, 3), (5, 5)):
            self.cp("vector", self.mA[:, dst], md(src), ["modT", "mA"], ["mA"])
        A.reset(m0)

    def norm_mod(self, seq, which, out_bf, okey, out_f32r=None, fkey=None, cb=None, local=False, bs=512):
        P, A = self.P, self.A
        ia, ish = (0, 1) if which == 1 else (3, 4)
        m0 = A.mark()
        mr0 = self.AR.mark()
        nsq = 2 if self.AR.n - self.AR.off >= 1024 else 1
        sqs = [self.AR.alloc([512], F32R) for _ in range(nsq)]
        rs = A.alloc([512], F32)
        tmps = [A.alloc([512], F32) for _ in range(2)]
        rk = seq.name + "T"
        nb_ = min(bs, seq.Tn)
        for (t0, n) in [(t, nb_) for t in range(0, seq.Tn, nb_)]:
            pst, pk = self.ps("c")
            for c in range(8):
                sq = sqs[c % nsq]
                sk = ("nm_sq", c % nsq)
                self.act(sq[:, 0:n], seq.res[:, c, t0:t0 + n], AF.Square, [rk], [sk])
                self.mm(pst[:, 0:n], self.R_o1024, sq[:, 0:n], c == 0, c == 7, [sk, "cstR"], [pk])
            self.act(rs[:, 0:n], pst[:, 0:n], AF.Ln, [pk], ["nm_rs"], bias=EPS)
            self.act(rs[:, 0:n], rs[:, 0:n], AF.Exp, ["nm_rs"], ["nm_rs"], scale=-0.5)
            for c in range(8):
                tmp = tmps[c % 2]
                tk = ("nm_tmp", c % 2)
                self.stt("vector", tmp[:, 0:n], seq.res[:, c, t0:t0 + n], self.mA[:, ia, c, seq.mcol:seq.mcol + 1],
                         rs[:, 0:n], ALU.mult, ALU.mult, [rk, "mA", "nm_rs"], [tk])
                if out_f32r is not None:
                    self.ts("vector", out_f32r[:, c, 0:n], tmp[:, 0:n], self.mA[:, ish, c, seq.mcol:seq.mcol + 1], None,
                            ALU.add, None, [tk, "mA"], [(fkey, c)])
                    ob = out_bf[:, c, 0:n] if local else out_bf[:, c, t0:t0 + n]
                    self.cp("scalar", ob, out_f32r[:, c, 0:n].bitcast(F32), [(fkey, c)], [(okey, c)])
                else:
                    self.ts("vector", out_bf[:, c, t0:t0 + n], tmp[:, 0:n], self.mA[:, ish, c, seq.mcol:seq.mcol + 1], None,
                            ALU.add, None, [tk, "mA"], [okey])
            if cb is not None:
                cb(t0, n)
        A.reset(m0)
        self.AR.reset(mr0)

    def load_w(self, dst, src_view, key):
        self.P.dma("gpsimd", dst, src_view, writes=[key], slot=self.wslot())

    def proj_fm(self, pst, pk, seq, wt, wkey, col0, M, t0, n, hkey):
        for c in range(8):
            self.mm(pst[0:M, 0:n], wt[:, c, col0:col0 + M], seq.h[:, c, t0:t0 + n], c == 0, c == 7, [wkey, hkey], [pk])

    def proj_tm(self, pst, pk, seq, wt, wkey, col0, ncols, tc, hkey):
        for c in range(8):
            self.mm(pst[:, 0:ncols], seq.h[:, c, tc * 128:(tc + 1) * 128], wt[:, c, col0:col0 + ncols], c == 0, c == 7,
                    [wkey, hkey], [pk])

    def wout_partial(self, l, seq, mixT, mkey, grp):
        A = self.A
        m0 = A.mark()
        wo = A.alloc([2, 1024], BF16)
        self.load_w(wo, self.I[f"w_out{l}"][grp * 256:(grp + 1) * 256, :].rearrange("(k p) d -> p k d", p=128), "wo")
        rk = seq.name + "T"
        for (t0, n) in seq.blocks:
            for dc in range(8):
                pst, pk = self.ps("d")
                for k in range(2):
                    self.mm(pst[:, 0:n], wo[:, k, dc * 128:(dc + 1) * 128], mixT[:, k, t0:t0 + n], k == 0, k == 1,
                            ["wo", mkey], [pk])
                self.stt("vector", seq.res[:, dc, t0:t0 + n], pst[:, 0:n], self.mA[:, 2, dc, seq.mcol:seq.mcol + 1],
                         seq.res[:, dc, t0:t0 + n], ALU.mult, ALU.add, [pk, "mA", rk], [rk])
        A.reset(m0)

    def qk_norm(self, pst, pk, n, gcol, rope_t0, out_bf, okey, Ws):
        wi_ = self._qkrr = getattr(self, "_qkrr", 0) + 1
        wi_ %= len(Ws)
        sq, rs, qn, t1, rc, rsn = Ws[wi_]
        self.act(sq[:, 0:n], pst[:, 0:n], AF.Square, [pk], [("qk_sq", wi_)])
        ps2, pk2 = self.ps("c")
        self.mm(ps2[:, 0:n], self.R_bd32, sq[:, 0:n], True, True, [("qk_sq", wi_), "cstR"], [pk2])
        self.act(rs[:, 0:n], ps2[:, 0:n], AF.Ln, [pk2], [("qk_rs", wi_)], bias=EPS)
        self.act(rs[:, 0:n], rs[:, 0:n], AF.Exp, [("qk_rs", wi_)], [("qk_rs", wi_)], scale=-0.5)
        if rope_t0 is None:
            self.stt("vector", out_bf, pst[:, 0:n], self.pvec[:, gcol:gcol + 1], rs[:, 0:n], ALU.mult, ALU.mult,
                     [pk, "pvec", ("qk_rs", wi_)], [okey])
            return
        self.stt("vector", qn[:, 0:n], pst[:, 0:n], self.pvec[:, gcol:gcol + 1], rs[:, 0:n], ALU.mult, ALU.mult,
                 [pk, "pvec", ("qk_rs", wi_)], [("qk_qn", wi_)])
        self.P.dma("sync", rc[:, 0:n], self.I["rope_c"][:, rope_t0:rope_t0 + n], writes=[("rope_c", wi_)], slot=self.s_in)
        self.P.dma("sync", rsn[:, 0:n], self.I["rope_s"][:, rope_t0:rope_t0 + n], writes=[("rope_s", wi_)], slot=self.s_in)
        ps3, pk3 = self.ps("c")
        self.mm(ps3[:, 0:n], self.R_perm, qn[:, 0:n], True, True, [("qk_qn", wi_), "cstR"], [pk3])
        self.tt("gpsimd", t1[:, 0:n], qn[:, 0:n].bitcast(F32), rc[:, 0:n], ALU.mult, [("qk_qn", wi_), ("rope_c", wi_)], [("qk_t1", wi_)])
        self.tt("vector", rs[:, 0:n], ps3[:, 0:n], rsn[:, 0:n], ALU.mult, [pk3, ("rope_s", wi_), ("qk_rs", wi_)], [("qk_rs", wi_)])
        self.tt("vector", out_bf, t1[:, 0:n], rs[:, 0:n], ALU.add, [("qk_t1", wi_), ("qk_rs", wi_)], [okey])

    def attention(self, l, seqs, last):
        P, A = self.P, self.A
        X, C = self.X, self.C
        m0 = A.mark()
        lam_init = 0.8 - 0.6 * math.exp(-0.3 * l)
        mr0 = self.AR.mark()
        kT = A.alloc([2, T + TC], BF16)
        qTs = {"x": A.alloc([2, T], BF16), "c": A.alloc([2, TC], BF16)}
        vaug = A.alloc([18, 4, 128], BF16)
        lamv = A.alloc([128], F32)
        lt = A.alloc([64], F32)
        mq = A.mark()
        wq = A.alloc([8, 768], BF16)
        self.load_w(wq, self.I[f"w_in{l}"][:, 0:768].rearrange("(c p) m -> p c m", p=128), "wq")
        W = []
        for _ in range(2):
            Wf = [A.alloc([512], F32) for _ in range(4)]
            W.append([self.AR.alloc([512], F32R), Wf[0], self.AR.alloc([512], F32R), Wf[1], Wf[2], Wf[3]])
        self.memset("gpsimd", vaug, 1.0, ["vaug"])
        P.dma("sync", lamv, self.I[f"rowv{l}"][512:640].partition_broadcast(128), writes=["lamv"], slot=self.s_in)
        self.tt("vector", lt[:, 0:32], lamv[:, 0:32], lamv[:, 32:64], ALU.mult, ["lamv"], ["lt"])
        self.tt("vector", lt[:, 32:64], lamv[:, 64:96], lamv[:, 96:128], ALU.mult, ["lamv", "lt"], ["lt"])
        sm = self.small
        P.op("vector", lambda e: e.reduce_sum(out=sm[:, 0:2], in_=lt.rearrange("p (a b) -> p a b", a=2),
                                              axis=mybir.AxisListType.X), ["lt"], ["sm_lam"])
        self.act(sm[:, 2:4], sm[:, 0:2], AF.Exp, ["sm_lam"], ["sm_lam2"])
        self.stt("vector", sm[:, 4:5], sm[:, 3:4], -lam_init, sm[:, 2:3], ALU.add, ALU.subtract, ["sm_lam2"], ["nlam"])
        self.ts("vector", sm[:, 5:6], self.pvec[:, PV_GAO:PV_GAO + 1], 1.0 - lam_init, None, ALU.mult, None, ["pvec"], ["gao2"])
        nlam = sm[:, 4:5]
        gao2 = sm[:, 5:6]
        for seq in (C, X):
            hk = seq.name + "h"
            for (t0, n) in seq.blocks:
                for kc in range(2):
                    pst, pk = self.ps("a")
                    self.proj_fm(pst, pk, seq, wq, "wq", 256 + kc * 128, 128, t0, n, hk)
                    for _d in range(2):
                        self.mm(self.PS[7][:, 0:n], wq[:, 0, 0:128], seq.h[:, 0, t0:t0 + n], True, True, ["wq", hk], [("ps", 7)])
                    self.qk_norm(pst, pk, n, PV_GK, t0 if seq.rope else None,
                                 kT[:, kc, seq.off + t0:seq.off + t0 + n], "kT", W)
            for tc in range(seq.nch):
                pst, pk = self.ps("b")
                self.proj_tm(pst, pk, seq, wq, "wq", 512, 256, tc, hk)
                kcg = seq.off // 128 + tc
                pv = pst[:, 0:256].rearrange("p (a b c) -> p a b c", a=2, b=2)
                va = vaug[:, kcg].rearrange("p (a b) c -> p a b c", a=2)
                self.cp("vector", va[:, :, 0, 0:64], pv[:, :, 0, :], [pk], ["vaug"])
                self.cp("scalar", va[:, :, 1, 64:128], pv[:, :, 1, :], [pk], ["vaug"])
        for seq in seqs:
            hk = seq.name + "h"
            for (t0, n) in seq.blocks:
                for qc in range(2):
                    pst, pk = self.ps("a")
                    self.proj_fm(pst, pk, seq, wq, "wq", qc * 128, 128, t0, n, hk)
                    for _d in range(2):
                        self.mm(self.PS[7][:, 0:n], wq[:, 0, 0:128], seq.h[:, 0, t0:t0 + n], True, True, ["wq", hk], [("ps", 7)])
                    self.qk_norm(pst, pk, n, PV_GQ, t0 if seq.rope else None, qTs[seq.name][:, qc, t0:t0 + n], "qT" + seq.name, W)
        P.barrier()
        A.reset(mq)
        O1r = W[0][0]
        O1 = A.alloc([512], F32)
        R = [A.alloc([512], F32) for _ in range(2)]
        Tm = [A.alloc([512], F32) for _ in range(2)]
        ocat = A.alloc([512], F32)
        pTT = [A.alloc([2, 512], BF16) for _ in range(2)]
        mixT = self.mixT
        sc = 32 ** -0.5
        for seq in seqs:
            hk = seq.name + "h"
            mk = "mix" + seq.name
            kchunks = list(range(0, 2)) + (list(range(2, 18)) if seq is X else [])
            qT = qTs[seq.name]
            qk_ = "qT" + seq.name
            for (t0, n) in seq.blocks:
                for qc in range(2):
                    for hh in range(2):
                        h = qc * 2 + hh
                        nb, db = hh * 64, 64 - hh * 64
                        obanks = [self.ps("b"), self.ps("b")]
                        pend = None
                        for i, kc in enumerate(kchunks):
                            b0 = 0 if i % 2 == 0 else 6
                            for s_ in range(2):
                                base = hh * 64 + s_ * 32
                                self.mm(self.PS[b0 + s_][:, 0:n], kT[base:base + 32, qc, kc * 128:(kc + 1) * 128],
                                        qT[base:base + 32, qc, t0:t0 + n], True, True, ["kT", qk_], [("ps", b0 + s_)], tp=(base, 0))
                            if pend is not None:
                                for s_ in range(2):
                                    po, pok = obanks[s_]
                                    pe_ = pend[s_]
                                    self.mm(po[:, 0:n], pe_[0], pe_[1], pe_[2], False, ["vaug", pe_[3]], [pok])
                            for _d in range(WARM_DUMMY):
                                self.mm(self.PS[4][:, 0:n], kT[:, qc, kc * 128:(kc + 1) * 128], qT[:, qc, t0:t0 + n], True, True,
                                        ["kT", qk_], [("ps", 4)])
                            pt2 = pTT[i % 2]
                            ptk = ("pTT", i % 2)
                            src = self.psall[:, b0 * 512:(b0 + 2) * 512].rearrange("p (s n) -> p s n", s=2)[:, :, 0:n]
                            self.act(pt2[:, :, 0:n], src, AF.Exp, [("ps", b0), ("ps", b0 + 1)], [ptk], scale=sc)
                            pend = [(vaug[:, kc, h, :], pt2[:, s_, 0:n], i == 0, ptk) for s_ in range(2)]
                        for s_ in range(2):
                            po, pok = obanks[s_]
                            pe_ = pend[s_]
                            self.mm(po[:, 0:n], pe_[0], pe_[1], pe_[2], True, ["vaug", pe_[3]], [pok])
                        for s_ in range(2):
                            po, pok = obanks[s_]
                            self.act(Tm[s_][db:db + 64, 0:n], po[db:db + 64, 0:n], AF.Ln, [pok], [("Tm", s_)])
                            self.act(R[s_][nb:nb + 64, 0:n], Tm[s_][db:db + 64, 0:n], AF.Exp, [("Tm", s_)], [("R", s_)], scale=-1.0)
                            self.tt("vector", Tm[s_][nb:nb + 64, 0:n], po[nb:nb + 64, 0:n], R[s_][nb:nb + 64, 0:n], ALU.mult,
                                    [pok, ("R", s_), ("Tm", s_)], [("Tm", s_)])
                        self.stt("vector", ocat[nb:nb + 64, 0:n], Tm[1][nb:nb + 64, 0:n], nlam[nb:nb + 64, :],
                                 Tm[0][nb:nb + 64, 0:n], ALU.mult, ALU.add, [("Tm", 0), ("Tm", 1), "nlam"], ["ocat"])
                    self.act(O1r[:, 0:n], ocat[:, 0:n], AF.Square, ["ocat"], ["O1r"])
                    ps2, pk2 = self.ps("c")
                    self.mm(ps2[:, 0:n], self.R_bd64, O1r[:, 0:n], True, True, ["O1r", "cstR"], [pk2])
                    self.act(O1[:, 0:n], ps2[:, 0:n], AF.Ln, [pk2], ["O1"], bias=EPS)
                    self.act(O1[:, 0:n], O1[:, 0:n], AF.Exp, ["O1"], ["O1"], scale=-0.5)
                    self.stt("vector", mixT[:, qc, t0:t0 + n], ocat[:, 0:n], gao2, O1[:, 0:n], ALU.mult, ALU.mult,
                             ["ocat", "gao2", "O1"], [mk])
            self.dump(f"attn_{seq.name}{l}", mixT[:, :, 0:seq.Tn], [mk], [128, 2, seq.Tn])
            self.wout_partial(l, seq, mixT, mk, 0)
        A.reset(m0)
        self.AR.reset(mr0)

    def pool_mixer(self, l, seq):
        P, A = self.P, self.A
        m0 = A.mark()
        Tn = seq.Tn
        Wd = Tn + 16
        hk = seq.name + "h"
        mk = "mix" + seq.name
        wp = A.alloc([8, 256], BF16)
        self.load_w(wp, self.I[f"w_in{l}"][:, 768:1024].rearrange("(c p) m -> p c m", p=128), "wp")
        wbd = A.alloc([2, 128], BF16)
        self.memset("gpsimd", wbd, 0.0, ["wbd"])
        for g in range(4):
            c, hf = g // 2, g % 2
            P.dma("gpsimd", wbd[hf * 64:(hf + 1) * 64, c, hf * 64:(hf + 1) * 64], self.I[f"wpool{l}"][g],
                  reads=["wbd"], writes=["wbd"], slot=self.wslot())
        zps = [A.alloc([Wd], F32) for _ in range(2)]
        Ab1 = A.alloc([Wd], F32)
        Abs_ = [Ab1, Ab1]
        Bb = A.alloc([Wd], F32)
        Cb = A.alloc([Wd], F32)
        Sb1 = A.alloc([Tn], F32)
        Sbs = [Sb1, Sb1]
        dTs = [A.alloc([Tn], BF16) for _ in range(2)]
        mixT = self.mixT
        G = "gpsimd"
        for c in range(2):
            zp, Ab, Sb, dT = zps[c], Abs_[c], Sbs[c], dTs[c]
            self.memset(G, zp[:, 0:8], 0.0, [("zp", c)])
            self.memset(G, zp[:, 8 + Tn:Wd], 0.0, [("zp", c)])
            for (t0, n) in seq.blocks:
                pst, pk = self.ps("a")
                self.proj_fm(pst, pk, seq, wp, "wp", c * 128, 128, t0, n, hk)
                self.cp("scalar", zp[:, 8 + t0:8 + t0 + n], pst[:, 0:n], [pk], [("zp", c)])
            lo, hi = slice(0, 64), slice(64, 128)
            if c == 0:
                self.tt(G, Sb[lo, :], zp[lo, 7:7 + Tn], zp[lo, 8:8 + Tn], ALU.add, [("zp", c)], ["Sb"])
                self.tt(G, Ab[hi, 0:Wd - 1], zp[hi, 0:Wd - 1], zp[hi, 1:Wd], ALU.add, [("zp", c)], ["Ab"])
                self.tt(G, Sb[hi, :], Ab[hi, 6:6 + Tn], Ab[hi, 8:8 + Tn], ALU.add, ["Ab", "Sb"], ["Sb"])
            else:
                self.tt(G, Ab[:, 0:Wd - 1], zp[:, 0:Wd - 1], zp[:, 1:Wd], ALU.add, [("zp", c)], ["Ab"])
                self.tt(G, Bb[:, 0:Wd - 3], Ab[:, 0:Wd - 3], Ab[:, 2:Wd - 1], ALU.add, ["Ab"], ["Bb"])
                self.tt(G, Sb[lo, :], Bb[lo, 4:4 + Tn], Bb[lo, 8:8 + Tn], ALU.add, ["Bb"], ["Sb"])
                self.tt(G, Cb[hi, 0:Wd - 7], Bb[hi, 0:Wd - 7], Bb[hi, 4:Wd - 3], ALU.add, ["Bb"], ["Cb"])
                self.tt(G, Sb[hi, :], Cb[hi, 0:Tn], Cb[hi, 8:8 + Tn], ALU.add, ["Cb", "Sb"], ["Sb"])
            self.stt("vector", dT[:, :], Sb[:, :], self.cst[:, C_PRW + c:C_PRW + c + 1], zp[:, 8:8 + Tn], ALU.mult, ALU.subtract,
                     ["Sb", "cst", ("zp", c)], [("dT", c)])
            for side, (a0, f0) in enumerate(((0, 0), (Tn - 8, 8))):
                fx = self.cst[:, C_PFIX + c * 16 + f0:C_PFIX + c * 16 + f0 + 8]
                self.tt("vector", Sb[:, a0:a0 + 8], Sb[:, a0:a0 + 8], fx, ALU.mult, ["Sb", "cst", ("dT", c)], ["Sb"])
                self.tt("vector", dT[:, a0:a0 + 8], Sb[:, a0:a0 + 8], zp[:, 8 + a0:8 + a0 + 8], ALU.subtract, ["Sb", ("zp", c)], [("dT", c)])
            for (t0, n) in seq.blocks:
                pst, pk = self.ps("b")
                self.mm(pst[:, 0:n], wbd[:, c, :], dT[:, t0:t0 + n], True, True, ["wbd", ("dT", c)], [pk])
                self.ts("vector", mixT[:, c, t0:t0 + n], pst[:, 0:n], self.pvec[:, PV_BPOOL + c:PV_BPOOL + c + 1],
                        self.pvec[:, PV_PSC + c:PV_PSC + c + 1], ALU.add, ALU.mult, [pk, "pvec"], [mk])
        self.dump(f"pool_{seq.name}{l}", mixT[:, :, 0:Tn], [mk], [128, 2, Tn])
        self.wout_partial(l, seq, mixT, mk, 1)
        A.reset(m0)

    def conv_mixer(self, l, seq):
        P, A = self.P, self.A
        m0 = A.mark()
        Tn = seq.Tn
        hk = seq.name + "h"
        mk = "mix" + seq.name
        wc = A.alloc([8, 512], BF16)
        self.load_w(wc, self.I[f"w_in{l}"][:, 1024:1536].rearrange("(c p) m -> p c m", p=128), "wc")
        wpw = A.alloc([2, 256], BF16)
        self.load_w(wpw, self.I[f"wpw2{l}"].rearrange("(c p) m -> p c m", p=128), "wpw")
        yp = A.alloc([2, Tn + 30], BF16)
        dg = A.alloc([2, 31, 128], BF16)
        sgt = A.alloc([512], F32)
        mr0 = self.AR.mark()
        sq = self.AR.alloc([512], F32R)
        accR = self.AR.alloc([2, 512], F32R)
        m2 = A.alloc([512], F32)
        var = A.alloc([512], F32)
        tt_ = A.alloc([512], F32)
        actT = A.alloc([2, 512], BF16)
        mixT = self.mixT
        identF = self.R_ident.bitcast(F32)
        for c in range(2):
            for k in range(31):
                self.ts("vector", dg[:, c, k, :], identF, self.pvec[:, PV_CONVW + c * 31 + k:PV_CONVW + c * 31 + k + 1], None,
                        ALU.mult, None, ["cstR", "pvec"], [("dg", c, k)])
        for c in range(2):
            self.memset("gpsimd", yp[:, c, 0:15], 0.0, [("yp", c)])
            self.memset("gpsimd", yp[:, c, 15 + Tn:30 + Tn], 0.0, [("yp", c)])
            for (t0, n) in seq.blocks:
                pv, pvk = self.ps("a")
                self.proj_fm(pv, pvk, seq, wc, "wc", c * 128, 128, t0, n, hk)
                pg, pgk = self.ps("b")
                self.proj_fm(pg, pgk, seq, wc, "wc", 256 + c * 128, 128, t0, n, hk)
                self.act(sgt[:, 0:n], pg[:, 0:n], AF.Sigmoid, [pgk], ["sgt"])
                self.tt("vector", yp[:, c, 15 + t0:15 + t0 + n], pv[:, 0:n], sgt[:, 0:n], ALU.mult, [pvk, "sgt"], [("yp", c)])
        for (t0, n) in seq.blocks:
            pm, pmk = self.ps("c")
            for c in range(2):
                pc_, pck = self.ps("d")
                for k in range(31):
                    self.mm(pc_[:, 0:n], dg[:, c, k, :], yp[:, c, t0 + k:t0 + k + n], k == 0, k == 30, [("dg", c, k), ("yp", c)], [pck])
                self.ts("vector", accR[:, c, 0:n], pc_[:, 0:n], self.pvec[:, PV_CONVB + c:PV_CONVB + c + 1], None, ALU.add, None,
                        [pck, "pvec"], [("accR", c)])
                self.mm(pm[:, 0:n], self.R_o256, accR[:, c, 0:n], c == 0, c == 1, [("accR", c), "cstR"], [pmk])
            pq, pqk = self.ps("c")
            for c in range(2):
                self.act(sq[:, 0:n], accR[:, c, 0:n].bitcast(F32), AF.Square, [("accR", c)], ["cv_sq"])
                self.mm(pq[:, 0:n], self.R_o256, sq[:, 0:n], c == 0, c == 1, ["cv_sq", "cstR"], [pqk])
            self.act(m2[:, 0:n], pm[:, 0:n], AF.Square, [pmk], ["cv_m2"])
            self.tt("vector", var[:, 0:n], pq[:, 0:n], m2[:, 0:n], ALU.subtract, [pqk, "cv_m2"], ["cv_var"])
            self.act(var[:, 0:n], var[:, 0:n], AF.Ln, ["cv_var"], ["cv_var"], bias=EPS)
            self.act(var[:, 0:n], var[:, 0:n], AF.Exp, ["cv_var"], ["cv_var"], scale=-0.5)
            for c in range(2):
                self.tt("vector", tt_[:, 0:n], accR[:, c, 0:n].bitcast(F32), pm[:, 0:n], ALU.subtract, [("accR", c), pmk], ["cv_t"])
                self.tt("vector", tt_[:, 0:n], tt_[:, 0:n], var[:, 0:n], ALU.mult, ["cv_t", "cv_var"], ["cv_t"])
                self.act(actT[:, c, 0:n], tt_[:, 0:n], AF.Silu, ["cv_t", "pvec"], [("cv_act", c)],
                         bias=self.pvec[:, PV_CLNB + c:PV_CLNB + c + 1], scale=self.pvec[:, PV_CLNG + c:PV_CLNG + c + 1])
            for co in range(2):
                po, pok = self.ps("b")
                for ci in range(2):
                    self.mm(po[:, 0:n], wpw[:, ci, co * 128:(co + 1) * 128], actT[:, ci, 0:n], ci == 0, ci == 1,
                            ["wpw", ("cv_act", ci)], [pok])
                self.cp("scalar", mixT[:, co, t0:t0 + n], po[:, 0:n], [pok], [mk])
        self.dump(f"conv_{seq.name}{l}", mixT[:, :, 0:Tn], [mk], [128, 2, Tn])
        self.wout_partial(l, seq, mixT, mk, 2)
        A.reset(m0)
        self.AR.reset(mr0)

    def sgu_mixer(self, l, seq):
        P, A = self.P, self.A
        m0 = A.mark()
        Tn = seq.Tn
        hk = seq.name + "h"
        mk = "mix" + seq.name
        ws = A.alloc([8, 512], BF16)
        self.load_w(ws, self.I[f"w_in{l}"][:, 1536:2048].rearrange("(c p) m -> p c m", p=128), "ws")
        wsp = A.alloc([4, 128], BF16)
        self.load_w(wsp, self.I[f"wspT{l}"], "wsp")
        bsp = A.alloc([2, 128], F32)
        P.dma("sync", bsp, self.I[f"bsp{l}"], writes=["bsp"], slot=self.s_in)
        lnr = A.alloc([512], F32)
        P.dma("sync", lnr, self.I[f"rowv{l}"][0:512].partition_broadcast(128), writes=["lnr"], slot=self.s_in)
        uT = A.alloc([2, Tn], F32)
        vgs = [A.alloc([256], F32) for _ in range(2)]
        vts = [A.alloc([256], F32) for _ in range(2)]
        st6s = [A.alloc([8], F32) for _ in range(2)]
        VA = [A.alloc([256], BF16) for _ in range(2)]
        VB = [A.alloc([256], BF16) for _ in range(2)]
        tmp = A.alloc([512], F32)
        mixT = self.mixT
        for i in range(2):
            self.memset("gpsimd", VA[i], 0.0, [("VA", i)])
            self.memset("gpsimd", VB[i], 0.0, [("VB", i)])
        for c in range(2):
            for (t0, n) in seq.blocks:
                pst, pk = self.ps("a")
                self.proj_fm(pst, pk, seq, ws, "ws", c * 128, 128, t0, n, hk)
                self.act(uT[:, c, t0:t0 + n], pst[:, 0:n], AF.Gelu_apprx_tanh, [pk], ["uT"])
        nblk = len(seq.blocks)
        per = seq.blocks[0][1] // 128
        for bi, (t0, n) in enumerate(seq.blocks):
            pcc = [self.ps("d"), self.ps("d")]
            for j in range(per):
                tc = t0 // 128 + j
                i = tc % 2
                pst, pk = self.ps("b")
                self.proj_tm(pst, pk, seq, ws, "ws", 256, 256, tc, hk)
                vg, vt, st6 = vgs[i], vts[i], st6s[i]
                kvg, kvt, kst = ("vg", i), ("vt", i), ("st", i)
                self.act(vg, pst[:, 0:256], AF.Gelu_apprx_tanh, [pk], [kvg])
                P.op("vector", lambda e, st6=st6, vg=vg: e.bn_stats(out=st6[:, 0:6], in_=vg), [kvg], [kst])
                P.op("vector", lambda e, st6=st6: e.bn_aggr(out=st6[:, 6:8], in_=st6[:, 0:6]), [kst], [kst])
                self.act(st6[:, 7:8], st6[:, 7:8], AF.Ln, [kst], [kst], bias=EPS)
                self.act(st6[:, 7:8], st6[:, 7:8], AF.Exp, [kst], [kst], scale=-0.5)
                self.ts("vector", vt, vg, st6[:, 6:7], st6[:, 7:8], ALU.subtract, ALU.mult, [kvg, kst], [kvt])
                self.tt("vector", vt, vt, lnr[:, 0:256], ALU.mult, [kvt, "lnr"], [kvt])
                v3 = vt.rearrange("p (a b) -> p a b", a=2)
                b3 = lnr[:, 256:512].rearrange("p (a b) -> p a b", a=2)
                self.tt("vector", VA[i].rearrange("p (a b) -> p a b", a=2)[:, :, 0:64], v3[:, :, 0:64], b3[:, :, 0:64], ALU.add,
                        [kvt, "lnr"], [("VA", i)])
                self.tt("vector", VB[i].rearrange("p (a b) -> p a b", a=2)[:, :, 64:128], v3[:, :, 64:128], b3[:, :, 64:128], ALU.add,
                        [kvt, "lnr"], [("VB", i)])
                for cc in range(2):
                    po, pok = pcc[cc]
                    self.mm(po[:, j * 128:(j + 1) * 128], VA[i][:, cc * 128:(cc + 1) * 128], wsp[:, 2 * cc, :], True, False,
                            [("VA", i), "wsp"], [pok])
                    self.mm(po[:, j * 128:(j + 1) * 128], VB[i][:, cc * 128:(cc + 1) * 128], wsp[:, 2 * cc + 1, :], False, True,
                            [("VB", i), "wsp"], [pok])
            for cc in range(2):
                po, pok = pcc[cc]
                self.tt("vector", tmp[:, 0:n].rearrange("p (a b) -> p a b", b=128), po[:, 0:n].rearrange("p (a b) -> p a b", b=128),
                        bsp[:, cc, :].unsqueeze(1).broadcast_to([128, per, 128]), ALU.add, [pok, "bsp"], ["sg_tmp"])
                self.tt("vector", mixT[:, cc, t0:t0 + n], tmp[:, 0:n], uT[:, cc, t0:t0 + n], ALU.mult, ["sg_tmp", "uT"], [mk])
        self.dump(f"sgu_{seq.name}{l}", mixT[:, :, 0:Tn], [mk], [128, 2, Tn])
        self.wout_partial(l, seq, mixT, mk, 3)
        A.reset(m0)

    def moe(self, l, li, seqs):
        P, A = self.P, self.A
        m0 = A.mark()
        GE = 2
        NJ = 256 + (32 if len(seqs) > 1 else 0)
        njc = 3 if len(seqs) > 1 else 2
        mr0 = self.AR.mark()
        wrR = self.AR.alloc([8 * 128], F32R)
        h2f = self.AR.alloc([8, 256], F32R)
        affEb = self.AR.alloc([256], F32R)
        h2tok = {}
        postok = {}
        for seq in seqs:
            h2tok[seq.name] = A.alloc([seq.nch, 1024], BF16)
            postok[seq.name] = A.alloc([seq.nch, NE], F32)
        m1 = A.mark()
        identB = A.alloc([128], BF16)
        self.cp("vector", identB, self.R_ident.bitcast(F32), ["cstR"], ["identB"])
        wr0 = A.alloc([8, NE], F32)
        P.dma("sync", wr0, self.I[f"wr{l}"], writes=["wr0"], slot=self.s_in)
        self.ts("vector", wrR, self.xT[:, 0, 0:1024], 0.0, None, ALU.mult, None, ["xT"], ["wrR"])
        for c in range(8):
            self.cp("vector", wrR[:, c * 128:c * 128 + NE], wr0[:, c, :], ["wr0", "wrR"], ["wrR"])
        m1b = A.mark()
        for seq in seqs:
            Tn = seq.Tn
            cap = 2 * Tn // NE
            sn = seq.name
            P.barrier()
            A.reset(m1b)
            hb = A.alloc([8, 256], BF16)
            affE = None
            aff = A.alloc([Tn], F32)
            work = A.alloc([Tn], F32)
            ones = A.alloc([Tn], F32)
            cs = A.alloc([Tn], F32)
            mask = A.alloc([Tn], F32)
            posb = A.alloc([Tn], BF16)
            gateb = A.alloc([Tn], BF16)
            mx = A.alloc([8], F32)

            def cb(t0, n, seq=seq, hb=hb, sn=sn, aff=aff, work=work):
                pl, plk = self.ps("a")
                for c in range(8):
                    self.mm(pl[:, 0:n], wrR[:, c * 128:(c + 1) * 128], h2f[:, c, 0:n], c == 0, c == 7,
                            ["wrR", ("h2f", c)], [plk])
                self.act(affEb[0:NE, 0:n], pl[0:NE, 0:n], AF.Exp, [plk], ["affEb"])
                pss, psk = self.ps("c")
                self.mm(pss[:, 0:n], self.R_o1024[0:NE, :], affEb[0:NE, 0:n], True, True, ["affEb", "cstR"], [psk])
                self.act(work[0:NE, t0:t0 + n], pss[0:NE, 0:n], AF.Ln, [psk], ["work"])
                self.act(work[0:NE, t0:t0 + n], work[0:NE, t0:t0 + n], AF.Exp, ["work"], ["work"], scale=-1.0)
                self.stt("vector", aff[0:NE, t0:t0 + n], affEb[0:NE, 0:n].bitcast(F32), 1.0 / 1024, work[0:NE, t0:t0 + n], ALU.mult, ALU.mult,
                         ["affEb", "work"], ["aff"])
                for j in range(n // 128):
                    tc = t0 // 128 + j
                    pt, ptk = self.ps("b")
                    ptb = pt.bitcast(BF16)
                    for dc in range(8):
                        P.op("tensor", lambda e, dc=dc, j=j, ptb=ptb: e.transpose(out=ptb[:, dc * 128:(dc + 1) * 128],
                                                                                 in_=hb[:, dc, j * 128:(j + 1) * 128], identity=identB),
                             [("hb", dc), "identB"], [ptk], is_mm=True)
                    self.cp("scalar", h2tok[sn][:, tc, :], ptb[:, 0:1024], [ptk], ["h2tok" + sn])

            self.norm_mod(seq, 2, hb, "hb", out_f32r=h2f, fkey="h2f", cb=cb, local=True, bs=256)
            self.dump(f"aff_{sn}{l}", aff[0:NE, :], ["aff"], [NE, Tn])
            lo_, mid_, cnt_, tmp_ = mx[0:NE, 0:1], mx[0:NE, 1:2], mx[0:NE, 2:3], mx[0:NE, 3:4]
            self.memset("vector", lo_, 0.0, ["bs_lo"])
            for k in range(26):
                wk = 0.5 ** (k + 1)
                self.ts("vector", mid_, lo_, wk, None, ALU.add, None, ["bs_lo"], ["bs_mid"])
                P.op("vector", lambda e, work=work, aff=aff, mid_=mid_, cnt_=cnt_: e.tensor_scalar(
                    out=work[0:NE, :], in0=aff[0:NE, :], scalar1=mid_, scalar2=0.0, op0=ALU.is_ge, op1=ALU.add, accum_out=cnt_),
                    ["aff", "bs_mid"], ["work", "bs_cnt"])
                self.ts("vector", tmp_, cnt_, cap - 0.5, wk, ALU.is_ge, ALU.mult, ["bs_cnt"], ["bs_tmp"])
                self.tt("vector", lo_, lo_, tmp_, ALU.add, ["bs_lo", "bs_tmp"], ["bs_lo"])
            self.ts("vector", mask[0:NE, :], aff[0:NE, :], lo_, None, ALU.is_ge, None, ["aff", "bs_lo"], ["mask"])
            self.memset("gpsimd", ones[0:NE, :], 1.0, ["ones"])
            P.op("vector", lambda e, cs=cs, ones=ones, mask=mask: e.tensor_tensor_scan(out=cs[0:NE, :], data0=ones[0:NE, :], data1=mask[0:NE, :], initial=0.0,
                                                          op0=ALU.mult, op1=ALU.add), ["ones", "mask"], ["cs"])
            self.tt("vector", cs[0:NE, :], cs[0:NE, :], mask[0:NE, :], ALU.mult, ["cs", "mask"], ["cs"])
            self.ts("vector", cs[0:NE, :], cs[0:NE, :], -1.0, None, ALU.add, None, ["cs"], ["cs"])
            self.cp("vector", posb[0:NE, :], cs[0:NE, :], ["cs"], ["posb"])
            self.tt("vector", gateb[0:NE, :], aff[0:NE, :], mask[0:NE, :], ALU.mult, ["aff", "mask"], ["gateb"])
            P.dma("sync", self.scr_pos[li, :, seq.off:seq.off + Tn], posb[0:NE, :], reads=["posb"], writes=["scr_pos"], slot=self.s_in)
            P.dma("sync", self.scr_gate[li, :, seq.off:seq.off + Tn], gateb[0:NE, :], reads=["gateb"], writes=["scr_gate"], slot=self.s_in)
            pt, ptk = self.ps("c")
            for tc in range(seq.nch):
                P.op("tensor", lambda e, tc=tc, pt=pt, cs=cs: e.transpose(out=pt[:, tc * NE:(tc + 1) * NE], in_=cs[0:NE, tc * 128:(tc + 1) * 128],
                                                                   identity=self.R_ident.bitcast(F32)[0:NE, 0:NE]),
                     ["cs", "cstR"], [ptk], is_mm=True)
            self.cp("vector", postok[sn], pt[:, 0:seq.nch * NE].rearrange("p (a b) -> p a b", b=NE), [ptk], ["postok" + sn])
        P.barrier()
        A.reset(m1)
        X = seqs[0]
        Cq = seqs[1] if len(seqs) > 1 else None
        S = A.alloc([16, 256], FP8)
        Sc = A.alloc([2, 32], FP8)
        xsT = A.alloc([8, NJ], BF16)
        actT = A.alloc([16, NJ], BF16)
        sa = A.alloc([NJ], F32)
        NR = 3
        wgp = [A.alloc([8, 256], BF16) for _ in range(NR)]
        wup = [A.alloc([8, 256], BF16) for _ in range(NR)]
        wdp = [A.alloc([16, 256], BF16) for _ in range(2)]
        ygrp = A.alloc([GE * 3, 1024], BF16)
        _pb = [A.alloc([256], BF16) for _ in range(GE)]
        _gb = [A.alloc([256], BF16) for _ in range(GE)]
        posB = [_pb, _pb]
        gateB = [_gb, _gb]
        STs = [A.alloc([GE * 2, 256], BF16) for _ in range(2)]
        stmp = A.alloc([256], BF16)
        iota = self.cst[:, C_IOTA:C_IOTA + 256]
        jidx = lambda jc: self.cst[:, C_JIDX + jc:C_JIDX + jc + 1]
        def scatter_gen(e):
            e0 = e - (GE - 1)
            blocks = [(seq, t0, 256) for seq in seqs for t0 in range(0, seq.Tn, 256)]

            def prep(bi):
                seq, t0, n = blocks[bi]
                par = bi % 2
                isx = seq is X
                for g in range(GE):
                    P.dma("sync", posB[par][g][:, 0:n], self.scr_pos[li, e0 + g, seq.off + t0:seq.off + t0 + n].partition_broadcast(128),
                          reads=["scr_pos"], writes=[("posB", g)], slot=self.s_in)
                    P.dma("sync", gateB[par][g][:, 0:n], self.scr_gate[li, e0 + g, seq.off + t0:seq.off + t0 + n].partition_broadcast(128),
                          reads=["scr_gate"], writes=[("gateB", g)], slot=self.s_in)
                    for jc in (range(2) if isx else (2,)):
                        si = g * 2 + (jc if isx else 0)
                        self.ts("gpsimd", stmp[:, 0:n], posB[par][g][:, 0:n], jidx(jc), None, ALU.is_equal, None,
                                [("posB", g), "cst"], ["stmp"])
                        self.tt("gpsimd", STs[par][:, si, 0:n], stmp[:, 0:n], gateB[par][g][:, 0:n], ALU.mult,
                                ["stmp", ("gateB", g)], [("ST", par, si)])

            prep(0)
            for bi, (seq, t0, n) in enumerate(blocks):
                if bi + 1 < len(blocks):
                    prep(bi + 1)
                par = bi % 2
                ST = STs[par]
                isx = seq is X
                gt_i = 5
                rk = seq.name + "T"
                for dc in range(8):
                    po, pok = self.ps("sc")
                    terms = []
                    for g in range(GE):
                        if isx:
                            for jc in range(2):
                                terms.append((ygrp[:, g * 3 + jc, dc * 128:(dc + 1) * 128], ST[:, g * 2 + jc, 0:n], ("ST", par, g * 2 + jc)))
                        else:
                            terms.append((ygrp[0:32, g * 3 + 2, dc * 128:(dc + 1) * 128], ST[0:32, g * 2, 0:n], ("ST", par, g * 2)))
                    for ti, (lt_, rt_, sk) in enumerate(terms):
                        self.mm(po[:, 0:n], lt_, rt_, ti == 0, ti == len(terms) - 1, ["ygrp", sk], [pok])
                    self.stt("vector", seq.res[:, dc, t0:t0 + n], po[:, 0:n], self.mA[:, gt_i, dc, seq.mcol:seq.mcol + 1],
                             seq.res[:, dc, t0:t0 + n], ALU.mult, ALU.add, [pok, "mA", rk], [rk])
                    if dc % 2 == 1:
                        yield None

        pending = None
        wi = 0
        di = 0
        for e in range(NE):
            el = e % GE
            for tc in range(16):
                P.op("vector", lambda en, tc=tc, e=e: en.tensor_scalar(out=S[:, tc, :], in0=iota, scalar1=postok["x"][:, tc, e:e + 1],
                                                                     scalar2=None, op0=ALU.is_equal, saturate=False),
                     ["cst", "postokx"], [("S", tc)])
            if Cq is not None:
                for tc in range(2):
                    P.op("vector", lambda en, tc=tc, e=e: en.tensor_scalar(out=Sc[:, tc, :], in0=iota[:, 0:32],
                                                                         scalar1=postok["c"][:, tc, e:e + 1], scalar2=None,
                                                                         op0=ALU.is_equal, saturate=False),
                         ["cst", "postokc"], [("Sc", tc)])
            for dc in range(8):
                pg, pgk = self.ps("a")
                for tc in range(16):
                    self.mm(pg[:, 0:256], h2tok["x"][:, tc, dc * 128:(dc + 1) * 128], S[:, tc, :], tc == 0, tc == 15, ["h2tokx", ("S", tc)], [pgk])
                if Cq is not None:
                    for tc in range(2):
                        self.mm(pg[:, 256:288], h2tok["c"][:, tc, dc * 128:(dc + 1) * 128], Sc[:, tc, :], tc == 0, tc == 1,
                                ["h2tokc", ("Sc", tc)], [pgk])
                self.cp("scalar", xsT[:, dc, :], pg[:, 0:NJ], [pgk], ["xsT"])
            wd_pre = {}
            for pc in range(8):
                if pc in (5, 6):
                    q_ = pc - 5
                    rq = di % 2
                    di += 1
                    self.load_w(wdp[rq], self.I[f"wd{l}"][e, q_], ("wdp", rq))
                    wd_pre[q_] = rq
                r_ = wi % NR
                wi += 1
                self.load_w(wgp[r_], self.I[f"wg{l}"][e, pc], ("wgp", r_))
                self.load_w(wup[r_], self.I[f"wu{l}"][e, pc], ("wup", r_))
                for fc in range(2):
                    f = pc * 2 + fc
                    pa, pak = self.ps("b")
                    pu, puk = self.ps("c")
                    for dc in range(8):
                        self.mm(pa[:, 0:NJ], wgp[r_][:, dc, fc * 128:(fc + 1) * 128], xsT[:, dc, :], dc == 0, dc == 7, [("wgp", r_), "xsT"], [pak])
                    for dc in range(8):
                        self.mm(pu[:, 0:NJ], wup[r_][:, dc, fc * 128:(fc + 1) * 128], xsT[:, dc, :], dc == 0, dc == 7, [("wup", r_), "xsT"], [puk])
                    self.act(sa, pa[:, 0:NJ], AF.Silu, [pak], ["sa"])
                    self.tt("vector", actT[:, f, :], sa, pu[:, 0:NJ], ALU.mult, ["sa", puk], ["actT"])
                    for _ in range(3):
                        if pending is not None:
                            if next(pending, "done") == "done":
                                pending = None
            if pending is not None:
                for _ in pending:
                    pass
                pending = None
            for q in range(4):
                if q in wd_pre:
                    r_ = wd_pre[q]
                else:
                    r_ = di % 2
                    di += 1
                    self.load_w(wdp[r_], self.I[f"wd{l}"][e, q], ("wdp", r_))
                for jc in range(njc):
                    rows = 128 if jc < 2 else 32
                    py, pyk = self.ps("d")
                    for f in range(16):
                        self.mm(py[0:rows, 0:256], actT[:, f, jc * 128:jc * 128 + rows], wdp[r_][:, f, :], f == 0, f == 15,
                                ["actT", ("wdp", r_)], [pyk])
                    self.cp("scalar", ygrp[0:rows, el * 3 + jc, q * 256:(q + 1) * 256], py[0:rows, 0:256], [pyk], ["ygrp"])
            if el == GE - 1:
                pending = scatter_gen(e)
        if pending is not None:
            for _ in pending:
                pass
        P.barrier()
        A.reset(m0)
        self.AR.reset(mr0)

    def layer(self, l, last):
        P, A = self.P, self.A
        li = self.layers.index(l)
        A.reset(0)
        self.modulation(l)
        P.barrier()
        hx = A.alloc([8, T], BF16)
        hc = A.alloc([8, TC], BF16)
        self.mixT = A.alloc([2, T], BF16)
        self.X = Seq("x", T, self.xT, 0, True, hx, TC)
        self.C = Seq("c", TC, self.cT, 1, False, hc, 0)
        X, C = self.X, self.C
        self.norm_mod(X, 1, hx, "xh")
        self.norm_mod(C, 1, hc, "ch")
        P.barrier()
        self.dump(f"hx{l}", hx, ["xh"], [128, 8, T])
        seqs = [X] if last else [X, C]
        self.attention(l, seqs, last)
        P.barrier()
        for seq in seqs:
            self.pool_mixer(l, seq)
            P.barrier()
        for seq in seqs:
            self.conv_mixer(l, seq)
            P.barrier()
        for seq in seqs:
            self.sgu_mixer(l, seq)
            P.barrier()
        self.dump(f"xmid{l}", self.xT[:], ["xT"], [128, 8, T])
        if "nomoe" in self.dbg:
            return
        A.reset(0)
        self.moe(l, li, seqs)
        self.dump(f"xend{l}", self.xT[:], ["xT"], [128, 8, T])


_CACHE = {}


def _get_nc(layers, dbg=()):
    key = (tuple(layers), tuple(dbg))
    if key not in _CACHE:
        _CACHE[key] = KB(list(layers), dbg).build()
    return _CACHE[key]


def _run(inp, layers, cores, dbg=()):
    nc = _get_nc(layers, dbg)
    cst, rc, rs = _consts()
    shared = {"cst": cst, "rope_c": rc, "rope_s": rs}
    for l in layers:
        shared.update(_layer_arrays(inp, l))
    in_maps = []
    for b in cores:
        m = dict(shared)
        m["xT"] = np.ascontiguousarray(inp["x"][b].T)
        m["cT"] = np.ascontiguousarray(inp["ctx"][b].T)
        cv = np.stack([inp["c"][b], inp["c_ctx"]], axis=-1).astype(np.float32)
        m["cvec"] = np.ascontiguousarray(cv.reshape(8, 128, 2).transpose(1, 0, 2))
        in_maps.append(m)
    res = run_bass_kernel_spmd(nc, in_maps, core_ids=list(range(len(cores))))
    return res.results


def kernel(**inputs):
    inp = {k: np.asarray(v, dtype=np.float32) for k, v in inputs.items()}
    results = _run(inp, [0, 1], list(range(8)))
    out = np.stack([np.ascontiguousarray(r["outT"].T) for r in results], axis=0)
    return out.astype(np.float32)
```

```python
import contextlib
import math
import numpy as np
import concourse.bass as bass
import concourse.mybir as mybir
from concourse.bass_utils import run_bass_kernel_spmd

F32 = mybir.dt.float32
BF16 = mybir.dt.bfloat16
F32R = mybir.dt.float32r
FP8 = mybir.dt.float8e4
ALU = mybir.AluOpType
AF = mybir.ActivationFunctionType

ENGINES = ("sync", "scalar", "vector", "gpsimd", "tensor")
D = 1024
T = 2048
TC = 256
NE = 16
EPS = 1e-6
NV = 139
WARM_DUMMY = 2
PV_G1, PV_G2, PV_BPOOL, PV_PSC, PV_CONVB, PV_CLNG, PV_CLNB, PV_CONVW, PV_GQ, PV_GK, PV_GAO, PV_BMOD = (
    0, 8, 16, 18, 20, 22, 24, 26, 88, 89, 90, 91)
C_PERM, C_BD32, C_BD64, C_O1024, C_O256, C_IDENT, C_IOTA, C_JIDX, C_PRW, C_PFIX = (
    0, 128, 256, 384, 512, 640, 768, 1024, 1027, 1029)
NCST = 1029 + 32


class DmaSlot:
    def __init__(self, name):
        self.name = name
        self.sem = None
        self.count = 0


class _Op:
    __slots__ = ("eng", "fn", "deps", "raw", "signal", "sigval", "slot", "idx", "is_mm")

    def __init__(self, eng, fn, slot, is_mm):
        self.eng = eng
        self.fn = fn
        self.deps = set()
        self.raw = set()
        self.signal = False
        self.sigval = 0
        self.slot = slot
        self.is_mm = is_mm


class Prog:
    def __init__(self, nc):
        self.nc = nc
        self.ops = []
        self.last_writer = {}
        self.readers = {}
        self.slots = []
        self.last_on_eng = {}
        self.pool = {"hw": [self.slot(f"h{i}") for i in range(48)], "sw": [self.slot(f"s{i}") for i in range(48)]}
        self.pool_rr = {"hw": 0, "sw": 0}

    def slot(self, name):
        s = DmaSlot(name)
        self.slots.append(s)
        return s

    def op(self, eng, fn, reads=(), writes=(), slot=None, is_mm=False):
        o = _Op(eng, fn, slot, is_mm)
        o.idx = len(self.ops)
        for k in reads:
            w = self.last_writer.get(k)
            if w is not None:
                o.deps.add(w)
                o.raw.add(w)
        for k in writes:
            w = self.last_writer.get(k)
            if w is not None:
                o.deps.add(w)
            for r in self.readers.get(k, ()):
                o.deps.add(r)
        for k in reads:
            self.readers.setdefault(k, []).append(o.idx)
        for k in writes:
            self.last_writer[k] = o.idx
            self.readers[k] = []
        o.deps.discard(o.idx)
        self.ops.append(o)
        self.last_on_eng[(eng, slot)] = o.idx
        return o

    def dma(self, eng, out, in_, reads=(), writes=(), slot=None, **kw):
        kind = "sw" if eng == "gpsimd" else "hw"
        slot = self.pool[kind][self.pool_rr[kind] % len(self.pool[kind])]
        self.pool_rr[kind] += 1
        return self.op(eng, lambda e: e.dma_start(out=out, in_=in_, **kw), reads, writes, slot=slot)

    def barrier(self):
        last = set(self.last_on_eng.values())
        for e in ENGINES:
            o = self.op(e, lambda en: en.nop())
            o.deps |= {i for i in last if i != o.idx}
            o.raw |= o.deps

    def wait_all(self, eng, keys):
        return self.op(eng, lambda e: e.nop(), reads=keys)

    def _needs_wait(self, o, d):
        if d.slot is not None:
            return True
        if d.eng != o.eng:
            return True
        if o.slot is not None:
            return True
        if d.is_mm and o.is_mm:
            return False
        return True

    def emit(self, st):
        nc = self.nc
        ops = self.ops
        for o in ops:
            for di in o.deps:
                d = ops[di]
                if d.slot is None and self._needs_wait(o, d):
                    d.signal = True
        cnt = {e: 0 for e in ENGINES}
        for o in ops:
            if o.slot is not None:
                o.slot.count += 16
                o.sigval = o.slot.count
            elif o.signal:
                cnt[o.eng] += 1
                o.sigval = cnt[o.eng]
        esem = {e: st.enter_context(nc.semaphore("s_" + e)) for e in ENGINES}
        for s in self.slots:
            s.sem = st.enter_context(nc.semaphore("d_" + s.name))
        block = st.enter_context(nc.Block())

        def make(ename):
            def body(eng):
                known = {}
                for o in ops:
                    if o.eng != ename:
                        continue
                    need = {}
                    for di in o.deps:
                        d = ops[di]
                        if not self._needs_wait(o, d):
                            continue
                        sem = d.slot.sem if d.slot is not None else esem[d.eng]
                        k = id(sem)
                        if need.get(k, (None, 0))[1] < d.sigval:
                            need[k] = (sem, d.sigval)
                    for k, (sem, val) in need.items():
                        if known.get(k, 0) >= val:
                            continue
                        eng.wait_ge(sem, val)
                        known[k] = val
                    ins = o.fn(eng)
                    if o.slot is not None:
                        ins.then_inc(o.slot.sem, 16)
                    elif o.signal:
                        ins.then_inc(esem[ename], 1)
            return body

        for ename in ENGINES:
            if any(o.eng == ename for o in ops):
                getattr(block, ename)(make(ename))


class Arena:
    def __init__(self, tile, nwords):
        self.t = tile
        self.n = nwords
        self.off = 0

    def mark(self):
        return self.off

    def reset(self, to=0):
        self.off = to

    def alloc(self, free_shape, dtype, parts=128):
        n = int(np.prod(free_shape))
        esz = 2 if dtype == BF16 else (1 if dtype == FP8 else 4)
        words = (n * esz + 3) // 4
        words = (words + 7) // 8 * 8
        assert self.off + words <= self.n, ("arena overflow", self.off, words, self.n)
        ap = self.t[0:parts, self.off:self.off + words]
        self.off += words
        if dtype == BF16 or dtype == FP8:
            ap = ap.bitcast(dtype)
        ap = ap[:, 0:n]
        if len(free_shape) == 2:
            ap = ap.rearrange("p (a b) -> p a b", a=free_shape[0])
        elif len(free_shape) == 3:
            ap = ap.rearrange("p (a b c) -> p a b c", a=free_shape[0], b=free_shape[1])
        return ap


def _consts():
    cst = np.zeros((128, NCST), np.float32)
    p = np.arange(128)
    d = p % 32
    within = d % 16
    partner = np.where(within < 8, p + 8, p - 8)
    cst[partner, C_PERM + p] = 1.0
    for g in range(4):
        cst[g * 32:(g + 1) * 32, C_BD32 + g * 32:C_BD32 + (g + 1) * 32] = 1.0 / 32
    for g in range(2):
        cst[g * 64:(g + 1) * 64, C_BD64 + g * 64:C_BD64 + (g + 1) * 64] = 1.0 / 64
    cst[:, C_O1024:C_O1024 + 128] = 1.0 / 1024
    cst[:, C_O256:C_O256 + 128] = 1.0 / 256
    cst[p, C_IDENT + p] = 1.0
    cst[:, C_IOTA:C_IOTA + 256] = np.arange(256)[None, :]
    cst[:, C_JIDX] = p
    cst[:, C_JIDX + 1] = p + 128
    cst[:, C_JIDX + 2] = p
    wins = (2, 4, 8, 16)
    for c in range(2):
        for hf in range(2):
            w = wins[c * 2 + hf]
            rows = slice(hf * 64, hf * 64 + 64)
            cst[rows, C_PRW + c] = 1.0 / w
            for i in range(8):
                cnt_l = min(i + w // 2, 10 ** 9) - max(i - w // 2, 0)
                cst[rows, C_PFIX + c * 16 + i] = 1.0 / cnt_l
                cnt_r = min(w // 2, 8 - i) + w // 2
                cst[rows, C_PFIX + c * 16 + 8 + i] = 1.0 / cnt_r
    half = d // 16
    i_f = within % 8
    sign = np.where(within < 8, -1.0, 1.0)
    inv = 10000.0 ** (-(np.arange(8, dtype=np.float32)) / 8.0)
    t = np.arange(T)
    rows = (t // 64).astype(np.float32)
    cols = (t % 64).astype(np.float32)
    pos = np.where(half[:, None] == 0, rows[None, :], cols[None, :]).astype(np.float32)
    ang = pos * inv[i_f][:, None].astype(np.float32)
    rc = np.cos(ang).astype(np.float32)
    rs = (np.sin(ang) * sign[:, None]).astype(np.float32)
    return cst, np.ascontiguousarray(rc), np.ascontiguousarray(rs)


def _fm(v):
    return np.ascontiguousarray(v.reshape(-1, 128).T)


def _layer_arrays(inp, l):
    pv = np.zeros((128, NV), np.float32)
    pv[:, PV_G1:PV_G1 + 8] = _fm(inp["g_norm1"][l])
    pv[:, PV_G2:PV_G2 + 8] = _fm(inp["g_norm2"][l])
    pv[:, PV_BPOOL:PV_BPOOL + 2] = _fm(inp["b_pool"][l].reshape(-1))
    pv[:, PV_PSC:PV_PSC + 2] = _fm(inp["pool_scale"][l])
    pv[:, PV_CONVB:PV_CONVB + 2] = _fm(inp["conv_b"][l])
    pv[:, PV_CLNG:PV_CLNG + 2] = _fm(inp["conv_ln_g"][l])
    pv[:, PV_CLNB:PV_CLNB + 2] = _fm(inp["conv_ln_b"][l])
    cw = inp["conv_w"][l]
    for c in range(2):
        pv[:, PV_CONVW + c * 31:PV_CONVW + (c + 1) * 31] = cw[:, c * 128:(c + 1) * 128].T
    pv[:, PV_GQ] = np.tile(inp["g_q"][l], 4)
    pv[:, PV_GK] = np.tile(inp["g_k"][l], 4)
    pv[:, PV_GAO] = np.tile(inp["g_attn_out"][l], 2)
    pv[:, PV_BMOD:PV_BMOD + 48] = _fm(inp["b_mod"][l])
    rowv = np.concatenate([inp["sgu_ln_g"][l], inp["sgu_ln_b"][l], inp["lam_q1"][l], inp["lam_k1"][l],
                           inp["lam_q2"][l], inp["lam_k2"][l]]).astype(np.float32)
    wspT = np.ascontiguousarray(inp["w_spatial"][l].transpose(2, 0, 1))
    bs = inp["b_spatial"][l]
    bsp = np.zeros((128, 2, 128), np.float32)
    for cc in range(2):
        for hf in range(2):
            bsp[hf * 64:(hf + 1) * 64, cc, :] = bs[cc * 2 + hf][None, :]
    wr = np.ascontiguousarray(inp["w_router"][l].reshape(8, 128, NE).transpose(1, 0, 2))
    return {
        f"wmod{l}": inp["w_mod"][l], f"pvec{l}": pv, f"rowv{l}": rowv, f"w_in{l}": inp["w_in"][l],
        f"w_out{l}": inp["w_out"][l], f"wpool{l}": inp["w_pool"][l], f"wpw2{l}": inp["w_pw2"][l],
        f"wspT{l}": wspT, f"bsp{l}": bsp, f"wr{l}": wr,
        f"wg{l}": np.ascontiguousarray(inp["w_gate"][l].reshape(NE, 8, 128, 8, 256).transpose(0, 3, 2, 1, 4)),
        f"wu{l}": np.ascontiguousarray(inp["w_up"][l].reshape(NE, 8, 128, 8, 256).transpose(0, 3, 2, 1, 4)),
        f"wd{l}": np.ascontiguousarray(inp["w_down"][l].reshape(NE, 16, 128, 4, 256).transpose(0, 3, 2, 1, 4)),
    }


LAYER_SHAPES = {
    "wmod": [1024, 6144], "pvec": [128, NV], "rowv": [640], "w_in": [1024, 2048], "w_out": [1024, 1024],
    "wpool": [4, 64, 64], "wpw2": [256, 256], "wspT": [128, 4, 128], "bsp": [128, 2, 128], "wr": [128, 8, NE],
    "wg": [NE, 8, 128, 8, 256], "wu": [NE, 8, 128, 8, 256], "wd": [NE, 4, 128, 16, 256],
}


class Seq:
    def __init__(self, name, Tn, res, mcol, rope, h_ap, off):
        self.name = name
        self.Tn = Tn
        self.res = res
        self.mcol = mcol
        self.rope = rope
        self.h = h_ap
        self.off = off
        n = min(512, Tn)
        self.blocks = [(t0, n) for t0 in range(0, Tn, n)]
        self.nch = Tn // 128


class KB:
    def __init__(self, layers, dbg=()):
        self.layers = layers
        self.dbg = dbg
        self.nc = bass.Bass("TRN2", target_bir_lowering=False)
        self.P = Prog(self.nc)
        self._psrr = {}

    def tt(self, eng, out, in0, in1, op, r, w):
        return self.P.op(eng, lambda e: e.tensor_tensor(out=out, in0=in0, in1=in1, op=op), r, w)

    def ts(self, eng, out, in0, s1, s2, op0, op1=None, r=(), w=()):
        if op1 is None:
            return self.P.op(eng, lambda e: e.tensor_scalar(out=out, in0=in0, scalar1=s1, scalar2=None, op0=op0), r, w)
        return self.P.op(eng, lambda e: e.tensor_scalar(out=out, in0=in0, scalar1=s1, scalar2=s2, op0=op0, op1=op1), r, w)

    def stt(self, eng, out, in0, sc, in1, op0, op1, r, w):
        return self.P.op(eng, lambda e: e.scalar_tensor_tensor(out=out, in0=in0, scalar=sc, in1=in1, op0=op0, op1=op1), r, w)

    def cp(self, eng, out, in_, r, w):
        if eng == "scalar":
            return self.P.op(eng, lambda e: e.activation(out=out, in_=in_, func=AF.Copy), r, w)
        return self.P.op(eng, lambda e: e.tensor_copy(out=out, in_=in_), r, w)

    def act(self, out, in_, func, r, w, bias=None, scale=None):
        kw = {}
        if bias is not None:
            kw["bias"] = bias
        if scale is not None:
            kw["scale"] = scale
        return self.P.op("scalar", lambda e: e.activation(out=out, in_=in_, func=func, **kw), r, w)

    def mm(self, out, lhsT, rhs, start, stop, r, w, tp=None):
        kw = {}
        if tp is not None:
            kw["tile_position"] = tp
        return self.P.op("tensor", lambda e: e.matmul(out, lhsT=lhsT, rhs=rhs, start=start, stop=stop, **kw),
                         r, w, is_mm=True)

    def memset(self, eng, ap, val, w):
        return self.P.op(eng, lambda e: e.memset(ap, val), (), w)

    def ps(self, pool):
        lst = self.pspools[pool]
        i = self._psrr.get(pool, 0)
        self._psrr[pool] = i + 1
        b = lst[i % len(lst)]
        return self.PS[b], ("ps", b)

    def rstd_from(self, out, in_, r, w, tmpkey=None):
        self.act(out, in_, AF.Ln, r, w, bias=self.eps_ap[0:out.shape[0], :] if hasattr(out, "shape") else self.eps_ap)
        self.act(out, out, AF.Exp, w, w, scale=-0.5)

    def dump(self, name, ap, rkeys, shape):
        if name not in self.dbg:
            return
        d = self.nc.dram_tensor("dbg_" + name, list(shape), F32, kind="ExternalOutput").ap()
        self.P.dma("gpsimd", d, ap, reads=rkeys, writes=["dbgout_" + name], slot=self.s_out)
        self.outkeys.append("dbgout_" + name)

    def build(self):
        nc, P = self.nc, self.P
        st = contextlib.ExitStack()
        self.st = st
        dram = lambda n, s: nc.dram_tensor(n, list(s), F32, kind="ExternalInput").ap()
        self.I = {"xT": dram("xT", [D, T]), "cT": dram("cT", [D, TC]), "cvec": dram("cvec", [128, 8, 2]),
                  "cst": dram("cst", [128, NCST]), "rope_c": dram("rope_c", [128, T]), "rope_s": dram("rope_s", [128, T])}
        for l in self.layers:
            for k, s in LAYER_SHAPES.items():
                self.I[f"{k}{l}"] = dram(f"{k}{l}", s)
        self.out = nc.dram_tensor("outT", [D, T], F32, kind="ExternalOutput").ap()
        self.scr_pos = nc.dram_tensor("scr_pos", [2, NE, T + TC], BF16, kind="Internal").ap()
        self.scr_gate = nc.dram_tensor("scr_gate", [2, NE, T + TC], BF16, kind="Internal").ap()
        sb = lambda n, s, d: st.enter_context(nc.sbuf_tensor(n, s, d))
        self.xT = sb("xT_sb", [128, 8, T], F32)
        self.cT = sb("cT_sb", [128, 8, TC], F32)
        self.cst_p = sb("cst_sb", [128, NCST - 768], F32)
        self.cstR = sb("cstR_sb", [128, 768], F32R)
        self.pvec = sb("pvec_sb", [128, NV], F32)
        self.modT = sb("modT_sb", [128, 48, 2], F32)
        self.mA = sb("mA_sb", [128, 6, 8, 2], F32)
        self.small = sb("small_sb", [128, 64], F32)
        AW = 29328
        self.arena_t = sb("arena_sb", [128, AW], F32)
        self.A = Arena(self.arena_t, AW)
        self.arenaR_t = sb("arenaR_sb", [128, 3840], F32R)
        self.AR = Arena(self.arenaR_t, 3840)
        self.psall = st.enter_context(nc.psum_tensor("psall", [128, 4096], F32))
        self.PS = [self.psall[:, i * 512:(i + 1) * 512] for i in range(8)]
        self.pspools = {"a": [0, 1], "b": [2, 3], "c": [4, 5], "d": [6, 7], "sc": [0, 6, 1, 7]}
        self.s_in = None
        self.s_out = None
        self.s_w = [None]
        self.outkeys = []
        self.wslot_rr = 0

        P.dma("sync", self.xT[:], self.I["xT"].rearrange("(c p) t -> p c t", p=128), writes=["xT"], slot=self.s_in)
        P.dma("sync", self.cT[:], self.I["cT"].rearrange("(c p) t -> p c t", p=128), writes=["cT"], slot=self.s_in)
        P.dma("sync", self.cst_p[:], self.I["cst"][:, 768:NCST], writes=["cst"], slot=self.s_in)
        cst0 = self.A.alloc([768], F32)
        P.dma("sync", cst0, self.I["cst"][:, 0:768], writes=["cst0"], slot=self.s_in)
        self.cp("vector", self.cstR[:], cst0, ["cst0"], ["cstR"])
        P.barrier()
        self.A.reset(0)

        class _CstView:
            def __getitem__(_s, key):
                ps_, cs_ = key
                assert cs_.start >= 768, cs_
                return self.cst_p[ps_, cs_.start - 768:cs_.stop - 768]
        self.cst = _CstView()
        self.R_perm = self.cstR[:, C_PERM:C_PERM + 128]
        self.R_bd32 = self.cstR[:, C_BD32:C_BD32 + 128]
        self.R_bd64 = self.cstR[:, C_BD64:C_BD64 + 128]
        self.R_o1024 = self.cstR[:, C_O1024:C_O1024 + 128]
        self.R_o256 = self.cstR[:, C_O256:C_O256 + 128]
        self.R_ident = self.cstR[:, C_IDENT:C_IDENT + 128]

        for li, l in enumerate(self.layers):
            self.layer(l, last=(l == 1))
        for c in range(8):
            P.dma("sync", self.out[c * 128:(c + 1) * 128, :], self.xT[:, c, :], reads=["xT"], writes=[f"out{c}"], slot=self.s_out)
            self.outkeys.append(f"out{c}")
        P.wait_all("sync", self.outkeys)
        P.emit(st)
        return nc

    def wslot(self):
        s = self.s_w[self.wslot_rr % len(self.s_w)]
        self.wslot_rr += 1
        return s

    def modulation(self, l):
        P, A = self.P, self.A
        m0 = A.mark()
        cv = A.alloc([8, 2], F32)
        cvb = A.alloc([8, 2], BF16)
        sg = A.alloc([8, 2], F32)
        P.dma("sync", cv, self.I["cvec"], writes=["cv"], slot=self.s_in)
        P.dma("sync", self.pvec[:], self.I[f"pvec{l}"], writes=["pvec"], slot=self.s_in)
        self.act(sg, cv, AF.Sigmoid, ["cv"], ["sg"])
        self.tt("vector", cvb, cv, sg, ALU.mult, ["cv", "sg"], ["cvb"])
        wm = [A.alloc([8, 1024], BF16) for _ in range(2)]
        pst, pk = self.ps("d")
        wv = self.I[f"wmod{l}"].rearrange("(c p) m -> p c m", p=128)
        for j in range(6):
            P.dma("gpsimd", wm[j % 2], wv[:, :, j * 1024:(j + 1) * 1024], writes=[("wm", j % 2)], slot=self.wslot())
            for jj in range(8):
                col = j * 8 + jj
                for c in range(8):
                    self.mm(pst[:, 2 * col:2 * col + 2], wm[j % 2][:, c, jj * 128:(jj + 1) * 128], cvb[:, c, :],
                            c == 0, c == 7, [("wm", j % 2), "cvb"], [pk])
        self.tt("vector", self.modT[:], pst[:, 0:96].rearrange("p (j n) -> p j n", n=2),
                self.pvec[:, PV_BMOD:PV_BMOD + 48].unsqueeze(2).broadcast_to([128, 48, 2]), ALU.add,
                [pk, "pvec"], ["modT"])
        md = lambda i: self.modT[:, i * 8:(i + 1) * 8, :]
        for (dst, sc_i, g_off) in ((0, 1, PV_G1), (3, 4, PV_G2)):
            self.stt("vector", self.mA[:, dst], md(sc_i), 1.0,
                     self.pvec[:, g_off:g_off + 8].unsqueeze(2).broadcast_to([128, 8, 2]), ALU.add, ALU.mult,
                     ["modT", "pvec"], ["mA"])
        for (dst, src) in ((1, 0), (2, 2), (4, 3), (5, 5)):
            self.cp("vector", self.mA[:, dst], md(src), ["modT", "mA"], ["mA"])
        A.reset(m0)

    def norm_mod(self, seq, which, out_bf, okey, out_f32r=None, fkey=None, cb=None, local=False, bs=512):
        P, A = self.P, self.A
        ia, ish = (0, 1) if which == 1 else (3, 4)
        m0 = A.mark()
        mr0 = self.AR.mark()
        nsq = 2 if self.AR.n - self.AR.off >= 1024 else 1
        sqs = [self.AR.alloc([512], F32R) for _ in range(nsq)]
        rs = A.alloc([512], F32)
        tmps = [A.alloc([512], F32) for _ in range(2)]
        rk = seq.name + "T"
        nb_ = min(bs, seq.Tn)
        for (t0, n) in [(t, nb_) for t in range(0, seq.Tn, nb_)]:
            pst, pk = self.ps("c")
            for c in range(8):
                sq = sqs[c % nsq]
                sk = ("nm_sq", c % nsq)
                self.act(sq[:, 0:n], seq.res[:, c, t0:t0 + n], AF.Square, [rk], [sk])
                self.mm(pst[:, 0:n], self.R_o1024, sq[:, 0:n], c == 0, c == 7, [sk, "cstR"], [pk])
            self.act(rs[:, 0:n], pst[:, 0:n], AF.Ln, [pk], ["nm_rs"], bias=EPS)
            self.act(rs[:, 0:n], rs[:, 0:n], AF.Exp, ["nm_rs"], ["nm_rs"], scale=-0.5)
            for c in range(8):
                tmp = tmps[c % 2]
                tk = ("nm_tmp", c % 2)
                self.stt("vector", tmp[:, 0:n], seq.res[:, c, t0:t0 + n], self.mA[:, ia, c, seq.mcol:seq.mcol + 1],
                         rs[:, 0:n], ALU.mult, ALU.mult, [rk, "mA", "nm_rs"], [tk])
                if out_f32r is not None:
                    self.ts("vector", out_f32r[:, c, 0:n], tmp[:, 0:n], self.mA[:, ish, c, seq.mcol:seq.mcol + 1], None,
                            ALU.add, None, [tk, "mA"], [(fkey, c)])
                    ob = out_bf[:, c, 0:n] if local else out_bf[:, c, t0:t0 + n]
                    self.cp("scalar", ob, out_f32r[:, c, 0:n].bitcast(F32), [(fkey, c)], [(okey, c)])
                else:
                    self.ts("vector", out_bf[:, c, t0:t0 + n], tmp[:, 0:n], self.mA[:, ish, c, seq.mcol:seq.mcol + 1], None,
                            ALU.add, None, [tk, "mA"], [okey])
            if cb is not None:
                cb(t0, n)
        A.reset(m0)
        self.AR.reset(mr0)

    def load_w(self, dst, src_view, key):
        self.P.dma("gpsimd", dst, src_view, writes=[key], slot=self.wslot())

    def proj_fm(self, pst, pk, seq, wt, wkey, col0, M, t0, n, hkey):
        for c in range(8):
            self.mm(pst[0:M, 0:n], wt[:, c, col0:col0 + M], seq.h[:, c, t0:t0 + n], c == 0, c == 7, [wkey, hkey], [pk])

    def proj_tm(self, pst, pk, seq, wt, wkey, col0, ncols, tc, hkey):
        for c in range(8):
            self.mm(pst[:, 0:ncols], seq.h[:, c, tc * 128:(tc + 1) * 128], wt[:, c, col0:col0 + ncols], c == 0, c == 7,
                    [wkey, hkey], [pk])

    def wout_partial(self, l, seq, mixT, mkey, grp):
        A = self.A
        m0 = A.mark()
        wo = A.alloc([2, 1024], BF16)
        self.load_w(wo, self.I[f"w_out{l}"][grp * 256:(grp + 1) * 256, :].rearrange("(k p) d -> p k d", p=128), "wo")
        rk = seq.name + "T"
        for (t0, n) in seq.blocks:
            for dc in range(8):
                pst, pk = self.ps("d")
                for k in range(2):
                    self.mm(pst[:, 0:n], wo[:, k, dc * 128:(dc + 1) * 128], mixT[:, k, t0:t0 + n], k == 0, k == 1,
                            ["wo", mkey], [pk])
                self.stt("vector", seq.res[:, dc, t0:t0 + n], pst[:, 0:n], self.mA[:, 2, dc, seq.mcol:seq.mcol + 1],
                         seq.res[:, dc, t0:t0 + n], ALU.mult, ALU.add, [pk, "mA", rk], [rk])
        A.reset(m0)

    def qk_norm(self, pst, pk, n, gcol, rope_t0, out_bf, okey, Ws):
        wi_ = self._qkrr = getattr(self, "_qkrr", 0) + 1
        wi_ %= len(Ws)
        sq, rs, qn, t1, rc, rsn = Ws[wi_]
        self.act(sq[:, 0:n], pst[:, 0:n], AF.Square, [pk], [("qk_sq", wi_)])
        ps2, pk2 = self.ps("c")
        self.mm(ps2[:, 0:n], self.R_bd32, sq[:, 0:n], True, True, [("qk_sq", wi_), "cstR"], [pk2])
        self.act(rs[:, 0:n], ps2[:, 0:n], AF.Ln, [pk2], [("qk_rs", wi_)], bias=EPS)
        self.act(rs[:, 0:n], rs[:, 0:n], AF.Exp, [("qk_rs", wi_)], [("qk_rs", wi_)], scale=-0.5)
        if rope_t0 is None:
            self.stt("vector", out_bf, pst[:, 0:n], self.pvec[:, gcol:gcol + 1], rs[:, 0:n], ALU.mult, ALU.mult,
                     [pk, "pvec", ("qk_rs", wi_)], [okey])
            return
        self.stt("vector", qn[:, 0:n], pst[:, 0:n], self.pvec[:, gcol:gcol + 1], rs[:, 0:n], ALU.mult, ALU.mult,
                 [pk, "pvec", ("qk_rs", wi_)], [("qk_qn", wi_)])
        self.P.dma("sync", rc[:, 0:n], self.I["rope_c"][:, rope_t0:rope_t0 + n], writes=[("rope_c", wi_)], slot=self.s_in)
        self.P.dma("sync", rsn[:, 0:n], self.I["rope_s"][:, rope_t0:rope_t0 + n], writes=[("rope_s", wi_)], slot=self.s_in)
        ps3, pk3 = self.ps("c")
        self.mm(ps3[:, 0:n], self.R_perm, qn[:, 0:n], True, True, [("qk_qn", wi_), "cstR"], [pk3])
        self.tt("gpsimd", t1[:, 0:n], qn[:, 0:n].bitcast(F32), rc[:, 0:n], ALU.mult, [("qk_qn", wi_), ("rope_c", wi_)], [("qk_t1", wi_)])
        self.tt("vector", rs[:, 0:n], ps3[:, 0:n], rsn[:, 0:n], ALU.mult, [pk3, ("rope_s", wi_), ("qk_rs", wi_)], [("qk_rs", wi_)])
        self.tt("vector", out_bf, t1[:, 0:n], rs[:, 0:n], ALU.add, [("qk_t1", wi_), ("qk_rs", wi_)], [okey])

    def attention(self, l, seqs, last):
        P, A = self.P, self.A
        X, C = self.X, self.C
        m0 = A.mark()
        lam_init = 0.8 - 0.6 * math.exp(-0.3 * l)
        mr0 = self.AR.mark()
        kT = A.alloc([2, T + TC], BF16)
        qTs = {"x": A.alloc([2, T], BF16), "c": A.alloc([2, TC], BF16)}
        vaug = A.alloc([18, 4, 128], BF16)
        lamv = A.alloc([128], F32)
        lt = A.alloc([64], F32)
        mq = A.mark()
        wq = A.alloc([8, 768], BF16)
        self.load_w(wq, self.I[f"w_in{l}"][:, 0:768].rearrange("(c p) m -> p c m", p=128), "wq")
        W = []
        for _ in range(2):
            Wf = [A.alloc([512], F32) for _ in range(4)]
            W.append([self.AR.alloc([512], F32R), Wf[0], self.AR.alloc([512], F32R), Wf[1], Wf[2], Wf[3]])
        self.memset("gpsimd", vaug, 1.0, ["vaug"])
        P.dma("sync", lamv, self.I[f"rowv{l}"][512:640].partition_broadcast(128), writes=["lamv"], slot=self.s_in)
        self.tt("vector", lt[:, 0:32], lamv[:, 0:32], lamv[:, 32:64], ALU.mult, ["lamv"], ["lt"])
        self.tt("vector", lt[:, 32:64], lamv[:, 64:96], lamv[:, 96:128], ALU.mult, ["lamv", "lt"], ["lt"])
        sm = self.small
        P.op("vector", lambda e: e.reduce_sum(out=sm[:, 0:2], in_=lt.rearrange("p (a b) -> p a b", a=2),
                                              axis=mybir.AxisListType.X), ["lt"], ["sm_lam"])
        self.act(sm[:, 2:4], sm[:, 0:2], AF.Exp, ["sm_lam"], ["sm_lam2"])
        self.stt("vector", sm[:, 4:5], sm[:, 3:4], -lam_init, sm[:, 2:3], ALU.add, ALU.subtract, ["sm_lam2"], ["nlam"])
        self.ts("vector", sm[:, 5:6], self.pvec[:, PV_GAO:PV_GAO + 1], 1.0 - lam_init, None, ALU.mult, None, ["pvec"], ["gao2"])
        nlam = sm[:, 4:5]
        gao2 = sm[:, 5:6]
        for seq in (C, X):
            hk = seq.name + "h"
            for (t0, n) in seq.blocks:
                for kc in range(2):
                    pst, pk = self.ps("a")
                    self.proj_fm(pst, pk, seq, wq, "wq", 256 + kc * 128, 128, t0, n, hk)
                    for _d in range(2):
                        self.mm(self.PS[7][:, 0:n], wq[:, 0, 0:128], seq.h[:, 0, t0:t0 + n], True, True, ["wq", hk], [("ps", 7)])
                    self.qk_norm(pst, pk, n, PV_GK, t0 if seq.rope else None,
                                 kT[:, kc, seq.off + t0:seq.off + t0 + n], "kT", W)
            for tc in range(seq.nch):
                pst, pk = self.ps("b")
                self.proj_tm(pst, pk, seq, wq, "wq", 512, 256, tc, hk)
                kcg = seq.off // 128 + tc
                pv = pst[:, 0:256].rearrange("p (a b c) -> p a b c", a=2, b=2)
                va = vaug[:, kcg].rearrange("p (a b) c -> p a b c", a=2)
                self.cp("vector", va[:, :, 0, 0:64], pv[:, :, 0, :], [pk], ["vaug"])
                self.cp("scalar", va[:, :, 1, 64:128], pv[:, :, 1, :], [pk], ["vaug"])
        for seq in seqs:
            hk = seq.name + "h"
            for (t0, n) in seq.blocks:
                for qc in range(2):
                    pst, pk = self.ps("a")
                    self.proj_fm(pst, pk, seq, wq, "wq", qc * 128, 128, t0, n, hk)
                    for _d in range(2):
                        self.mm(self.PS[7][:, 0:n], wq[:, 0, 0:128], seq.h[:, 0, t0:t0 + n], True, True, ["wq", hk], [("ps", 7)])
                    self.qk_norm(pst, pk, n, PV_GQ, t0 if seq.rope else None, qTs[seq.name][:, qc, t0:t0 + n], "qT" + seq.name, W)
        P.barrier()
        A.reset(mq)
        O1r = W[0][0]
        O1 = A.alloc([512], F32)
        R = [A.alloc([512], F32) for _ in range(2)]
        Tm = [A.alloc([512], F32) for _ in range(2)]
        ocat = A.alloc([512], F32)
        pTT = [A.alloc([2, 512], BF16) for _ in range(2)]
        mixT = self.mixT
        sc = 32 ** -0.5
        for seq in seqs:
            hk = seq.name + "h"
            mk = "mix" + seq.name
            kchunks = list(range(0, 2)) + (list(range(2, 18)) if seq is X else [])
            qT = qTs[seq.name]
            qk_ = "qT" + seq.name
            for (t0, n) in seq.blocks:
                for qc in range(2):
                    for hh in range(2):
                        h = qc * 2 + hh
                        nb, db = hh * 64, 64 - hh * 64
                        obanks = [self.ps("b"), self.ps("b")]
                        pend = None
                        for i, kc in enumerate(kchunks):
                            b0 = 0 if i % 2 == 0 else 6
                            for s_ in range(2):
                                base = hh * 64 + s_ * 32
                                self.mm(self.PS[b0 + s_][:, 0:n], kT[base:base + 32, qc, kc * 128:(kc + 1) * 128],
                                        qT[base:base + 32, qc, t0:t0 + n], True, True, ["kT", qk_], [("ps", b0 + s_)], tp=(base, 0))
                            if pend is not None:
                                for s_ in range(2):
                                    po, pok = obanks[s_]
                                    pe_ = pend[s_]
                                    self.mm(po[:, 0:n], pe_[0], pe_[1], pe_[2], False, ["vaug", pe_[3]], [pok])
                            for _d in range(WARM_DUMMY):
                                self.mm(self.PS[4][:, 0:n], kT[:, qc, kc * 128:(kc + 1) * 128], qT[:, qc, t0:t0 + n], True, True,
                                        ["kT", qk_], [("ps", 4)])
                            pt2 = pTT[i % 2]
                            ptk = ("pTT", i % 2)
                            src = self.psall[:, b0 * 512:(b0 + 2) * 512].rearrange("p (s n) -> p s n", s=2)[:, :, 0:n]
                            self.act(pt2[:, :, 0:n], src, AF.Exp, [("ps", b0), ("ps", b0 + 1)], [ptk], scale=sc)
                            pend = [(vaug[:, kc, h, :], pt2[:, s_, 0:n], i == 0, ptk) for s_ in range(2)]
                        for s_ in range(2):
                            po, pok = obanks[s_]
                            pe_ = pend[s_]
                            self.mm(po[:, 0:n], pe_[0], pe_[1], pe_[2], True, ["vaug", pe_[3]], [pok])
                        for s_ in range(2):
                            po, pok = obanks[s_]
                            self.act(Tm[s_][db:db + 64, 0:n], po[db:db + 64, 0:n], AF.Ln, [pok], [("Tm", s_)])
                            self.act(R[s_][nb:nb + 64, 0:n], Tm[s_][db:db + 64, 0:n], AF.Exp, [("Tm", s_)], [("R", s_)], scale=-1.0)
                            self.tt("vector", Tm[s_][nb:nb + 64, 0:n], po[nb:nb + 64, 0:n], R[s_][nb:nb + 64, 0:n], ALU.mult,
                                    [pok, ("R", s_), ("Tm", s_)], [("Tm", s_)])
                        self.stt("vector", ocat[nb:nb + 64, 0:n], Tm[1][nb:nb + 64, 0:n], nlam[nb:nb + 64, :],
                                 Tm[0][nb:nb + 64, 0:n], ALU.mult, ALU.add, [("Tm", 0), ("Tm", 1), "nlam"], ["ocat"])
                    self.act(O1r[:, 0:n], ocat[:, 0:n], AF.Square, ["ocat"], ["O1r"])
                    ps2, pk2 = self.ps("c")
                    self.mm(ps2[:, 0:n], self.R_bd64, O1r[:, 0:n], True, True, ["O1r", "cstR"], [pk2])
                    self.act(O1[:, 0:n], ps2[:, 0:n], AF.Ln, [pk2], ["O1"], bias=EPS)
                    self.act(O1[:, 0:n], O1[:, 0:n], AF.Exp, ["O1"], ["O1"], scale=-0.5)
                    self.stt("vector", mixT[:, qc, t0:t0 + n], ocat[:, 0:n], gao2, O1[:, 0:n], ALU.mult, ALU.mult,
                             ["ocat", "gao2", "O1"], [mk])
            self.dump(f"attn_{seq.name}{l}", mixT[:, :, 0:seq.Tn], [mk], [128, 2, seq.Tn])
            self.wout_partial(l, seq, mixT, mk, 0)
        A.reset(m0)
        self.AR.reset(mr0)

    def pool_mixer(self, l, seq):
        P, A = self.P, self.A
        m0 = A.mark()
        Tn = seq.Tn
        Wd = Tn + 16
        hk = seq.name + "h"
        mk = "mix" + seq.name
        wp = A.alloc([8, 256], BF16)
        self.load_w(wp, self.I[f"w_in{l}"][:, 768:1024].rearrange("(c p) m -> p c m", p=128), "wp")
        wbd = A.alloc([2, 128], BF16)
        self.memset("gpsimd", wbd, 0.0, ["wbd"])
        for g in range(4):
            c, hf = g // 2, g % 2
            P.dma("gpsimd", wbd[hf * 64:(hf + 1) * 64, c, hf * 64:(hf + 1) * 64], self.I[f"wpool{l}"][g],
                  reads=["wbd"], writes=["wbd"], slot=self.wslot())
        zps = [A.alloc([Wd], F32) for _ in range(2)]
        Ab1 = A.alloc([Wd], F32)
        Abs_ = [Ab1, Ab1]
        Bb = A.alloc([Wd], F32)
        Cb = A.alloc([Wd], F32)
        Sb1 = A.alloc([Tn], F32)
        Sbs = [Sb1, Sb1]
        dTs = [A.alloc([Tn], BF16) for _ in range(2)]
        mixT = self.mixT
        G = "gpsimd"
        for c in range(2):
            zp, Ab, Sb, dT = zps[c], Abs_[c], Sbs[c], dTs[c]
            self.memset(G, zp[:, 0:8], 0.0, [("zp", c)])
            self.memset(G, zp[:, 8 + Tn:Wd], 0.0, [("zp", c)])
            for (t0, n) in seq.blocks:
                pst, pk = self.ps("a")
                self.proj_fm(pst, pk, seq, wp, "wp", c * 128, 128, t0, n, hk)
                self.cp("scalar", zp[:, 8 + t0:8 + t0 + n], pst[:, 0:n], [pk], [("zp", c)])
            lo, hi = slice(0, 64), slice(64, 128)
            if c == 0:
                self.tt(G, Sb[lo, :], zp[lo, 7:7 + Tn], zp[lo, 8:8 + Tn], ALU.add, [("zp", c)], ["Sb"])
                self.tt(G, Ab[hi, 0:Wd - 1], zp[hi, 0:Wd - 1], zp[hi, 1:Wd], ALU.add, [("zp", c)], ["Ab"])
                self.tt(G, Sb[hi, :], Ab[hi, 6:6 + Tn], Ab[hi, 8:8 + Tn], ALU.add, ["Ab", "Sb"], ["Sb"])
            else:
                self.tt(G, Ab[:, 0:Wd - 1], zp[:, 0:Wd - 1], zp[:, 1:Wd], ALU.add, [("zp", c)], ["Ab"])
                self.tt(G, Bb[:, 0:Wd - 3], Ab[:, 0:Wd - 3], Ab[:, 2:Wd - 1], ALU.add, ["Ab"], ["Bb"])
                self.tt(G, Sb[lo, :], Bb[lo, 4:4 + Tn], Bb[lo, 8:8 + Tn], ALU.add, ["Bb"], ["Sb"])
                self.tt(G, Cb[hi, 0:Wd - 7], Bb[hi, 0:Wd - 7], Bb[hi, 4:Wd - 3], ALU.add, ["Bb"], ["Cb"])
                self.tt(G, Sb[hi, :], Cb[hi, 0:Tn], Cb[hi, 8:8 + Tn], ALU.add, ["Cb", "Sb"], ["Sb"])
            self.stt("vector", dT[:, :], Sb[:, :], self.cst[:, C_PRW + c:C_PRW + c + 1], zp[:, 8:8 + Tn], ALU.mult, ALU.subtract,
                     ["Sb", "cst", ("zp", c)], [("dT", c)])
            for side, (a0, f0) in enumerate(((0, 0), (Tn - 8, 8))):
                fx = self.cst[:, C_PFIX + c * 16 + f0:C_PFIX + c * 16 + f0 + 8]
                self.tt("vector", Sb[:, a0:a0 + 8], Sb[:, a0:a0 + 8], fx, ALU.mult, ["Sb", "cst", ("dT", c)], ["Sb"])
                self.tt("vector", dT[:, a0:a0 + 8], Sb[:, a0:a0 + 8], zp[:, 8 + a0:8 + a0 + 8], ALU.subtract, ["Sb", ("zp", c)], [("dT", c)])
            for (t0, n) in seq.blocks:
                pst, pk = self.ps("b")
                self.mm(pst[:, 0:n], wbd[:, c, :], dT[:, t0:t0 + n], True, True, ["wbd", ("dT", c)], [pk])
                self.ts("vector", mixT[:, c, t0:t0 + n], pst[:, 0:n], self.pvec[:, PV_BPOOL + c:PV_BPOOL + c + 1],
                        self.pvec[:, PV_PSC + c:PV_PSC + c + 1], ALU.add, ALU.mult, [pk, "pvec"], [mk])
        self.dump(f"pool_{seq.name}{l}", mixT[:, :, 0:Tn], [mk], [128, 2, Tn])
        self.wout_partial(l, seq, mixT, mk, 1)
        A.reset(m0)

    def conv_mixer(self, l, seq):
        P, A = self.P, self.A
        m0 = A.mark()
        Tn = seq.Tn
        hk = seq.name + "h"
        mk = "mix" + seq.name
        wc = A.alloc([8, 512], BF16)
        self.load_w(wc, self.I[f"w_in{l}"][:, 1024:1536].rearrange("(c p) m -> p c m", p=128), "wc")
        wpw = A.alloc([2, 256], BF16)
        self.load_w(wpw, self.I[f"wpw2{l}"].rearrange("(c p) m -> p c m", p=128), "wpw")
        yp = A.alloc([2, Tn + 30], BF16)
        dg = A.alloc([2, 31, 128], BF16)
        sgt = A.alloc([512], F32)
        mr0 = self.AR.mark()
        sq = self.AR.alloc([512], F32R)
        accR = self.AR.alloc([2, 512], F32R)
        m2 = A.alloc([512], F32)
        var = A.alloc([512], F32)
        tt_ = A.alloc([512], F32)
        actT = A.alloc([2, 512], BF16)
        mixT = self.mixT
        identF = self.R_ident.bitcast(F32)
        for c in range(2):
            for k in range(31):
                self.ts("vector", dg[:, c, k, :], identF, self.pvec[:, PV_CONVW + c * 31 + k:PV_CONVW + c * 31 + k + 1], None,
                        ALU.mult, None, ["cstR", "pvec"], [("dg", c, k)])
        for c in range(2):
            self.memset("gpsimd", yp[:, c, 0:15], 0.0, [("yp", c)])
            self.memset("gpsimd", yp[:, c, 15 + Tn:30 + Tn], 0.0, [("yp", c)])
            for (t0, n) in seq.blocks:
                pv, pvk = self.ps("a")
                self.proj_fm(pv, pvk, seq, wc, "wc", c * 128, 128, t0, n, hk)
                pg, pgk = self.ps("b")
                self.proj_fm(pg, pgk, seq, wc, "wc", 256 + c * 128, 128, t0, n, hk)
                self.act(sgt[:, 0:n], pg[:, 0:n], AF.Sigmoid, [pgk], ["sgt"])
                self.tt("vector", yp[:, c, 15 + t0:15 + t0 + n], pv[:, 0:n], sgt[:, 0:n], ALU.mult, [pvk, "sgt"], [("yp", c)])
        for (t0, n) in seq.blocks:
            pm, pmk = self.ps("c")
            for c in range(2):
                pc_, pck = self.ps("d")
                for k in range(31):
                    self.mm(pc_[:, 0:n], dg[:, c, k, :], yp[:, c, t0 + k:t0 + k + n], k == 0, k == 30, [("dg", c, k), ("yp", c)], [pck])
                self.ts("vector", accR[:, c, 0:n], pc_[:, 0:n], self.pvec[:, PV_CONVB + c:PV_CONVB + c + 1], None, ALU.add, None,
                        [pck, "pvec"], [("accR", c)])
                self.mm(pm[:, 0:n], self.R_o256, accR[:, c, 0:n], c == 0, c == 1, [("accR", c), "cstR"], [pmk])
            pq, pqk = self.ps("c")
            for c in range(2):
                self.act(sq[:, 0:n], accR[:, c, 0:n].bitcast(F32), AF.Square, [("accR", c)], ["cv_sq"])
                self.mm(pq[:, 0:n], self.R_o256, sq[:, 0:n], c == 0, c == 1, ["cv_sq", "cstR"], [pqk])
            self.act(m2[:, 0:n], pm[:, 0:n], AF.Square, [pmk], ["cv_m2"])
            self.tt("vector", var[:, 0:n], pq[:, 0:n], m2[:, 0:n], ALU.subtract, [pqk, "cv_m2"], ["cv_var"])
            self.act(var[:, 0:n], var[:, 0:n], AF.Ln, ["cv_var"], ["cv_var"], bias=EPS)
            self.act(var[:, 0:n], var[:, 0:n], AF.Exp, ["cv_var"], ["cv_var"], scale=-0.5)
            for c in range(2):
                self.tt("vector", tt_[:, 0:n], accR[:, c, 0:n].bitcast(F32), pm[:, 0:n], ALU.subtract, [("accR", c), pmk], ["cv_t"])
                self.tt("vector", tt_[:, 0:n], tt_[:, 0:n], var[:, 0:n], ALU.mult, ["cv_t", "cv_var"], ["cv_t"])
                self.act(actT[:, c, 0:n], tt_[:, 0:n], AF.Silu, ["cv_t", "pvec"], [("cv_act", c)],
                         bias=self.pvec[:, PV_CLNB + c:PV_CLNB + c + 1], scale=self.pvec[:, PV_CLNG + c:PV_CLNG + c + 1])
            for co in range(2):
                po, pok = self.ps("b")
                for ci in range(2):
                    self.mm(po[:, 0:n], wpw[:, ci, co * 128:(co + 1) * 128], actT[:, ci, 0:n], ci == 0, ci == 1,
                            ["wpw", ("cv_act", ci)], [pok])
                self.cp("scalar", mixT[:, co, t0:t0 + n], po[:, 0:n], [pok], [mk])
        self.dump(f"conv_{seq.name}{l}", mixT[:, :, 0:Tn], [mk], [128, 2, Tn])
        self.wout_partial(l, seq, mixT, mk, 2)
        A.reset(m0)
        self.AR.reset(mr0)

    def sgu_mixer(self, l, seq):
        P, A = self.P, self.A
        m0 = A.mark()
        Tn = seq.Tn
        hk = seq.name + "h"
        mk = "mix" + seq.name
        ws = A.alloc([8, 512], BF16)
        self.load_w(ws, self.I[f"w_in{l}"][:, 1536:2048].rearrange("(c p) m -> p c m", p=128), "ws")
        wsp = A.alloc([4, 128], BF16)
        self.load_w(wsp, self.I[f"wspT{l}"], "wsp")
        bsp = A.alloc([2, 128], F32)
        P.dma("sync", bsp, self.I[f"bsp{l}"], writes=["bsp"], slot=self.s_in)
        lnr = A.alloc([512], F32)
        P.dma("sync", lnr, self.I[f"rowv{l}"][0:512].partition_broadcast(128), writes=["lnr"], slot=self.s_in)
        uT = A.alloc([2, Tn], F32)
        vgs = [A.alloc([256], F32) for _ in range(2)]
        vts = [A.alloc([256], F32) for _ in range(2)]
        st6s = [A.alloc([8], F32) for _ in range(2)]
        VA = [A.alloc([256], BF16) for _ in range(2)]
        VB = [A.alloc([256], BF16) for _ in range(2)]
        tmp = A.alloc([512], F32)
        mixT = self.mixT
        for i in range(2):
            self.memset("gpsimd", VA[i], 0.0, [("VA", i)])
            self.memset("gpsimd", VB[i], 0.0, [("VB", i)])
        for c in range(2):
            for (t0, n) in seq.blocks:
                pst, pk = self.ps("a")
                self.proj_fm(pst, pk, seq, ws, "ws", c * 128, 128, t0, n, hk)
                self.act(uT[:, c, t0:t0 + n], pst[:, 0:n], AF.Gelu_apprx_tanh, [pk], ["uT"])
        nblk = len(seq.blocks)
        per = seq.blocks[0][1] // 128
        for bi, (t0, n) in enumerate(seq.blocks):
            pcc = [self.ps("d"), self.ps("d")]
            for j in range(per):
                tc = t0 // 128 + j
                i = tc % 2
                pst, pk = self.ps("b")
                self.proj_tm(pst, pk, seq, ws, "ws", 256, 256, tc, hk)
                vg, vt, st6 = vgs[i], vts[i], st6s[i]
                kvg, kvt, kst = ("vg", i), ("vt", i), ("st", i)
                self.act(vg, pst[:, 0:256], AF.Gelu_apprx_tanh, [pk], [kvg])
                P.op("vector", lambda e, st6=st6, vg=vg: e.bn_stats(out=st6[:, 0:6], in_=vg), [kvg], [kst])
                P.op("vector", lambda e, st6=st6: e.bn_aggr(out=st6[:, 6:8], in_=st6[:, 0:6]), [kst], [kst])
                self.act(st6[:, 7:8], st6[:, 7:8], AF.Ln, [kst], [kst], bias=EPS)
                self.act(st6[:, 7:8], st6[:, 7:8], AF.Exp, [kst], [kst], scale=-0.5)
                self.ts("vector", vt, vg, st6[:, 6:7], st6[:, 7:8], ALU.subtract, ALU.mult, [kvg, kst], [kvt])
                self.tt("vector", vt, vt, lnr[:, 0:256], ALU.mult, [kvt, "lnr"], [kvt])
                v3 = vt.rearrange("p (a b) -> p a b", a=2)
                b3 = lnr[:, 256:512].rearrange("p (a b) -> p a b", a=2)
                self.tt("vector", VA[i].rearrange("p (a b) -> p a b", a=2)[:, :, 0:64], v3[:, :, 0:64], b3[:, :, 0:64], ALU.add,
                        [kvt, "lnr"], [("VA", i)])
                self.tt("vector", VB[i].rearrange("p (a b) -> p a b", a=2)[:, :, 64:128], v3[:, :, 64:128], b3[:, :, 64:128], ALU.add,
                        [kvt, "lnr"], [("VB", i)])
                for cc in range(2):
                    po, pok = pcc[cc]
                    self.mm(po[:, j * 128:(j + 1) * 128], VA[i][:, cc * 128:(cc + 1) * 128], wsp[:, 2 * cc, :], True, False,
                            [("VA", i), "wsp"], [pok])
                    self.mm(po[:, j * 128:(j + 1) * 128], VB[i][:, cc * 128:(cc + 1) * 128], wsp[:, 2 * cc + 1, :], False, True,
                            [("VB", i), "wsp"], [pok])
            for cc in range(2):
                po, pok = pcc[cc]
                self.tt("vector", tmp[:, 0:n].rearrange("p (a b) -> p a b", b=128), po[:, 0:n].rearrange("p (a b) -> p a b", b=128),
                        bsp[:, cc, :].unsqueeze(1).broadcast_to([128, per, 128]), ALU.add, [pok, "bsp"], ["sg_tmp"])
                self.tt("vector", mixT[:, cc, t0:t0 + n], tmp[:, 0:n], uT[:, cc, t0:t0 + n], ALU.mult, ["sg_tmp", "uT"], [mk])
        self.dump(f"sgu_{seq.name}{l}", mixT[:, :, 0:Tn], [mk], [128, 2, Tn])
        self.wout_partial(l, seq, mixT, mk, 3)
        A.reset(m0)

    def moe(self, l, li, seqs):
        P, A = self.P, self.A
        m0 = A.mark()
        GE = 2
        NJ = 256 + (32 if len(seqs) > 1 else 0)
        njc = 3 if len(seqs) > 1 else 2
        mr0 = self.AR.mark()
        wrR = self.AR.alloc([8 * 128], F32R)
        h2f = self.AR.alloc([8, 256], F32R)
        affEb = self.AR.alloc([256], F32R)
        h2tok = {}
        postok = {}
        for seq in seqs:
            h2tok[seq.name] = A.alloc([seq.nch, 1024], BF16)
            postok[seq.name] = A.alloc([seq.nch, NE], F32)
        m1 = A.mark()
        identB = A.alloc([128], BF16)
        self.cp("vector", identB, self.R_ident.bitcast(F32), ["cstR"], ["identB"])
        wr0 = A.alloc([8, NE], F32)
        P.dma("sync", wr0, self.I[f"wr{l}"], writes=["wr0"], slot=self.s_in)
        self.ts("vector", wrR, self.xT[:, 0, 0:1024], 0.0, None, ALU.mult, None, ["xT"], ["wrR"])
        for c in range(8):
            self.cp("vector", wrR[:, c * 128:c * 128 + NE], wr0[:, c, :], ["wr0", "wrR"], ["wrR"])
        m1b = A.mark()
        for seq in seqs:
            Tn = seq.Tn
            cap = 2 * Tn // NE
            sn = seq.name
            P.barrier()
            A.reset(m1b)
            hb = A.alloc([8, 256], BF16)
            affE = None
            aff = A.alloc([Tn], F32)
            work = A.alloc([Tn], F32)
            ones = A.alloc([Tn], F32)
            cs = A.alloc([Tn], F32)
            mask = A.alloc([Tn], F32)
            posb = A.alloc([Tn], BF16)
            gateb = A.alloc([Tn], BF16)
            mx = A.alloc([8], F32)

            def cb(t0, n, seq=seq, hb=hb, sn=sn, aff=aff, work=work):
                pl, plk = self.ps("a")
                for c in range(8):
                    self.mm(pl[:, 0:n], wrR[:, c * 128:(c + 1) * 128], h2f[:, c, 0:n], c == 0, c == 7,
                            ["wrR", ("h2f", c)], [plk])
                self.act(affEb[0:NE, 0:n], pl[0:NE, 0:n], AF.Exp, [plk], ["affEb"])
                pss, psk = self.ps("c")
                self.mm(pss[:, 0:n], self.R_o1024[0:NE, :], affEb[0:NE, 0:n], True, True, ["affEb", "cstR"], [psk])
                self.act(work[0:NE, t0:t0 + n], pss[0:NE, 0:n], AF.Ln, [psk], ["work"])
                self.act(work[0:NE, t0:t0 + n], work[0:NE, t0:t0 + n], AF.Exp, ["work"], ["work"], scale=-1.0)
                self.stt("vector", aff[0:NE, t0:t0 + n], affEb[0:NE, 0:n].bitcast(F32), 1.0 / 1024, work[0:NE, t0:t0 + n], ALU.mult, ALU.mult,
                         ["affEb", "work"], ["aff"])
                for j in range(n // 128):
                    tc = t0 // 128 + j
                    pt, ptk = self.ps("b")
                    ptb = pt.bitcast(BF16)
                    for dc in range(8):
                        P.op("tensor", lambda e, dc=dc, j=j, ptb=ptb: e.transpose(out=ptb[:, dc * 128:(dc + 1) * 128],
                                                                                 in_=hb[:, dc, j * 128:(j + 1) * 128], identity=identB),
                             [("hb", dc), "identB"], [ptk], is_mm=True)
                    self.cp("scalar", h2tok[sn][:, tc, :], ptb[:, 0:1024], [ptk], ["h2tok" + sn])

            self.norm_mod(seq, 2, hb, "hb", out_f32r=h2f, fkey="h2f", cb=cb, local=True, bs=256)
            self.dump(f"aff_{sn}{l}", aff[0:NE, :], ["aff"], [NE, Tn])
            lo_, mid_, cnt_, tmp_ = mx[0:NE, 0:1], mx[0:NE, 1:2], mx[0:NE, 2:3], mx[0:NE, 3:4]
            self.memset("vector", lo_, 0.0, ["bs_lo"])
            for k in range(26):
                wk = 0.5 ** (k + 1)
                self.ts("vector", mid_, lo_, wk, None, ALU.add, None, ["bs_lo"], ["bs_mid"])
                P.op("vector", lambda e, work=work, aff=aff, mid_=mid_, cnt_=cnt_: e.tensor_scalar(
                    out=work[0:NE, :], in0=aff[0:NE, :], scalar1=mid_, scalar2=0.0, op0=ALU.is_ge, op1=ALU.add, accum_out=cnt_),
                    ["aff", "bs_mid"], ["work", "bs_cnt"])
                self.ts("vector", tmp_, cnt_, cap - 0.5, wk, ALU.is_ge, ALU.mult, ["bs_cnt"], ["bs_tmp"])
                self.tt("vector", lo_, lo_, tmp_, ALU.add, ["bs_lo", "bs_tmp"], ["bs_lo"])
            self.ts("vector", mask[0:NE, :], aff[0:NE, :], lo_, None, ALU.is_ge, None, ["aff", "bs_lo"], ["mask"])
            self.memset("gpsimd", ones[0:NE, :], 1.0, ["ones"])
            P.op("vector", lambda e, cs=cs, ones=ones, mask=mask: e.tensor_tensor_scan(out=cs[0:NE, :], data0=ones[0:NE, :], data1=mask[0:NE, :], initial=0.0,
                                                          op0=ALU.mult, op1=ALU.add), ["ones", "mask"], ["cs"])
            self.tt("vector", cs[0:NE, :], cs[0:NE, :], mask[0:NE, :], ALU.mult, ["cs", "mask"], ["cs"])
            self.ts("vector", cs[0:NE, :], cs[0:NE, :], -1.0, None, ALU.add, None, ["cs"], ["cs"])
            self.cp("vector", posb[0:NE, :], cs[0:NE, :], ["cs"], ["posb"])
            self.tt("vector", gateb[0:NE, :], aff[0:NE, :], mask[0:NE, :], ALU.mult, ["aff", "mask"], ["gateb"])
            P.dma("sync", self.scr_pos[li, :, seq.off:seq.off + Tn], posb[0:NE, :], reads=["posb"], writes=["scr_pos"], slot=self.s_in)
            P.dma("sync", self.scr_gate[li, :, seq.off:seq.off + Tn], gateb[0:NE, :], reads=["gateb"], writes=["scr_gate"], slot=self.s_in)
            pt, ptk = self.ps("c")
            for tc in range(seq.nch):
                P.op("tensor", lambda e, tc=tc, pt=pt, cs=cs: e.transpose(out=pt[:, tc * NE:(tc + 1) * NE], in_=cs[0:NE, tc * 128:(tc + 1) * 128],
                                                                   identity=self.R_ident.bitcast(F32)[0:NE, 0:NE]),
                     ["cs", "cstR"], [ptk], is_mm=True)
            self.cp("vector", postok[sn], pt[:, 0:seq.nch * NE].rearrange("p (a b) -> p a b", b=NE), [ptk], ["postok" + sn])
        P.barrier()
        A.reset(m1)
        X = seqs[0]
        Cq = seqs[1] if len(seqs) > 1 else None
        S = A.alloc([16, 256], FP8)
        Sc = A.alloc([2, 32], FP8)
        xsT = A.alloc([8, NJ], BF16)
        actT = A.alloc([16, NJ], BF16)
        sa = A.alloc([NJ], F32)
        NR = 3
        wgp = [A.alloc([8, 256], BF16) for _ in range(NR)]
        wup = [A.alloc([8, 256], BF16) for _ in range(NR)]
        wdp = [A.alloc([16, 256], BF16) for _ in range(2)]
        ygrp = A.alloc([GE * 3, 1024], BF16)
        _pb = [A.alloc([256], BF16) for _ in range(GE)]
        _gb = [A.alloc([256], BF16) for _ in range(GE)]
        posB = [_pb, _pb]
        gateB = [_gb, _gb]
        STs = [A.alloc([GE * 2, 256], BF16) for _ in range(2)]
        iota = self.cst[:, C_IOTA:C_IOTA + 256]
        jidx = lambda jc: self.cst[:, C_JIDX + jc:C_JIDX + jc + 1]
        def scatter_gen(e):
            e0 = e - (GE - 1)
            blocks = [(seq, t0, 256) for seq in seqs for t0 in range(0, seq.Tn, 256)]

            def prep(bi):
                seq, t0, n = blocks[bi]
                par = bi % 2
                isx = seq is X
                for g in range(GE):
                    P.dma("sync", posB[par][g][:, 0:n], self.scr_pos[li, e0 + g, seq.off + t0:seq.off + t0 + n].partition_broadcast(128),
                          reads=["scr_pos"], writes=[("posB", g)], slot=self.s_in)
                    P.dma("sync", gateB[par][g][:, 0:n], self.scr_gate[li, e0 + g, seq.off + t0:seq.off + t0 + n].partition_broadcast(128),
                          reads=["scr_gate"], writes=[("gateB", g)], slot=self.s_in)
                    for jc in (range(2) if isx else (2,)):
                        si = g * 2 + (jc if isx else 0)
                        self.stt("vector", STs[par][:, si, 0:n], posB[par][g][:, 0:n], jidx(jc), gateB[par][g][:, 0:n], ALU.is_equal, ALU.mult,
                                 [("posB", g), ("gateB", g), "cst"], [("ST", par, si)])

            prep(0)
            for bi, (seq, t0, n) in enumerate(blocks):
                if bi + 1 < len(blocks):
                    prep(bi + 1)
                par = bi % 2
                ST = STs[par]
                isx = seq is X
                gt_i = 5
                rk = seq.name + "T"
                for dc in range(8):
                    po, pok = self.ps("sc")
                    terms = []
                    for g in range(GE):
                        if isx:
                            for jc in range(2):
                                terms.append((ygrp[:, g * 3 + jc, dc * 128:(dc + 1) * 128], ST[:, g * 2 + jc, 0:n], ("ST", par, g * 2 + jc)))
                        else:
                            terms.append((ygrp[0:32, g * 3 + 2, dc * 128:(dc + 1) * 128], ST[0:32, g * 2, 0:n], ("ST", par, g * 2)))
                    for ti, (lt_, rt_, sk) in enumerate(terms):
                        self.mm(po[:, 0:n], lt_, rt_, ti == 0, ti == len(terms) - 1, ["ygrp", sk], [pok])
                    self.stt("vector", seq.res[:, dc, t0:t0 + n], po[:, 0:n], self.mA[:, gt_i, dc, seq.mcol:seq.mcol + 1],
                             seq.res[:, dc, t0:t0 + n], ALU.mult, ALU.add, [pok, "mA", rk], [rk])
                    if dc % 2 == 1:
                        yield None

        pending = None
        wi = 0
        di = 0
        for e in range(NE):
            el = e % GE
            for tc in range(16):
                P.op("vector", lambda en, tc=tc, e=e: en.tensor_scalar(out=S[:, tc, :], in0=iota, scalar1=postok["x"][:, tc, e:e + 1],
                                                                     scalar2=None, op0=ALU.is_equal, saturate=False),
                     ["cst", "postokx"], [("S", tc)])
            if Cq is not None:
                for tc in range(2):
                    P.op("vector", lambda en, tc=tc, e=e: en.tensor_scalar(out=Sc[:, tc, :], in0=iota[:, 0:32],
                                                                         scalar1=postok["c"][:, tc, e:e + 1], scalar2=None,
                                                                         op0=ALU.is_equal, saturate=False),
                         ["cst", "postokc"], [("Sc", tc)])
            for dc in range(8):
                pg, pgk = self.ps("a")
                for tc in range(16):
                    self.mm(pg[:, 0:256], h2tok["x"][:, tc, dc * 128:(dc + 1) * 128], S[:, tc, :], tc == 0, tc == 15, ["h2tokx", ("S", tc)], [pgk])
                if Cq is not None:
                    for tc in range(2):
                        self.mm(pg[:, 256:288], h2tok["c"][:, tc, dc * 128:(dc + 1) * 128], Sc[:, tc, :], tc == 0, tc == 1,
                                ["h2tokc", ("Sc", tc)], [pgk])
                self.cp("scalar", xsT[:, dc, :], pg[:, 0:NJ], [pgk], ["xsT"])
            wd_pre = {}
            for pc in range(8):
                if pc in (5, 6):
                    q_ = pc - 5
                    rq = di % 2
                    di += 1
                    self.load_w(wdp[rq], self.I[f"wd{l}"][e, q_], ("wdp", rq))
                    wd_pre[q_] = rq
                r_ = wi % NR
                wi += 1
                self.load_w(wgp[r_], self.I[f"wg{l}"][e, pc], ("wgp", r_))
                self.load_w(wup[r_], self.I[f"wu{l}"][e, pc], ("wup", r_))
                for fc in range(2):
                    f = pc * 2 + fc
                    pa, pak = self.ps("b")
                    pu, puk = self.ps("c")
                    for dc in range(8):
                        self.mm(pa[:, 0:NJ], wgp[r_][:, dc, fc * 128:(fc + 1) * 128], xsT[:, dc, :], dc == 0, dc == 7, [("wgp", r_), "xsT"], [pak])
                    for dc in range(8):
                        self.mm(pu[:, 0:NJ], wup[r_][:, dc, fc * 128:(fc + 1) * 128], xsT[:, dc, :], dc == 0, dc == 7, [("wup", r_), "xsT"], [puk])
                    self.act(sa, pa[:, 0:NJ], AF.Silu, [pak], ["sa"])
                    self.tt("vector", actT[:, f, :], sa, pu[:, 0:NJ], ALU.mult, ["sa", puk], ["actT"])
                    for _ in range(3):
                        if pending is not None:
                            if next(pending, "done") == "done":
                                pending = None
            if pending is not None:
                for _ in pending:
                    pass
                pending = None
            for q in range(4):
                if q in wd_pre:
                    r_ = wd_pre[q]
                else:
                    r_ = di % 2
                    di += 1
                    self.load_w(wdp[r_], self.I[f"wd{l}"][e, q], ("wdp", r_))
                for jc in range(njc):
                    rows = 128 if jc < 2 else 32
                    py, pyk = self.ps("d")
                    for f in range(16):
                        self.mm(py[0:rows, 0:256], actT[:, f, jc * 128:jc * 128 + rows], wdp[r_][:, f, :], f == 0, f == 15,
                                ["actT", ("wdp", r_)], [pyk])
                    self.cp("scalar", ygrp[0:rows, el * 3 + jc, q * 256:(q + 1) * 256], py[0:rows, 0:256], [pyk], ["ygrp"])
            if el == GE - 1:
                pending = scatter_gen(e)
        if pending is not None:
            for _ in pending:
                pass
        P.barrier()
        A.reset(m0)
        self.AR.reset(mr0)

    def layer(self, l, last):
        P, A = self.P, self.A
        li = self.layers.index(l)
        A.reset(0)
        self.modulation(l)
        P.barrier()
        hx = A.alloc([8, T], BF16)
        hc = A.alloc([8, TC], BF16)
        self.mixT = A.alloc([2, T], BF16)
        self.X = Seq("x", T, self.xT, 0, True, hx, TC)
        self.C = Seq("c", TC, self.cT, 1, False, hc, 0)
        X, C = self.X, self.C
        self.norm_mod(X, 1, hx, "xh")
        self.norm_mod(C, 1, hc, "ch")
        P.barrier()
        self.dump(f"hx{l}", hx, ["xh"], [128, 8, T])
        seqs = [X] if last else [X, C]
        self.attention(l, seqs, last)
        P.barrier()
        for seq in seqs:
            self.pool_mixer(l, seq)
            P.barrier()
        for seq in seqs:
            self.conv_mixer(l, seq)
            P.barrier()
        for seq in seqs:
            self.sgu_mixer(l, seq)
            P.barrier()
        self.dump(f"xmid{l}", self.xT[:], ["xT"], [128, 8, T])
        if "nomoe" in self.dbg:
            return
        A.reset(0)
        self.moe(l, li, seqs)
        self.dump(f"xend{l}", self.xT[:], ["xT"], [128, 8, T])


_CACHE = {}


def _get_nc(layers, dbg=()):
    key = (tuple(layers), tuple(dbg))
    if key not in _CACHE:
        _CACHE[key] = KB(list(layers), dbg).build()
    return _CACHE[key]


def _run(inp, layers, cores, dbg=()):
    nc = _get_nc(layers, dbg)
    cst, rc, rs = _consts()
    shared = {"cst": cst, "rope_c": rc, "rope_s": rs}
    for l in layers:
        shared.update(_layer_arrays(inp, l))
    in_maps = []
    for b in cores:
        m = dict(shared)
        m["xT"] = np.ascontiguousarray(inp["x"][b].T)
        m["cT"] = np.ascontiguousarray(inp["ctx"][b].T)
        cv = np.stack([inp["c"][b], inp["c_ctx"]], axis=-1).astype(np.float32)
        m["cvec"] = np.ascontiguousarray(cv.reshape(8, 128, 2).transpose(1, 0, 2))
        in_maps.append(m)
    res = run_bass_kernel_spmd(nc, in_maps, core_ids=list(range(len(cores))))
    return res.results


def kernel(**inputs):
    inp = {k: np.asarray(v, dtype=np.float32) for k, v in inputs.items()}
    results = _run(inp, [0, 1], list(range(8)))
    out = np.stack([np.ascontiguousarray(r["outT"].T) for r in results], axis=0)
    return out.astype(np.float32)
```

```python
import contextlib
import math
import numpy as np
import concourse.bass as bass
import concourse.mybir as mybir
from concourse.bass_utils import run_bass_kernel_spmd

F32 = mybir.dt.float32
BF16 = mybir.dt.bfloat16
F32R = mybir.dt.float32r
FP8 = mybir.dt.float8e4
ALU = mybir.AluOpType
AF = mybir.ActivationFunctionType

ENGINES = ("sync", "scalar", "vector", "gpsimd", "tensor")
D = 1024
T = 2048
TC = 256
NE = 16
EPS = 1e-6
NV = 139
WARM_DUMMY = 2
PV_G1, PV_G2, PV_BPOOL, PV_PSC, PV_CONVB, PV_CLNG, PV_CLNB, PV_CONVW, PV_GQ, PV_GK, PV_GAO, PV_BMOD = (
    0, 8, 16, 18, 20, 22, 24, 26, 88, 89, 90, 91)
C_PERM, C_BD32, C_BD64, C_O1024, C_O256, C_IDENT, C_IOTA, C_JIDX, C_PRW, C_PFIX = (
    0, 128, 256, 384, 512, 640, 768, 1024, 1027, 1029)
NCST = 1029 + 32


class DmaSlot:
    def __init__(self, name):
        self.name = name
        self.sem = None
        self.count = 0


class _Op:
    __slots__ = ("eng", "fn", "deps", "raw", "signal", "sigval", "slot", "idx", "is_mm")

    def __init__(self, eng, fn, slot, is_mm):
        self.eng = eng
        self.fn = fn
        self.deps = set()
        self.raw = set()
        self.signal = False
        self.sigval = 0
        self.slot = slot
        self.is_mm = is_mm


class Prog:
    def __init__(self, nc):
        self.nc = nc
        self.ops = []
        self.last_writer = {}
        self.readers = {}
        self.slots = []
        self.last_on_eng = {}
        self.pool = {"hw": [self.slot(f"h{i}") for i in range(48)], "sw": [self.slot(f"s{i}") for i in range(48)]}
        self.pool_rr = {"hw": 0, "sw": 0}

    def slot(self, name):
        s = DmaSlot(name)
        self.slots.append(s)
        return s

    def op(self, eng, fn, reads=(), writes=(), slot=None, is_mm=False):
        o = _Op(eng, fn, slot, is_mm)
        o.idx = len(self.ops)
        for k in reads:
            w = self.last_writer.get(k)
            if w is not None:
                o.deps.add(w)
                o.raw.add(w)
        for k in writes:
            w = self.last_writer.get(k)
            if w is not None:
                o.deps.add(w)
            for r in self.readers.get(k, ()):
                o.deps.add(r)
        for k in reads:
            self.readers.setdefault(k, []).append(o.idx)
        for k in writes:
            self.last_writer[k] = o.idx
            self.readers[k] = []
        o.deps.discard(o.idx)
        self.ops.append(o)
        self.last_on_eng[(eng, slot)] = o.idx
        return o

    def dma(self, eng, out, in_, reads=(), writes=(), slot=None, **kw):
        kind = "sw" if eng == "gpsimd" else "hw"
        slot = self.pool[kind][self.pool_rr[kind] % len(self.pool[kind])]
        self.pool_rr[kind] += 1
        return self.op(eng, lambda e: e.dma_start(out=out, in_=in_, **kw), reads, writes, slot=slot)

    def barrier(self):
        last = set(self.last_on_eng.values())
        for e in ENGINES:
            o = self.op(e, lambda en: en.nop())
            o.deps |= {i for i in last if i != o.idx}
            o.raw |= o.deps

    def wait_all(self, eng, keys):
        return self.op(eng, lambda e: e.nop(), reads=keys)

    def _needs_wait(self, o, d):
        if d.slot is not None:
            return True
        if d.eng != o.eng:
            return True
        if o.slot is not None:
            return True
        if d.is_mm and o.is_mm:
            return False
        return True

    def emit(self, st):
        nc = self.nc
        ops = self.ops
        for o in ops:
            for di in o.deps:
                d = ops[di]
                if d.slot is None and self._needs_wait(o, d):
                    d.signal = True
        cnt = {e: 0 for e in ENGINES}
        for o in ops:
            if o.slot is not None:
                o.slot.count += 16
                o.sigval = o.slot.count
            elif o.signal:
                cnt[o.eng] += 1
                o.sigval = cnt[o.eng]
        esem = {e: st.enter_context(nc.semaphore("s_" + e)) for e in ENGINES}
        for s in self.slots:
            s.sem = st.enter_context(nc.semaphore("d_" + s.name))
        block = st.enter_context(nc.Block())

        def make(ename):
            def body(eng):
                known = {}
                for o in ops:
                    if o.eng != ename:
                        continue
                    need = {}
                    for di in o.deps:
                        d = ops[di]
                        if not self._needs_wait(o, d):
                            continue
                        sem = d.slot.sem if d.slot is not None else esem[d.eng]
                        k = id(sem)
                        if need.get(k, (None, 0))[1] < d.sigval:
                            need[k] = (sem, d.sigval)
                    for k, (sem, val) in need.items():
                        if known.get(k, 0) >= val:
                            continue
                        eng.wait_ge(sem, val)
                        known[k] = val
                    ins = o.fn(eng)
                    if o.slot is not None:
                        ins.then_inc(o.slot.sem, 16)
                    elif o.signal:
                        ins.then_inc(esem[ename], 1)
            return body

        for ename in ENGINES:
            if any(o.eng == ename for o in ops):
                getattr(block, ename)(make(ename))


class Arena:
    def __init__(self, tile, nwords):
        self.t = tile
        self.n = nwords
        self.off = 0

    def mark(self):
        return self.off

    def reset(self, to=0):
        self.off = to

    def alloc(self, free_shape, dtype, parts=128):
        n = int(np.prod(free_shape))
        esz = 2 if dtype == BF16 else (1 if dtype == FP8 else 4)
        words = (n * esz + 3) // 4
        words = (words + 7) // 8 * 8
        assert self.off + words <= self.n, ("arena overflow", self.off, words, self.n)
        ap = self.t[0:parts, self.off:self.off + words]
        self.off += words
        if dtype == BF16 or dtype == FP8:
            ap = ap.bitcast(dtype)
        ap = ap[:, 0:n]
        if len(free_shape) == 2:
            ap = ap.rearrange("p (a b) -> p a b", a=free_shape[0])
        elif len(free_shape) == 3:
            ap = ap.rearrange("p (a b c) -> p a b c", a=free_shape[0], b=free_shape[1])
        return ap


def _consts():
    cst = np.zeros((128, NCST), np.float32)
    p = np.arange(128)
    d = p % 32
    within = d % 16
    partner = np.where(within < 8, p + 8, p - 8)
    cst[partner, C_PERM + p] = 1.0
    for g in range(4):
        cst[g * 32:(g + 1) * 32, C_BD32 + g * 32:C_BD32 + (g + 1) * 32] = 1.0 / 32
    for g in range(2):
        cst[g * 64:(g + 1) * 64, C_BD64 + g * 64:C_BD64 + (g + 1) * 64] = 1.0 / 64
    cst[:, C_O1024:C_O1024 + 128] = 1.0 / 1024
    cst[:, C_O256:C_O256 + 128] = 1.0 / 256
    cst[p, C_IDENT + p] = 1.0
    cst[:, C_IOTA:C_IOTA + 256] = np.arange(256)[None, :]
    cst[:, C_JIDX] = p
    cst[:, C_JIDX + 1] = p + 128
    cst[:, C_JIDX + 2] = p
    wins = (2, 4, 8, 16)
    for c in range(2):
        for hf in range(2):
            w = wins[c * 2 + hf]
            rows = slice(hf * 64, hf * 64 + 64)
            cst[rows, C_PRW + c] = 1.0 / w
            for i in range(8):
                cnt_l = min(i + w // 2, 10 ** 9) - max(i - w // 2, 0)
                cst[rows, C_PFIX + c * 16 + i] = 1.0 / cnt_l
                cnt_r = min(w // 2, 8 - i) + w // 2
                cst[rows, C_PFIX + c * 16 + 8 + i] = 1.0 / cnt_r
    half = d // 16
    i_f = within % 8
    sign = np.where(within < 8, -1.0, 1.0)
    inv = 10000.0 ** (-(np.arange(8, dtype=np.float32)) / 8.0)
    t = np.arange(T)
    rows = (t // 64).astype(np.float32)
    cols = (t % 64).astype(np.float32)
    pos = np.where(half[:, None] == 0, rows[None, :], cols[None, :]).astype(np.float32)
    ang = pos * inv[i_f][:, None].astype(np.float32)
    rc = np.cos(ang).astype(np.float32)
    rs = (np.sin(ang) * sign[:, None]).astype(np.float32)
    return cst, np.ascontiguousarray(rc), np.ascontiguousarray(rs)


def _fm(v):
    return np.ascontiguousarray(v.reshape(-1, 128).T)


def _layer_arrays(inp, l):
    pv = np.zeros((128, NV), np.float32)
    pv[:, PV_G1:PV_G1 + 8] = _fm(inp["g_norm1"][l])
    pv[:, PV_G2:PV_G2 + 8] = _fm(inp["g_norm2"][l])
    pv[:, PV_BPOOL:PV_BPOOL + 2] = _fm(inp["b_pool"][l].reshape(-1))
    pv[:, PV_PSC:PV_PSC + 2] = _fm(inp["pool_scale"][l])
    pv[:, PV_CONVB:PV_CONVB + 2] = _fm(inp["conv_b"][l])
    pv[:, PV_CLNG:PV_CLNG + 2] = _fm(inp["conv_ln_g"][l])
    pv[:, PV_CLNB:PV_CLNB + 2] = _fm(inp["conv_ln_b"][l])
    cw = inp["conv_w"][l]
    for c in range(2):
        pv[:, PV_CONVW + c * 31:PV_CONVW + (c + 1) * 31] = cw[:, c * 128:(c + 1) * 128].T
    pv[:, PV_GQ] = np.tile(inp["g_q"][l], 4)
    pv[:, PV_GK] = np.tile(inp["g_k"][l], 4)
    pv[:, PV_GAO] = np.tile(inp["g_attn_out"][l], 2)
    pv[:, PV_BMOD:PV_BMOD + 48] = _fm(inp["b_mod"][l])
    rowv = np.concatenate([inp["sgu_ln_g"][l], inp["sgu_ln_b"][l], inp["lam_q1"][l], inp["lam_k1"][l],
                           inp["lam_q2"][l], inp["lam_k2"][l]]).astype(np.float32)
    wspT = np.ascontiguousarray(inp["w_spatial"][l].transpose(2, 0, 1))
    bs = inp["b_spatial"][l]
    bsp = np.zeros((128, 2, 128), np.float32)
    for cc in range(2):
        for hf in range(2):
            bsp[hf * 64:(hf + 1) * 64, cc, :] = bs[cc * 2 + hf][None, :]
    wr = np.ascontiguousarray(inp["w_router"][l].reshape(8, 128, NE).transpose(1, 0, 2))
    return {
        f"wmod{l}": inp["w_mod"][l], f"pvec{l}": pv, f"rowv{l}": rowv, f"w_in{l}": inp["w_in"][l],
        f"w_out{l}": inp["w_out"][l], f"wpool{l}": inp["w_pool"][l], f"wpw2{l}": inp["w_pw2"][l],
        f"wspT{l}": wspT, f"bsp{l}": bsp, f"wr{l}": wr,
        f"wg{l}": np.ascontiguousarray(inp["w_gate"][l].reshape(NE, 8, 128, 8, 256).transpose(0, 3, 2, 1, 4)),
        f"wu{l}": np.ascontiguousarray(inp["w_up"][l].reshape(NE, 8, 128, 8, 256).transpose(0, 3, 2, 1, 4)),
        f"wd{l}": np.ascontiguousarray(inp["w_down"][l].reshape(NE, 16, 128, 4, 256).transpose(0, 3, 2, 1, 4)),
    }


LAYER_SHAPES = {
    "wmod": [1024, 6144], "pvec": [128, NV], "rowv": [640], "w_in": [1024, 2048], "w_out": [1024, 1024],
    "wpool": [4, 64, 64], "wpw2": [256, 256], "wspT": [128, 4, 128], "bsp": [128, 2, 128], "wr": [128, 8, NE],
    "wg": [NE, 8, 128, 8, 256], "wu": [NE, 8, 128, 8, 256], "wd": [NE, 4, 128, 16, 256],
}


class Seq:
    def __init__(self, name, Tn, res, mcol, rope, h_ap, off):
        self.name = name
        self.Tn = Tn
        self.res = res
        self.mcol = mcol
        self.rope = rope
        self.h = h_ap
        self.off = off
        n = min(512, Tn)
        self.blocks = [(t0, n) for t0 in range(0, Tn, n)]
        self.nch = Tn // 128


class KB:
    def __init__(self, layers, dbg=()):
        self.layers = layers
        self.dbg = dbg
        self.nc = bass.Bass("TRN2", target_bir_lowering=False)
        self.P = Prog(self.nc)
        self._psrr = {}

    def tt(self, eng, out, in0, in1, op, r, w):
        return self.P.op(eng, lambda e: e.tensor_tensor(out=out, in0=in0, in1=in1, op=op), r, w)

    def ts(self, eng, out, in0, s1, s2, op0, op1=None, r=(), w=()):
        if op1 is None:
            return self.P.op(eng, lambda e: e.tensor_scalar(out=out, in0=in0, scalar1=s1, scalar2=None, op0=op0), r, w)
        return self.P.op(eng, lambda e: e.tensor_scalar(out=out, in0=in0, scalar1=s1, scalar2=s2, op0=op0, op1=op1), r, w)

    def stt(self, eng, out, in0, sc, in1, op0, op1, r, w):
        return self.P.op(eng, lambda e: e.scalar_tensor_tensor(out=out, in0=in0, scalar=sc, in1=in1, op0=op0, op1=op1), r, w)

    def cp(self, eng, out, in_, r, w):
        if eng == "scalar":
            return self.P.op(eng, lambda e: e.activation(out=out, in_=in_, func=AF.Copy), r, w)
        return self.P.op(eng, lambda e: e.tensor_copy(out=out, in_=in_), r, w)

    def act(self, out, in_, func, r, w, bias=None, scale=None):
        kw = {}
        if bias is not None:
            kw["bias"] = bias
        if scale is not None:
            kw["scale"] = scale
        return self.P.op("scalar", lambda e: e.activation(out=out, in_=in_, func=func, **kw), r, w)

    def mm(self, out, lhsT, rhs, start, stop, r, w, tp=None):
        kw = {}
        if tp is not None:
            kw["tile_position"] = tp
        return self.P.op("tensor", lambda e: e.matmul(out, lhsT=lhsT, rhs=rhs, start=start, stop=stop, **kw),
                         r, w, is_mm=True)

    def memset(self, eng, ap, val, w):
        return self.P.op(eng, lambda e: e.memset(ap, val), (), w)

    def ps(self, pool):
        lst = self.pspools[pool]
        i = self._psrr.get(pool, 0)
        self._psrr[pool] = i + 1
        b = lst[i % len(lst)]
        return self.PS[b], ("ps", b)

    def rstd_from(self, out, in_, r, w, tmpkey=None):
        self.act(out, in_, AF.Ln, r, w, bias=self.eps_ap[0:out.shape[0], :] if hasattr(out, "shape") else self.eps_ap)
        self.act(out, out, AF.Exp, w, w, scale=-0.5)

    def dump(self, name, ap, rkeys, shape):
        if name not in self.dbg:
            return
        d = self.nc.dram_tensor("dbg_" + name, list(shape), F32, kind="ExternalOutput").ap()
        self.P.dma("gpsimd", d, ap, reads=rkeys, writes=["dbgout_" + name], slot=self.s_out)
        self.outkeys.append("dbgout_" + name)

    def build(self):
        nc, P = self.nc, self.P
        st = contextlib.ExitStack()
        self.st = st
        dram = lambda n, s: nc.dram_tensor(n, list(s), F32, kind="ExternalInput").ap()
        self.I = {"xT": dram("xT", [D, T]), "cT": dram("cT", [D, TC]), "cvec": dram("cvec", [128, 8, 2]),
                  "cst": dram("cst", [128, NCST]), "rope_c": dram("rope_c", [128, T]), "rope_s": dram("rope_s", [128, T])}
        for l in self.layers:
            for k, s in LAYER_SHAPES.items():
                self.I[f"{k}{l}"] = dram(f"{k}{l}", s)
        self.out = nc.dram_tensor("outT", [D, T], F32, kind="ExternalOutput").ap()
        self.scr_pos = nc.dram_tensor("scr_pos", [2, NE, T + TC], BF16, kind="Internal").ap()
        self.scr_gate = nc.dram_tensor("scr_gate", [2, NE, T + TC], BF16, kind="Internal").ap()
        sb = lambda n, s, d: st.enter_context(nc.sbuf_tensor(n, s, d))
        self.xT = sb("xT_sb", [128, 8, T], F32)
        self.cT = sb("cT_sb", [128, 8, TC], F32)
        self.cst_p = sb("cst_sb", [128, NCST - 768], F32)
        self.cstR = sb("cstR_sb", [128, 768], F32R)
        self.pvec = sb("pvec_sb", [128, NV], F32)
        self.modT = sb("modT_sb", [128, 48, 2], F32)
        self.mA = sb("mA_sb", [128, 6, 8, 2], F32)
        self.small = sb("small_sb", [128, 64], F32)
        AW = 29328
        self.arena_t = sb("arena_sb", [128, AW], F32)
        self.A = Arena(self.arena_t, AW)
        self.arenaR_t = sb("arenaR_sb", [128, 3840], F32R)
        self.AR = Arena(self.arenaR_t, 3840)
        self.psall = st.enter_context(nc.psum_tensor("psall", [128, 4096], F32))
        self.PS = [self.psall[:, i * 512:(i + 1) * 512] for i in range(8)]
        self.pspools = {"a": [0, 1], "b": [2, 3], "c": [4, 5], "d": [6, 7], "sc": [0, 6, 1, 7]}
        self.s_in = None
        self.s_out = None
        self.s_w = [None]
        self.outkeys = []
        self.wslot_rr = 0

        P.dma("sync", self.xT[:], self.I["xT"].rearrange("(c p) t -> p c t", p=128), writes=["xT"], slot=self.s_in)
        P.dma("sync", self.cT[:], self.I["cT"].rearrange("(c p) t -> p c t", p=128), writes=["cT"], slot=self.s_in)
        P.dma("sync", self.cst_p[:], self.I["cst"][:, 768:NCST], writes=["cst"], slot=self.s_in)
        cst0 = self.A.alloc([768], F32)
        P.dma("sync", cst0, self.I["cst"][:, 0:768], writes=["cst0"], slot=self.s_in)
        self.cp("vector", self.cstR[:], cst0, ["cst0"], ["cstR"])
        P.barrier()
        self.A.reset(0)

        class _CstView:
            def __getitem__(_s, key):
                ps_, cs_ = key
                assert cs_.start >= 768, cs_
                return self.cst_p[ps_, cs_.start - 768:cs_.stop - 768]
        self.cst = _CstView()
        self.R_perm = self.cstR[:, C_PERM:C_PERM + 128]
        self.R_bd32 = self.cstR[:, C_BD32:C_BD32 + 128]
        self.R_bd64 = self.cstR[:, C_BD64:C_BD64 + 128]
        self.R_o1024 = self.cstR[:, C_O1024:C_O1024 + 128]
        self.R_o256 = self.cstR[:, C_O256:C_O256 + 128]
        self.R_ident = self.cstR[:, C_IDENT:C_IDENT + 128]

        for li, l in enumerate(self.layers):
            self.layer(l, last=(l == 1))
        for c in range(8):
            P.dma("sync", self.out[c * 128:(c + 1) * 128, :], self.xT[:, c, :], reads=["xT"], writes=[f"out{c}"], slot=self.s_out)
            self.outkeys.append(f"out{c}")
        P.wait_all("sync", self.outkeys)
        P.emit(st)
        return nc

    def wslot(self):
        s = self.s_w[self.wslot_rr % len(self.s_w)]
        self.wslot_rr += 1
        return s

    def modulation(self, l):
        P, A = self.P, self.A
        m0 = A.mark()
        cv = A.alloc([8, 2], F32)
        cvb = A.alloc([8, 2], BF16)
        sg = A.alloc([8, 2], F32)
        P.dma("sync", cv, self.I["cvec"], writes=["cv"], slot=self.s_in)
        P.dma("sync", self.pvec[:], self.I[f"pvec{l}"], writes=["pvec"], slot=self.s_in)
        self.act(sg, cv, AF.Sigmoid, ["cv"], ["sg"])
        self.tt("vector", cvb, cv, sg, ALU.mult, ["cv", "sg"], ["cvb"])
        wm = [A.alloc([8, 1024], BF16) for _ in range(2)]
        pst, pk = self.ps("d")
        wv = self.I[f"wmod{l}"].rearrange("(c p) m -> p c m", p=128)
        for j in range(6):
            P.dma("gpsimd", wm[j % 2], wv[:, :, j * 1024:(j + 1) * 1024], writes=[("wm", j % 2)], slot=self.wslot())
            for jj in range(8):
                col = j * 8 + jj
                for c in range(8):
                    self.mm(pst[:, 2 * col:2 * col + 2], wm[j % 2][:, c, jj * 128:(jj + 1) * 128], cvb[:, c, :],
                            c == 0, c == 7, [("wm", j % 2), "cvb"], [pk])
        self.tt("vector", self.modT[:], pst[:, 0:96].rearrange("p (j n) -> p j n", n=2),
                self.pvec[:, PV_BMOD:PV_BMOD + 48].unsqueeze(2).broadcast_to([128, 48, 2]), ALU.add,
                [pk, "pvec"], ["modT"])
        md = lambda i: self.modT[:, i * 8:(i + 1) * 8, :]
        for (dst, sc_i, g_off) in ((0, 1, PV_G1), (3, 4, PV_G2)):
            self.stt("vector", self.mA[:, dst], md(sc_i), 1.0,
                     self.pvec[:, g_off:g_off + 8].unsqueeze(2).broadcast_to([128, 8, 2]), ALU.add, ALU.mult,
                     ["modT", "pvec"], ["mA"])
        for (dst, src) in ((1, 0), (2, 2), (4, 3), (5, 5)):
            self.cp("vector", self.mA[:, dst], md(src), ["modT", "mA"], ["mA"])
        A.reset(m0)

    def norm_mod(self, seq, which, out_bf, okey, out_f32r=None, fkey=None, cb=None, local=False, bs=512):
        P, A = self.P, self.A
        ia, ish = (0, 1) if which == 1 else (3, 4)
        m0 = A.mark()
        mr0 = self.AR.mark()
        nsq = 2 if self.AR.n - self.AR.off >= 1024 else 1
        sqs = [self.AR.alloc([512], F32R) for _ in range(nsq)]
        rs = A.alloc([512], F32)
        tmps = [A.alloc([512], F32) for _ in range(2)]
        rk = seq.name + "T"
        nb_ = min(bs, seq.Tn)
        for (t0, n) in [(t, nb_) for t in range(0, seq.Tn, nb_)]:
            pst, pk = self.ps("c")
            for c in range(8):
                sq = sqs[c % nsq]
                sk = ("nm_sq", c % nsq)
                self.act(sq[:, 0:n], seq.res[:, c, t0:t0 + n], AF.Square, [rk], [sk])
                self.mm(pst[:, 0:n], self.R_o1024, sq[:, 0:n], c == 0, c == 7, [sk, "cstR"], [pk])
            self.act(rs[:, 0:n], pst[:, 0:n], AF.Ln, [pk], ["nm_rs"], bias=EPS)
            self.act(rs[:, 0:n], rs[:, 0:n], AF.Exp, ["nm_rs"], ["nm_rs"], scale=-0.5)
            for c in range(8):
                tmp = tmps[c % 2]
                tk = ("nm_tmp", c % 2)
                self.stt("vector", tmp[:, 0:n], seq.res[:, c, t0:t0 + n], self.mA[:, ia, c, seq.mcol:seq.mcol + 1],
                         rs[:, 0:n], ALU.mult, ALU.mult, [rk, "mA", "nm_rs"], [tk])
                if out_f32r is not None:
                    self.ts("vector", out_f32r[:, c, 0:n], tmp[:, 0:n], self.mA[:, ish, c, seq.mcol:seq.mcol + 1], None,
                            ALU.add, None, [tk, "mA"], [(fkey, c)])
                    ob = out_bf[:, c, 0:n] if local else out_bf[:, c, t0:t0 + n]
                    self.cp("scalar", ob, out_f32r[:, c, 0:n].bitcast(F32), [(fkey, c)], [(okey, c)])
                else:
                    self.ts("vector", out_bf[:, c, t0:t0 + n], tmp[:, 0:n], self.mA[:, ish, c, seq.mcol:seq.mcol + 1], None,
                            ALU.add, None, [tk, "mA"], [okey])
            if cb is not None:
                cb(t0, n)
        A.reset(m0)
        self.AR.reset(mr0)

    def load_w(self, dst, src_view, key):
        self.P.dma("gpsimd", dst, src_view, writes=[key], slot=self.wslot())

    def proj_fm(self, pst, pk, seq, wt, wkey, col0, M, t0, n, hkey):
        for c in range(8):
            self.mm(pst[0:M, 0:n], wt[:, c, col0:col0 + M], seq.h[:, c, t0:t0 + n], c == 0, c == 7, [wkey, hkey], [pk])

    def proj_tm(self, pst, pk, seq, wt, wkey, col0, ncols, tc, hkey):
        for c in range(8):
            self.mm(pst[:, 0:ncols], seq.h[:, c, tc * 128:(tc + 1) * 128], wt[:, c, col0:col0 + ncols], c == 0, c == 7,
                    [wkey, hkey], [pk])

    def wout_partial(self, l, seq, mixT, mkey, grp):
        A = self.A
        m0 = A.mark()
        wo = A.alloc([2, 1024], BF16)
        self.load_w(wo, self.I[f"w_out{l}"][grp * 256:(grp + 1) * 256, :].rearrange("(k p) d -> p k d", p=128), "wo")
        rk = seq.name + "T"
        for (t0, n) in seq.blocks:
            for dc in range(8):
                pst, pk = self.ps("d")
                for k in range(2):
                    self.mm(pst[:, 0:n], wo[:, k, dc * 128:(dc + 1) * 128], mixT[:, k, t0:t0 + n], k == 0, k == 1,
                            ["wo", mkey], [pk])
                self.stt("vector", seq.res[:, dc, t0:t0 + n], pst[:, 0:n], self.mA[:, 2, dc, seq.mcol:seq.mcol + 1],
                         seq.res[:, dc, t0:t0 + n], ALU.mult, ALU.add, [pk, "mA", rk], [rk])
        A.reset(m0)

    def qk_norm(self, pst, pk, n, gcol, rope_t0, out_bf, okey, Ws):
        wi_ = self._qkrr = getattr(self, "_qkrr", 0) + 1
        wi_ %= len(Ws)
        sq, rs, qn, t1, rc, rsn = Ws[wi_]
        self.act(sq[:, 0:n], pst[:, 0:n], AF.Square, [pk], [("qk_sq", wi_)])
        ps2, pk2 = self.ps("c")
        self.mm(ps2[:, 0:n], self.R_bd32, sq[:, 0:n], True, True, [("qk_sq", wi_), "cstR"], [pk2])
        self.act(rs[:, 0:n], ps2[:, 0:n], AF.Ln, [pk2], [("qk_rs", wi_)], bias=EPS)
        self.act(rs[:, 0:n], rs[:, 0:n], AF.Exp, [("qk_rs", wi_)], [("qk_rs", wi_)], scale=-0.5)
        if rope_t0 is None:
            self.stt("vector", out_bf, pst[:, 0:n], self.pvec[:, gcol:gcol + 1], rs[:, 0:n], ALU.mult, ALU.mult,
                     [pk, "pvec", ("qk_rs", wi_)], [okey])
            return
        self.stt("vector", qn[:, 0:n], pst[:, 0:n], self.pvec[:, gcol:gcol + 1], rs[:, 0:n], ALU.mult, ALU.mult,
                 [pk, "pvec", ("qk_rs", wi_)], [("qk_qn", wi_)])
        self.P.dma("sync", rc[:, 0:n], self.I["rope_c"][:, rope_t0:rope_t0 + n], writes=[("rope_c", wi_)], slot=self.s_in)
        self.P.dma("sync", rsn[:, 0:n], self.I["rope_s"][:, rope_t0:rope_t0 + n], writes=[("rope_s", wi_)], slot=self.s_in)
        ps3, pk3 = self.ps("c")
        self.mm(ps3[:, 0:n], self.R_perm, qn[:, 0:n], True, True, [("qk_qn", wi_), "cstR"], [pk3])
        self.tt("vector", t1[:, 0:n], qn[:, 0:n].bitcast(F32), rc[:, 0:n], ALU.mult, [("qk_qn", wi_), ("rope_c", wi_)], [("qk_t1", wi_)])
        self.tt("vector", rs[:, 0:n], ps3[:, 0:n], rsn[:, 0:n], ALU.mult, [pk3, ("rope_s", wi_), ("qk_rs", wi_)], [("qk_rs", wi_)])
        self.tt("vector", out_bf, t1[:, 0:n], rs[:, 0:n], ALU.add, [("qk_t1", wi_), ("qk_rs", wi_)], [okey])

    def attention(self, l, seqs, last):
        P, A = self.P, self.A
        X, C = self.X, self.C
        m0 = A.mark()
        lam_init = 0.8 - 0.6 * math.exp(-0.3 * l)
        mr0 = self.AR.mark()
        kT = A.alloc([2, T + TC], BF16)
        qTs = {"x": A.alloc([2, T], BF16), "c": A.alloc([2, TC], BF16)}
        vaug = A.alloc([18, 4, 128], BF16)
        lamv = A.alloc([128], F32)
        lt = A.alloc([64], F32)
        mq = A.mark()
        wq = A.alloc([8, 768], BF16)
        self.load_w(wq, self.I[f"w_in{l}"][:, 0:768].rearrange("(c p) m -> p c m", p=128), "wq")
        W = []
        for _ in range(2):
            Wf = [A.alloc([512], F32) for _ in range(4)]
            W.append([self.AR.alloc([512], F32R), Wf[0], self.AR.alloc([512], F32R), Wf[1], Wf[2], Wf[3]])
        self.memset("gpsimd", vaug, 1.0, ["vaug"])
        P.dma("sync", lamv, self.I[f"rowv{l}"][512:640].partition_broadcast(128), writes=["lamv"], slot=self.s_in)
        self.tt("vector", lt[:, 0:32], lamv[:, 0:32], lamv[:, 32:64], ALU.mult, ["lamv"], ["lt"])
        self.tt("vector", lt[:, 32:64], lamv[:, 64:96], lamv[:, 96:128], ALU.mult, ["lamv", "lt"], ["lt"])
        sm = self.small
        P.op("vector", lambda e: e.reduce_sum(out=sm[:, 0:2], in_=lt.rearrange("p (a b) -> p a b", a=2),
                                              axis=mybir.AxisListType.X), ["lt"], ["sm_lam"])
        self.act(sm[:, 2:4], sm[:, 0:2], AF.Exp, ["sm_lam"], ["sm_lam2"])
        self.stt("vector", sm[:, 4:5], sm[:, 3:4], -lam_init, sm[:, 2:3], ALU.add, ALU.subtract, ["sm_lam2"], ["nlam"])
        self.ts("vector", sm[:, 5:6], self.pvec[:, PV_GAO:PV_GAO + 1], 1.0 - lam_init, None, ALU.mult, None, ["pvec"], ["gao2"])
        nlam = sm[:, 4:5]
        gao2 = sm[:, 5:6]
        for seq in (C, X):
            hk = seq.name + "h"
            for (t0, n) in seq.blocks:
                for kc in range(2):
                    pst, pk = self.ps("a")
                    self.proj_fm(pst, pk, seq, wq, "wq", 256 + kc * 128, 128, t0, n, hk)
                    for _d in range(2):
                        self.mm(self.PS[7][:, 0:n], wq[:, 0, 0:128], seq.h[:, 0, t0:t0 + n], True, True, ["wq", hk], [("ps", 7)])
                    self.qk_norm(pst, pk, n, PV_GK, t0 if seq.rope else None,
                                 kT[:, kc, seq.off + t0:seq.off + t0 + n], "kT", W)
            for tc in range(seq.nch):
                pst, pk = self.ps("b")
                self.proj_tm(pst, pk, seq, wq, "wq", 512, 256, tc, hk)
                kcg = seq.off // 128 + tc
                pv = pst[:, 0:256].rearrange("p (a b c) -> p a b c", a=2, b=2)
                va = vaug[:, kcg].rearrange("p (a b) c -> p a b c", a=2)
                self.cp("vector", va[:, :, 0, 0:64], pv[:, :, 0, :], [pk], ["vaug"])
                self.cp("scalar", va[:, :, 1, 64:128], pv[:, :, 1, :], [pk], ["vaug"])
        for seq in seqs:
            hk = seq.name + "h"
            for (t0, n) in seq.blocks:
                for qc in range(2):
                    pst, pk = self.ps("a")
                    self.proj_fm(pst, pk, seq, wq, "wq", qc * 128, 128, t0, n, hk)
                    for _d in range(2):
                        self.mm(self.PS[7][:, 0:n], wq[:, 0, 0:128], seq.h[:, 0, t0:t0 + n], True, True, ["wq", hk], [("ps", 7)])
                    self.qk_norm(pst, pk, n, PV_GQ, t0 if seq.rope else None, qTs[seq.name][:, qc, t0:t0 + n], "qT" + seq.name, W)
        P.barrier()
        A.reset(mq)
        O1r = W[0][0]
        O1 = A.alloc([512], F32)
        R = [A.alloc([512], F32) for _ in range(2)]
        Tm = [A.alloc([512], F32) for _ in range(2)]
        ocat = A.alloc([512], F32)
        pTT = [A.alloc([2, 512], BF16) for _ in range(2)]
        mixT = self.mixT
        sc = 32 ** -0.5
        for seq in seqs:
            hk = seq.name + "h"
            mk = "mix" + seq.name
            kchunks = list(range(0, 2)) + (list(range(2, 18)) if seq is X else [])
            qT = qTs[seq.name]
            qk_ = "qT" + seq.name
            for (t0, n) in seq.blocks:
                for qc in range(2):
                    for hh in range(2):
                        h = qc * 2 + hh
                        nb, db = hh * 64, 64 - hh * 64
                        obanks = [self.ps("b"), self.ps("b")]
                        pend = None
                        for i, kc in enumerate(kchunks):
                            b0 = 0 if i % 2 == 0 else 6
                            for s_ in range(2):
                                base = hh * 64 + s_ * 32
                                self.mm(self.PS[b0 + s_][:, 0:n], kT[base:base + 32, qc, kc * 128:(kc + 1) * 128],
                                        qT[base:base + 32, qc, t0:t0 + n], True, True, ["kT", qk_], [("ps", b0 + s_)], tp=(base, 0))
                            if pend is not None:
                                for s_ in range(2):
                                    po, pok = obanks[s_]
                                    pe_ = pend[s_]
                                    self.mm(po[:, 0:n], pe_[0], pe_[1], pe_[2], False, ["vaug", pe_[3]], [pok])
                            for _d in range(WARM_DUMMY):
                                self.mm(self.PS[4][:, 0:n], kT[:, qc, kc * 128:(kc + 1) * 128], qT[:, qc, t0:t0 + n], True, True,
                                        ["kT", qk_], [("ps", 4)])
                            pt2 = pTT[i % 2]
                            ptk = ("pTT", i % 2)
                            src = self.psall[:, b0 * 512:(b0 + 2) * 512].rearrange("p (s n) -> p s n", s=2)[:, :, 0:n]
                            self.act(pt2[:, :, 0:n], src, AF.Exp, [("ps", b0), ("ps", b0 + 1)], [ptk], scale=sc)
                            pend = [(vaug[:, kc, h, :], pt2[:, s_, 0:n], i == 0, ptk) for s_ in range(2)]
                        for s_ in range(2):
                            po, pok = obanks[s_]
                            pe_ = pend[s_]
                            self.mm(po[:, 0:n], pe_[0], pe_[1], pe_[2], True, ["vaug", pe_[3]], [pok])
                        for s_ in range(2):
                            po, pok = obanks[s_]
                            self.act(Tm[s_][db:db + 64, 0:n], po[db:db + 64, 0:n], AF.Ln, [pok], [("Tm", s_)])
                            self.act(R[s_][nb:nb + 64, 0:n], Tm[s_][db:db + 64, 0:n], AF.Exp, [("Tm", s_)], [("R", s_)], scale=-1.0)
                            self.tt("vector", Tm[s_][nb:nb + 64, 0:n], po[nb:nb + 64, 0:n], R[s_][nb:nb + 64, 0:n], ALU.mult,
                                    [pok, ("R", s_), ("Tm", s_)], [("Tm", s_)])
                        self.stt("vector", ocat[nb:nb + 64, 0:n], Tm[1][nb:nb + 64, 0:n], nlam[nb:nb + 64, :],
                                 Tm[0][nb:nb + 64, 0:n], ALU.mult, ALU.add, [("Tm", 0), ("Tm", 1), "nlam"], ["ocat"])
                    self.act(O1r[:, 0:n], ocat[:, 0:n], AF.Square, ["ocat"], ["O1r"])
                    ps2, pk2 = self.ps("c")
                    self.mm(ps2[:, 0:n], self.R_bd64, O1r[:, 0:n], True, True, ["O1r", "cstR"], [pk2])
                    self.act(O1[:, 0:n], ps2[:, 0:n], AF.Ln, [pk2], ["O1"], bias=EPS)
                    self.act(O1[:, 0:n], O1[:, 0:n], AF.Exp, ["O1"], ["O1"], scale=-0.5)
                    self.stt("vector", mixT[:, qc, t0:t0 + n], ocat[:, 0:n], gao2, O1[:, 0:n], ALU.mult, ALU.mult,
                             ["ocat", "gao2", "O1"], [mk])
            self.dump(f"attn_{seq.name}{l}", mixT[:, :, 0:seq.Tn], [mk], [128, 2, seq.Tn])
            self.wout_partial(l, seq, mixT, mk, 0)
        A.reset(m0)
        self.AR.reset(mr0)

    def pool_mixer(self, l, seq):
        P, A = self.P, self.A
        m0 = A.mark()
        Tn = seq.Tn
        Wd = Tn + 16
        hk = seq.name + "h"
        mk = "mix" + seq.name
        wp = A.alloc([8, 256], BF16)
        self.load_w(wp, self.I[f"w_in{l}"][:, 768:1024].rearrange("(c p) m -> p c m", p=128), "wp")
        wbd = A.alloc([2, 128], BF16)
        self.memset("gpsimd", wbd, 0.0, ["wbd"])
        for g in range(4):
            c, hf = g // 2, g % 2
            P.dma("gpsimd", wbd[hf * 64:(hf + 1) * 64, c, hf * 64:(hf + 1) * 64], self.I[f"wpool{l}"][g],
                  reads=["wbd"], writes=["wbd"], slot=self.wslot())
        zps = [A.alloc([Wd], F32) for _ in range(2)]
        Ab1 = A.alloc([Wd], F32)
        Abs_ = [Ab1, Ab1]
        Bb = A.alloc([Wd], F32)
        Cb = A.alloc([Wd], F32)
        Sb1 = A.alloc([Tn], F32)
        Sbs = [Sb1, Sb1]
        dTs = [A.alloc([Tn], BF16) for _ in range(2)]
        mixT = self.mixT
        G = "vector"
        for c in range(2):
            zp, Ab, Sb, dT = zps[c], Abs_[c], Sbs[c], dTs[c]
            self.memset(G, zp[:, 0:8], 0.0, [("zp", c)])
            self.memset(G, zp[:, 8 + Tn:Wd], 0.0, [("zp", c)])
            for (t0, n) in seq.blocks:
                pst, pk = self.ps("a")
                self.proj_fm(pst, pk, seq, wp, "wp", c * 128, 128, t0, n, hk)
                self.cp("scalar", zp[:, 8 + t0:8 + t0 + n], pst[:, 0:n], [pk], [("zp", c)])
            lo, hi = slice(0, 64), slice(64, 128)
            if c == 0:
                self.tt(G, Sb[lo, :], zp[lo, 7:7 + Tn], zp[lo, 8:8 + Tn], ALU.add, [("zp", c)], ["Sb"])
                self.tt(G, Ab[hi, 0:Wd - 1], zp[hi, 0:Wd - 1], zp[hi, 1:Wd], ALU.add, [("zp", c)], ["Ab"])
                self.tt(G, Sb[hi, :], Ab[hi, 6:6 + Tn], Ab[hi, 8:8 + Tn], ALU.add, ["Ab", "Sb"], ["Sb"])
            else:
                self.tt(G, Ab[:, 0:Wd - 1], zp[:, 0:Wd - 1], zp[:, 1:Wd], ALU.add, [("zp", c)], ["Ab"])
                self.tt(G, Bb[:, 0:Wd - 3], Ab[:, 0:Wd - 3], Ab[:, 2:Wd - 1], ALU.add, ["Ab"], ["Bb"])
                self.tt(G, Sb[lo, :], Bb[lo, 4:4 + Tn], Bb[lo, 8:8 + Tn], ALU.add, ["Bb"], ["Sb"])
                self.tt(G, Cb[hi, 0:Wd - 7], Bb[hi, 0:Wd - 7], Bb[hi, 4:Wd - 3], ALU.add, ["Bb"], ["Cb"])
                self.tt(G, Sb[hi, :], Cb[hi, 0:Tn], Cb[hi, 8:8 + Tn], ALU.add, ["Cb", "Sb"], ["Sb"])
            self.stt("vector", dT[:, :], Sb[:, :], self.cst[:, C_PRW + c:C_PRW + c + 1], zp[:, 8:8 + Tn], ALU.mult, ALU.subtract,
                     ["Sb", "cst", ("zp", c)], [("dT", c)])
            for side, (a0, f0) in enumerate(((0, 0), (Tn - 8, 8))):
                fx = self.cst[:, C_PFIX + c * 16 + f0:C_PFIX + c * 16 + f0 + 8]
                self.tt("vector", Sb[:, a0:a0 + 8], Sb[:, a0:a0 + 8], fx, ALU.mult, ["Sb", "cst", ("dT", c)], ["Sb"])
                self.tt("vector", dT[:, a0:a0 + 8], Sb[:, a0:a0 + 8], zp[:, 8 + a0:8 + a0 + 8], ALU.subtract, ["Sb", ("zp", c)], [("dT", c)])
            for (t0, n) in seq.blocks:
                pst, pk = self.ps("b")
                self.mm(pst[:, 0:n], wbd[:, c, :], dT[:, t0:t0 + n], True, True, ["wbd", ("dT", c)], [pk])
                self.ts("vector", mixT[:, c, t0:t0 + n], pst[:, 0:n], self.pvec[:, PV_BPOOL + c:PV_BPOOL + c + 1],
                        self.pvec[:, PV_PSC + c:PV_PSC + c + 1], ALU.add, ALU.mult, [pk, "pvec"], [mk])
        self.dump(f"pool_{seq.name}{l}", mixT[:, :, 0:Tn], [mk], [128, 2, Tn])
        self.wout_partial(l, seq, mixT, mk, 1)
        A.reset(m0)

    def conv_mixer(self, l, seq):
        P, A = self.P, self.A
        m0 = A.mark()
        Tn = seq.Tn
        hk = seq.name + "h"
        mk = "mix" + seq.name
        wc = A.alloc([8, 512], BF16)
        self.load_w(wc, self.I[f"w_in{l}"][:, 1024:1536].rearrange("(c p) m -> p c m", p=128), "wc")
        wpw = A.alloc([2, 256], BF16)
        self.load_w(wpw, self.I[f"wpw2{l}"].rearrange("(c p) m -> p c m", p=128), "wpw")
        yp = A.alloc([2, Tn + 30], BF16)
        dg = A.alloc([2, 31, 128], BF16)
        sgt = A.alloc([512], F32)
        mr0 = self.AR.mark()
        sq = self.AR.alloc([512], F32R)
        accR = self.AR.alloc([2, 512], F32R)
        m2 = A.alloc([512], F32)
        var = A.alloc([512], F32)
        tt_ = A.alloc([512], F32)
        actT = A.alloc([2, 512], BF16)
        mixT = self.mixT
        identF = self.R_ident.bitcast(F32)
        for c in range(2):
            for k in range(31):
                self.ts("vector", dg[:, c, k, :], identF, self.pvec[:, PV_CONVW + c * 31 + k:PV_CONVW + c * 31 + k + 1], None,
                        ALU.mult, None, ["cstR", "pvec"], [("dg", c, k)])
        for c in range(2):
            self.memset("gpsimd", yp[:, c, 0:15], 0.0, [("yp", c)])
            self.memset("gpsimd", yp[:, c, 15 + Tn:30 + Tn], 0.0, [("yp", c)])
            for (t0, n) in seq.blocks:
                pv, pvk = self.ps("a")
                self.proj_fm(pv, pvk, seq, wc, "wc", c * 128, 128, t0, n, hk)
                pg, pgk = self.ps("b")
                self.proj_fm(pg, pgk, seq, wc, "wc", 256 + c * 128, 128, t0, n, hk)
                self.act(sgt[:, 0:n], pg[:, 0:n], AF.Sigmoid, [pgk], ["sgt"])
                self.tt("vector", yp[:, c, 15 + t0:15 + t0 + n], pv[:, 0:n], sgt[:, 0:n], ALU.mult, [pvk, "sgt"], [("yp", c)])
        for (t0, n) in seq.blocks:
            pm, pmk = self.ps("c")
            for c in range(2):
                pc_, pck = self.ps("d")
                for k in range(31):
                    self.mm(pc_[:, 0:n], dg[:, c, k, :], yp[:, c, t0 + k:t0 + k + n], k == 0, k == 30, [("dg", c, k), ("yp", c)], [pck])
                self.ts("vector", accR[:, c, 0:n], pc_[:, 0:n], self.pvec[:, PV_CONVB + c:PV_CONVB + c + 1], None, ALU.add, None,
                        [pck, "pvec"], [("accR", c)])
                self.mm(pm[:, 0:n], self.R_o256, accR[:, c, 0:n], c == 0, c == 1, [("accR", c), "cstR"], [pmk])
            pq, pqk = self.ps("c")
            for c in range(2):
                self.act(sq[:, 0:n], accR[:, c, 0:n].bitcast(F32), AF.Square, [("accR", c)], ["cv_sq"])
                self.mm(pq[:, 0:n], self.R_o256, sq[:, 0:n], c == 0, c == 1, ["cv_sq", "cstR"], [pqk])
            self.act(m2[:, 0:n], pm[:, 0:n], AF.Square, [pmk], ["cv_m2"])
            self.tt("vector", var[:, 0:n], pq[:, 0:n], m2[:, 0:n], ALU.subtract, [pqk, "cv_m2"], ["cv_var"])
            self.act(var[:, 0:n], var[:, 0:n], AF.Ln, ["cv_var"], ["cv_var"], bias=EPS)
            self.act(var[:, 0:n], var[:, 0:n], AF.Exp, ["cv_var"], ["cv_var"], scale=-0.5)
            for c in range(2):
                self.tt("vector", tt_[:, 0:n], accR[:, c, 0:n].bitcast(F32), pm[:, 0:n], ALU.subtract, [("accR", c), pmk], ["cv_t"])
                self.tt("vector", tt_[:, 0:n], tt_[:, 0:n], var[:, 0:n], ALU.mult, ["cv_t", "cv_var"], ["cv_t"])
                self.act(actT[:, c, 0:n], tt_[:, 0:n], AF.Silu, ["cv_t", "pvec"], [("cv_act", c)],
                         bias=self.pvec[:, PV_CLNB + c:PV_CLNB + c + 1], scale=self.pvec[:, PV_CLNG + c:PV_CLNG + c + 1])
            for co in range(2):
                po, pok = self.ps("b")
                for ci in range(2):
                    self.mm(po[:, 0:n], wpw[:, ci, co * 128:(co + 1) * 128], actT[:, ci, 0:n], ci == 0, ci == 1,
                            ["wpw", ("cv_act", ci)], [pok])
                self.cp("scalar", mixT[:, co, t0:t0 + n], po[:, 0:n], [pok], [mk])
        self.dump(f"conv_{seq.name}{l}", mixT[:, :, 0:Tn], [mk], [128, 2, Tn])
        self.wout_partial(l, seq, mixT, mk, 2)
        A.reset(m0)
        self.AR.reset(mr0)

    def sgu_mixer(self, l, seq):
        P, A = self.P, self.A
        m0 = A.mark()
        Tn = seq.Tn
        hk = seq.name + "h"
        mk = "mix" + seq.name
        ws = A.alloc([8, 512], BF16)
        self.load_w(ws, self.I[f"w_in{l}"][:, 1536:2048].rearrange("(c p) m -> p c m", p=128), "ws")
        wsp = A.alloc([4, 128], BF16)
        self.load_w(wsp, self.I[f"wspT{l}"], "wsp")
        bsp = A.alloc([2, 128], F32)
        P.dma("sync", bsp, self.I[f"bsp{l}"], writes=["bsp"], slot=self.s_in)
        lnr = A.alloc([512], F32)
        P.dma("sync", lnr, self.I[f"rowv{l}"][0:512].partition_broadcast(128), writes=["lnr"], slot=self.s_in)
        uT = A.alloc([2, Tn], F32)
        vgs = [A.alloc([256], F32) for _ in range(2)]
        vts = [A.alloc([256], F32) for _ in range(2)]
        st6s = [A.alloc([8], F32) for _ in range(2)]
        VA = [A.alloc([256], BF16) for _ in range(2)]
        VB = [A.alloc([256], BF16) for _ in range(2)]
        tmp = A.alloc([512], F32)
        mixT = self.mixT
        for i in range(2):
            self.memset("gpsimd", VA[i], 0.0, [("VA", i)])
            self.memset("gpsimd", VB[i], 0.0, [("VB", i)])
        for c in range(2):
            for (t0, n) in seq.blocks:
                pst, pk = self.ps("a")
                self.proj_fm(pst, pk, seq, ws, "ws", c * 128, 128, t0, n, hk)
                self.act(uT[:, c, t0:t0 + n], pst[:, 0:n], AF.Gelu_apprx_tanh, [pk], ["uT"])
        nblk = len(seq.blocks)
        per = seq.blocks[0][1] // 128
        for bi, (t0, n) in enumerate(seq.blocks):
            pcc = [self.ps("d"), self.ps("d")]
            for j in range(per):
                tc = t0 // 128 + j
                i = tc % 2
                pst, pk = self.ps("b")
                self.proj_tm(pst, pk, seq, ws, "ws", 256, 256, tc, hk)
                vg, vt, st6 = vgs[i], vts[i], st6s[i]
                kvg, kvt, kst = ("vg", i), ("vt", i), ("st", i)
                self.act(vg, pst[:, 0:256], AF.Gelu_apprx_tanh, [pk], [kvg])
                P.op("vector", lambda e, st6=st6, vg=vg: e.bn_stats(out=st6[:, 0:6], in_=vg), [kvg], [kst])
                P.op("vector", lambda e, st6=st6: e.bn_aggr(out=st6[:, 6:8], in_=st6[:, 0:6]), [kst], [kst])
                self.act(st6[:, 7:8], st6[:, 7:8], AF.Ln, [kst], [kst], bias=EPS)
                self.act(st6[:, 7:8], st6[:, 7:8], AF.Exp, [kst], [kst], scale=-0.5)
                self.ts("vector", vt, vg, st6[:, 6:7], st6[:, 7:8], ALU.subtract, ALU.mult, [kvg, kst], [kvt])
                self.tt("vector", vt, vt, lnr[:, 0:256], ALU.mult, [kvt, "lnr"], [kvt])
                v3 = vt.rearrange("p (a b) -> p a b", a=2)
                b3 = lnr[:, 256:512].rearrange("p (a b) -> p a b", a=2)
                self.tt("vector", VA[i].rearrange("p (a b) -> p a b", a=2)[:, :, 0:64], v3[:, :, 0:64], b3[:, :, 0:64], ALU.add,
                        [kvt, "lnr"], [("VA", i)])
                self.tt("vector", VB[i].rearrange("p (a b) -> p a b", a=2)[:, :, 64:128], v3[:, :, 64:128], b3[:, :, 64:128], ALU.add,
                        [kvt, "lnr"], [("VB", i)])
                for cc in range(2):
                    po, pok = pcc[cc]
                    self.mm(po[:, j * 128:(j + 1) * 128], VA[i][:, cc * 128:(cc + 1) * 128], wsp[:, 2 * cc, :], True, False,
                            [("VA", i), "wsp"], [pok])
                    self.mm(po[:, j * 128:(j + 1) * 128], VB[i][:, cc * 128:(cc + 1) * 128], wsp[:, 2 * cc + 1, :], False, True,
                            [("VB", i), "wsp"], [pok])
            for cc in range(2):
                po, pok = pcc[cc]
                self.tt("vector", tmp[:, 0:n].rearrange("p (a b) -> p a b", b=128), po[:, 0:n].rearrange("p (a b) -> p a b", b=128),
                        bsp[:, cc, :].unsqueeze(1).broadcast_to([128, per, 128]), ALU.add, [pok, "bsp"], ["sg_tmp"])
                self.tt("vector", mixT[:, cc, t0:t0 + n], tmp[:, 0:n], uT[:, cc, t0:t0 + n], ALU.mult, ["sg_tmp", "uT"], [mk])
        self.dump(f"sgu_{seq.name}{l}", mixT[:, :, 0:Tn], [mk], [128, 2, Tn])
        self.wout_partial(l, seq, mixT, mk, 3)
        A.reset(m0)

    def moe(self, l, li, seqs):
        P, A = self.P, self.A
        m0 = A.mark()
        GE = 2
        NJ = 256 + (32 if len(seqs) > 1 else 0)
        njc = 3 if len(seqs) > 1 else 2
        mr0 = self.AR.mark()
        wrR = self.AR.alloc([8 * 128], F32R)
        h2f = self.AR.alloc([8, 256], F32R)
        affEb = self.AR.alloc([256], F32R)
        h2tok = {}
        postok = {}
        for seq in seqs:
            h2tok[seq.name] = A.alloc([seq.nch, 1024], BF16)
            postok[seq.name] = A.alloc([seq.nch, NE], F32)
        m1 = A.mark()
        identB = A.alloc([128], BF16)
        self.cp("vector", identB, self.R_ident.bitcast(F32), ["cstR"], ["identB"])
        wr0 = A.alloc([8, NE], F32)
        P.dma("sync", wr0, self.I[f"wr{l}"], writes=["wr0"], slot=self.s_in)
        self.ts("vector", wrR, self.xT[:, 0, 0:1024], 0.0, None, ALU.mult, None, ["xT"], ["wrR"])
        for c in range(8):
            self.cp("vector", wrR[:, c * 128:c * 128 + NE], wr0[:, c, :], ["wr0", "wrR"], ["wrR"])
        m1b = A.mark()
        for seq in seqs:
            Tn = seq.Tn
            cap = 2 * Tn // NE
            sn = seq.name
            P.barrier()
            A.reset(m1b)
            hb = A.alloc([8, 256], BF16)
            affE = None
            aff = A.alloc([Tn], F32)
            work = A.alloc([Tn], F32)
            ones = A.alloc([Tn], F32)
            cs = A.alloc([Tn], F32)
            mask = A.alloc([Tn], F32)
            posb = A.alloc([Tn], BF16)
            gateb = A.alloc([Tn], BF16)
            mx = A.alloc([8], F32)

            def cb(t0, n, seq=seq, hb=hb, sn=sn, aff=aff, work=work):
                pl, plk = self.ps("a")
                for c in range(8):
                    self.mm(pl[:, 0:n], wrR[:, c * 128:(c + 1) * 128], h2f[:, c, 0:n], c == 0, c == 7,
                            ["wrR", ("h2f", c)], [plk])
                self.act(affEb[0:NE, 0:n], pl[0:NE, 0:n], AF.Exp, [plk], ["affEb"])
                pss, psk = self.ps("c")
                self.mm(pss[:, 0:n], self.R_o1024[0:NE, :], affEb[0:NE, 0:n], True, True, ["affEb", "cstR"], [psk])
                self.act(work[0:NE, t0:t0 + n], pss[0:NE, 0:n], AF.Ln, [psk], ["work"])
                self.act(work[0:NE, t0:t0 + n], work[0:NE, t0:t0 + n], AF.Exp, ["work"], ["work"], scale=-1.0)
                self.stt("vector", aff[0:NE, t0:t0 + n], affEb[0:NE, 0:n].bitcast(F32), 1.0 / 1024, work[0:NE, t0:t0 + n], ALU.mult, ALU.mult,
                         ["affEb", "work"], ["aff"])
                for j in range(n // 128):
                    tc = t0 // 128 + j
                    pt, ptk = self.ps("b")
                    ptb = pt.bitcast(BF16)
                    for dc in range(8):
                        P.op("tensor", lambda e, dc=dc, j=j, ptb=ptb: e.transpose(out=ptb[:, dc * 128:(dc + 1) * 128],
                                                                                 in_=hb[:, dc, j * 128:(j + 1) * 128], identity=identB),
                             [("hb", dc), "identB"], [ptk], is_mm=True)
                    self.cp("scalar", h2tok[sn][:, tc, :], ptb[:, 0:1024], [ptk], ["h2tok" + sn])

            self.norm_mod(seq, 2, hb, "hb", out_f32r=h2f, fkey="h2f", cb=cb, local=True, bs=256)
            self.dump(f"aff_{sn}{l}", aff[0:NE, :], ["aff"], [NE, Tn])
            lo_, mid_, cnt_, tmp_ = mx[0:NE, 0:1], mx[0:NE, 1:2], mx[0:NE, 2:3], mx[0:NE, 3:4]
            self.memset("vector", lo_, 0.0, ["bs_lo"])
            for k in range(26):
                wk = 0.5 ** (k + 1)
                self.ts("vector", mid_, lo_, wk, None, ALU.add, None, ["bs_lo"], ["bs_mid"])
                P.op("vector", lambda e, work=work, aff=aff, mid_=mid_, cnt_=cnt_: e.tensor_scalar(
                    out=work[0:NE, :], in0=aff[0:NE, :], scalar1=mid_, scalar2=0.0, op0=ALU.is_ge, op1=ALU.add, accum_out=cnt_),
                    ["aff", "bs_mid"], ["work", "bs_cnt"])
                self.ts("vector", tmp_, cnt_, cap - 0.5, wk, ALU.is_ge, ALU.mult, ["bs_cnt"], ["bs_tmp"])
                self.tt("vector", lo_, lo_, tmp_, ALU.add, ["bs_lo", "bs_tmp"], ["bs_lo"])
            self.ts("vector", mask[0:NE, :], aff[0:NE, :], lo_, None, ALU.is_ge, None, ["aff", "bs_lo"], ["mask"])
            self.memset("gpsimd", ones[0:NE, :], 1.0, ["ones"])
            P.op("vector", lambda e, cs=cs, ones=ones, mask=mask: e.tensor_tensor_scan(out=cs[0:NE, :], data0=ones[0:NE, :], data1=mask[0:NE, :], initial=0.0,
                                                          op0=ALU.mult, op1=ALU.add), ["ones", "mask"], ["cs"])
            self.tt("vector", cs[0:NE, :], cs[0:NE, :], mask[0:NE, :], ALU.mult, ["cs", "mask"], ["cs"])
            self.ts("vector", cs[0:NE, :], cs[0:NE, :], -1.0, None, ALU.add, None, ["cs"], ["cs"])
            self.cp("vector", posb[0:NE, :], cs[0:NE, :], ["cs"], ["posb"])
            self.tt("vector", gateb[0:NE, :], aff[0:NE, :], mask[0:NE, :], ALU.mult, ["aff", "mask"], ["gateb"])
            P.dma("sync", self.scr_pos[li, :, seq.off:seq.off + Tn], posb[0:NE, :], reads=["posb"], writes=["scr_pos"], slot=self.s_in)
            P.dma("sync", self.scr_gate[li, :, seq.off:seq.off + Tn], gateb[0:NE, :], reads=["gateb"], writes=["scr_gate"], slot=self.s_in)
            pt, ptk = self.ps("c")
            for tc in range(seq.nch):
                P.op("tensor", lambda e, tc=tc, pt=pt, cs=cs: e.transpose(out=pt[:, tc * NE:(tc + 1) * NE], in_=cs[0:NE, tc * 128:(tc + 1) * 128],
                                                                   identity=self.R_ident.bitcast(F32)[0:NE, 0:NE]),
                     ["cs", "cstR"], [ptk], is_mm=True)
            self.cp("vector", postok[sn], pt[:, 0:seq.nch * NE].rearrange("p (a b) -> p a b", b=NE), [ptk], ["postok" + sn])
        P.barrier()
        A.reset(m1)
        X = seqs[0]
        Cq = seqs[1] if len(seqs) > 1 else None
        S = A.alloc([16, 256], FP8)
        Sc = A.alloc([2, 32], FP8)
        xsT = A.alloc([8, NJ], BF16)
        actT = A.alloc([16, NJ], BF16)
        sa = A.alloc([NJ], F32)
        NR = 3
        wgp = [A.alloc([8, 256], BF16) for _ in range(NR)]
        wup = [A.alloc([8, 256], BF16) for _ in range(NR)]
        wdp = [A.alloc([16, 256], BF16) for _ in range(2)]
        ygrp = A.alloc([GE * 3, 1024], BF16)
        _pb = [A.alloc([256], BF16) for _ in range(GE)]
        _gb = [A.alloc([256], BF16) for _ in range(GE)]
        posB = [_pb, _pb]
        gateB = [_gb, _gb]
        STs = [A.alloc([GE * 2, 256], BF16) for _ in range(2)]
        iota = self.cst[:, C_IOTA:C_IOTA + 256]
        jidx = lambda jc: self.cst[:, C_JIDX + jc:C_JIDX + jc + 1]
        def scatter_gen(e):
            e0 = e - (GE - 1)
            blocks = [(seq, t0, 256) for seq in seqs for t0 in range(0, seq.Tn, 256)]

            def prep(bi):
                seq, t0, n = blocks[bi]
                par = bi % 2
                isx = seq is X
                for g in range(GE):
                    P.dma("sync", posB[par][g][:, 0:n], self.scr_pos[li, e0 + g, seq.off + t0:seq.off + t0 + n].partition_broadcast(128),
                          reads=["scr_pos"], writes=[("posB", g)], slot=self.s_in)
                    P.dma("sync", gateB[par][g][:, 0:n], self.scr_gate[li, e0 + g, seq.off + t0:seq.off + t0 + n].partition_broadcast(128),
                          reads=["scr_gate"], writes=[("gateB", g)], slot=self.s_in)
                    for jc in (range(2) if isx else (2,)):
                        si = g * 2 + (jc if isx else 0)
                        self.stt("vector", STs[par][:, si, 0:n], posB[par][g][:, 0:n], jidx(jc), gateB[par][g][:, 0:n], ALU.is_equal, ALU.mult,
                                 [("posB", g), ("gateB", g), "cst"], [("ST", par, si)])

            prep(0)
            for bi, (seq, t0, n) in enumerate(blocks):
                if bi + 1 < len(blocks):
                    prep(bi + 1)
                par = bi % 2
                ST = STs[par]
                isx = seq is X
                gt_i = 5
                rk = seq.name + "T"
                for dc in range(8):
                    po, pok = self.ps("sc")
                    terms = []
                    for g in range(GE):
                        if isx:
                            for jc in range(2):
                                terms.append((ygrp[:, g * 3 + jc, dc * 128:(dc + 1) * 128], ST[:, g * 2 + jc, 0:n], ("ST", par, g * 2 + jc)))
                        else:
                            terms.append((ygrp[0:32, g * 3 + 2, dc * 128:(dc + 1) * 128], ST[0:32, g * 2, 0:n], ("ST", par, g * 2)))
                    for ti, (lt_, rt_, sk) in enumerate(terms):
                        self.mm(po[:, 0:n], lt_, rt_, ti == 0, ti == len(terms) - 1, ["ygrp", sk], [pok])
                    self.stt("vector", seq.res[:, dc, t0:t0 + n], po[:, 0:n], self.mA[:, gt_i, dc, seq.mcol:seq.mcol + 1],
                             seq.res[:, dc, t0:t0 + n], ALU.mult, ALU.add, [pok, "mA", rk], [rk])
                    if dc % 2 == 1:
                        yield None

        pending = None
        wi = 0
        di = 0
        for e in range(NE):
            el = e % GE
            for tc in range(16):
                P.op("vector", lambda en, tc=tc, e=e: en.tensor_scalar(out=S[:, tc, :], in0=iota, scalar1=postok["x"][:, tc, e:e + 1],
                                                                     scalar2=None, op0=ALU.is_equal, saturate=False),
                     ["cst", "postokx"], [("S", tc)])
            if Cq is not None:
                for tc in range(2):
                    P.op("vector", lambda en, tc=tc, e=e: en.tensor_scalar(out=Sc[:, tc, :], in0=iota[:, 0:32],
                                                                         scalar1=postok["c"][:, tc, e:e + 1], scalar2=None,
                                                                         op0=ALU.is_equal, saturate=False),
                         ["cst", "postokc"], [("Sc", tc)])
            for dc in range(8):
                pg, pgk = self.ps("a")
                for tc in range(16):
                    self.mm(pg[:, 0:256], h2tok["x"][:, tc, dc * 128:(dc + 1) * 128], S[:, tc, :], tc == 0, tc == 15, ["h2tokx", ("S", tc)], [pgk])
                if Cq is not None:
                    for tc in range(2):
                        self.mm(pg[:, 256:288], h2tok["c"][:, tc, dc * 128:(dc + 1) * 128], Sc[:, tc, :], tc == 0, tc == 1,
                                ["h2tokc", ("Sc", tc)], [pgk])
                self.cp("scalar", xsT[:, dc, :], pg[:, 0:NJ], [pgk], ["xsT"])
            wd_pre = {}
            for pc in range(8):
                if pc in (5, 6):
                    q_ = pc - 5
                    rq = di % 2
                    di += 1
                    self.load_w(wdp[rq], self.I[f"wd{l}"][e, q_], ("wdp", rq))
                    wd_pre[q_] = rq
                r_ = wi % NR
                wi += 1
                self.load_w(wgp[r_], self.I[f"wg{l}"][e, pc], ("wgp", r_))
                self.load_w(wup[r_], self.I[f"wu{l}"][e, pc], ("wup", r_))
                for fc in range(2):
                    f = pc * 2 + fc
                    pa, pak = self.ps("b")
                    pu, puk = self.ps("c")
                    for dc in range(8):
                        self.mm(pa[:, 0:NJ], wgp[r_][:, dc, fc * 128:(fc + 1) * 128], xsT[:, dc, :], dc == 0, dc == 7, [("wgp", r_), "xsT"], [pak])
                    for dc in range(8):
                        self.mm(pu[:, 0:NJ], wup[r_][:, dc, fc * 128:(fc + 1) * 128], xsT[:, dc, :], dc == 0, dc == 7, [("wup", r_), "xsT"], [puk])
                    self.act(sa, pa[:, 0:NJ], AF.Silu, [pak], ["sa"])
                    self.tt("vector", actT[:, f, :], sa, pu[:, 0:NJ], ALU.mult, ["sa", puk], ["actT"])
                    for _ in range(3):
                        if pending is not None:
                            if next(pending, "done") == "done":
                                pending = None
            if pending is not None:
                for _ in pending:
                    pass
                pending = None
            for q in range(4):
                if q in wd_pre:
                    r_ = wd_pre[q]
                else:
                    r_ = di % 2
                    di += 1
                    self.load_w(wdp[r_], self.I[f"wd{l}"][e, q], ("wdp", r_))
                for jc in range(njc):
                    rows = 128 if jc < 2 else 32
                    py, pyk = self.ps("d")
                    for f in range(16):
                        self.mm(py[0:rows, 0:256], actT[:, f, jc * 128:jc * 128 + rows], wdp[r_][:, f, :], f == 0, f == 15,
                                ["actT", ("wdp", r_)], [pyk])
                    self.cp("scalar", ygrp[0:rows, el * 3 + jc, q * 256:(q + 1) * 256], py[0:rows, 0:256], [pyk], ["ygrp"])
            if el == GE - 1:
                pending = scatter_gen(e)
        if pending is not None:
            for _ in pending:
                pass
        P.barrier()
        A.reset(m0)
        self.AR.reset(mr0)

    def layer(self, l, last):
        P, A = self.P, self.A
        li = self.layers.index(l)
        A.reset(0)
        self.modulation(l)
        P.barrier()
        hx = A.alloc([8, T], BF16)
        hc = A.alloc([8, TC], BF16)
        self.mixT = A.alloc([2, T], BF16)
        self.X = Seq("x", T, self.xT, 0, True, hx, TC)
        self.C = Seq("c", TC, self.cT, 1, False, hc, 0)
        X, C = self.X, self.C
        self.norm_mod(X, 1, hx, "xh")
        self.norm_mod(C, 1, hc, "ch")
        P.barrier()
        self.dump(f"hx{l}", hx, ["xh"], [128, 8, T])
        seqs = [X] if last else [X, C]
        self.attention(l, seqs, last)
        P.barrier()
        for seq in seqs:
            self.pool_mixer(l, seq)
            P.barrier()
        for seq in seqs:
            self.conv_mixer(l, seq)
            P.barrier()
        for seq in seqs:
            self.sgu_mixer(l, seq)
            P.barrier()
        self.dump(f"xmid{l}", self.xT[:], ["xT"], [128, 8, T])
        if "nomoe" in self.dbg:
            return
        A.reset(0)
        self.moe(l, li, seqs)
        self.dump(f"xend{l}", self.xT[:], ["xT"], [128, 8, T])


_CACHE = {}


def _get_nc(layers, dbg=()):
    key = (tuple(layers), tuple(dbg))
    if key not in _CACHE:
        _CACHE[key] = KB(list(layers), dbg).build()
    return _CACHE[key]


def _run(inp, layers, cores, dbg=()):
    nc = _get_nc(layers, dbg)
    cst, rc, rs = _consts()
    shared = {"cst": cst, "rope_c": rc, "rope_s": rs}
    for l in layers:
        shared.update(_layer_arrays(inp, l))
    in_maps = []
    for b in cores:
        m = dict(shared)
        m["xT"] = np.ascontiguousarray(inp["x"][b].T)
        m["cT"] = np.ascontiguousarray(inp["ctx"][b].T)
        cv = np.stack([inp["c"][b], inp["c_ctx"]], axis=-1).astype(np.float32)
        m["cvec"] = np.ascontiguousarray(cv.reshape(8, 128, 2).transpose(1, 0, 2))
        in_maps.append(m)
    res = run_bass_kernel_spmd(nc, in_maps, core_ids=list(range(len(cores))))
    return res.results


def kernel(**inputs):
    inp = {k: np.asarray(v, dtype=np.float32) for k, v in inputs.items()}
    results = _run(inp, [0, 1], list(range(8)))
    out = np.stack([np.ascontiguousarray(r["outT"].T) for r in results], axis=0)
    return out.astype(np.float32)
```
